# Optimizing a Trainium2 kernel written in Bass

```python
import jax, jax.numpy as jnp
from jax import lax
import numpy as np

D_MODEL = 1024
BATCH = 8
SEQ = 4096
DEPTH = 4

MLA_HEADS = 4
MLA_Q_RANK = 256
MLA_KV_RANK = 128
MLA_NOPE = 64
MLA_ROPE = 32
MLA_V = 64
ROPE_THETA = 10000.0
CONV_WIDTH = 256
CONV_K = 3
DSA_HEADS = 4
DSA_HEAD_DIM = 64
IDX_HEADS = 8
IDX_DIM = 32
DSA_TOPK_MAX = 256
GLA_HEADS = 4
GLA_DK = 32
GLA_DV = 64
GLA_GATE_RANK = 16
GLA_TAU = 16.0
GLA_CHUNK = 64
N_BRANCH = 4
BRANCH_WIDTH = 256
D_FF = ((8 * D_MODEL + 3 * 256 - 1) // (3 * 256)) * 256
Q_BLOCK = 128
EPS = 1e-6
NEG = -1e30

IN_SIZES = (
    MLA_Q_RANK, MLA_KV_RANK, MLA_ROPE,
    CONV_WIDTH, CONV_WIDTH, CONV_WIDTH,
    DSA_HEADS * DSA_HEAD_DIM, DSA_HEAD_DIM, DSA_HEAD_DIM,
    IDX_HEADS * IDX_DIM, IDX_DIM, IDX_HEADS,
    GLA_HEADS * GLA_DK, GLA_HEADS * GLA_DK, GLA_HEADS * GLA_DV,
    GLA_GATE_RANK, GLA_HEADS * GLA_DV,
    N_BRANCH * D_MODEL,
)
IN_TOTAL = sum(IN_SIZES)

kernel_name = 'hybrid_gated_parallel_mixer_trunk'


def rmsnorm(x, g):
    x32 = x.astype(jnp.float32)
    y = x32 * lax.rsqrt(jnp.mean(x32 * x32, axis=-1, keepdims=True) + EPS)
    return (y * g.astype(jnp.float32)).astype(x.dtype)


def apply_rope(x, positions):
    half = x.shape[-1] // 2
    inv_freq = ROPE_THETA ** (-jnp.arange(half, dtype=jnp.float32) / half)
    ang = positions.astype(jnp.float32)[:, :, None, None] * inv_freq
    cos, sin = jnp.cos(ang), jnp.sin(ang)
    x32 = x.astype(jnp.float32)
    x1, x2 = x32[..., :half], x32[..., half:]
    return jnp.concatenate([x1 * cos - x2 * sin, x1 * sin + x2 * cos], axis=-1).astype(x.dtype)


def to_blocks(a):
    b, s = a.shape[:2]
    return jnp.moveaxis(a.reshape((b, s // Q_BLOCK, Q_BLOCK) + a.shape[2:]), 1, 0)


def from_blocks(a):
    nb, b, qb = a.shape[:3]
    return jnp.moveaxis(a, 0, 1).reshape((b, nb * qb) + a.shape[3:])


def causal_block_attention(q, k, v, scale):
    s_len = k.shape[1]
    key_pos = jnp.arange(s_len)

    def one_block(args):
        qb, start = args
        qpos = start + jnp.arange(Q_BLOCK)
        s = jnp.einsum('bqhd,bkhd->bhqk', qb, k).astype(jnp.float32) * scale
        s = jnp.where(key_pos[None, :] <= qpos[:, None], s, NEG)
        p = jax.nn.softmax(s, axis=-1).astype(v.dtype)
        return jnp.einsum('bhqk,bkhd->bqhd', p, v)

    o = lax.map(one_block, (to_blocks(q), jnp.arange(s_len // Q_BLOCK) * Q_BLOCK))
    return from_blocks(o)


def mla_branch(cq, ckv, krope, positions, q_norm_g, w_q_up, kv_norm_g, w_kv_up):
    b, s, _ = cq.shape
    q = (rmsnorm(cq, q_norm_g) @ w_q_up).reshape(b, s, MLA_HEADS, MLA_NOPE + MLA_ROPE)
    q = jnp.concatenate([q[..., :MLA_NOPE], apply_rope(q[..., MLA_NOPE:], positions)], axis=-1)
    kv = (rmsnorm(ckv, kv_norm_g) @ w_kv_up).reshape(b, s, MLA_HEADS, MLA_NOPE + MLA_V)
    k_rope = apply_rope(krope[:, :, None, :], positions)
    k = jnp.concatenate([kv[..., :MLA_NOPE], jnp.broadcast_to(k_rope, (b, s, MLA_HEADS, MLA_ROPE))], axis=-1)
    v = kv[..., MLA_NOPE:]
    o = causal_block_attention(q, k, v, (MLA_NOPE + MLA_ROPE) ** -0.5)
    return o.reshape(b, s, MLA_HEADS * MLA_V)


def shortconv_branch(cb, cc, cx, conv_w):
    u = cc * cx
    y = lax.conv_general_dilated(
        u, conv_w[:, None, :], window_strides=(1,), padding=[(CONV_K - 1, 0)],
        dimension_numbers=('NWC', 'WIO', 'NWC'), feature_group_count=CONV_WIDTH)
    return cb * y


def dsa_branch(dq, dk, dv, iq, ik, iw):
    b, s, _ = dq.shape
    topk = min(DSA_TOPK_MAX, s // 4)
    q = dq.reshape(b, s, DSA_HEADS, DSA_HEAD_DIM)
    q_idx = iq.reshape(b, s, IDX_HEADS, IDX_DIM)
    key_pos = jnp.arange(s)
    gather = jax.vmap(lambda a, i: a[i])

    def one_block(args):
        qb, qib, wb, start = args
        qpos = start + jnp.arange(Q_BLOCK)
        rel = jax.nn.relu(jnp.einsum('bqhd,bsd->bqhs', qib, ik).astype(jnp.float32) * IDX_DIM ** -0.5)
        score = jnp.einsum('bqh,bqhs->bqs', wb.astype(jnp.float32) * IDX_HEADS ** -0.5, rel)
        score = jnp.where(key_pos[None, None, :] <= qpos[None, :, None], score, NEG)
        _, idx = lax.top_k(score, topk)
        valid = idx <= qpos[None, :, None]
        k_sel = gather(dk, idx)
        v_sel = gather(dv, idx)
        att = jnp.einsum('bqhd,bqkd->bqhk', qb, k_sel).astype(jnp.float32) * DSA_HEAD_DIM ** -0.5
        att = jnp.where(valid[:, :, None, :], att, NEG)
        p = jax.nn.softmax(att, axis=-1).astype(dv.dtype)
        return jnp.einsum('bqhk,bqkd->bqhd', p, v_sel)

    o = lax.map(one_block, (to_blocks(q), to_blocks(q_idx), to_blocks(iw), jnp.arange(s // Q_BLOCK) * Q_BLOCK))
    return from_blocks(o).reshape(b, s, DSA_HEADS * DSA_HEAD_DIM)


def gla_branch(gq, gk, gv, glr, gr, w_gate_up, b_gate, norm_g):
    b, s, _ = gq.shape
    nc = s // GLA_CHUNK
    f32 = jnp.float32

    def chunks(a, d):
        return a.astype(f32).reshape(b, nc, GLA_CHUNK, GLA_HEADS, d).transpose(1, 0, 3, 2, 4)

    q = chunks(gq, GLA_DK) * GLA_DK ** -0.5
    k = chunks(gk, GLA_DK)
    v = chunks(gv, GLA_DV)
    log_alpha = jax.nn.log_sigmoid((glr @ w_gate_up + b_gate).astype(f32)) / GLA_TAU
    g = chunks(log_alpha, GLA_DK)
    causal = jnp.tril(jnp.ones((GLA_CHUNK, GLA_CHUNK), dtype=bool))

    def step(state, inp):
        qc, kc, vc, gc = inp
        cum = lax.cumsum(gc, axis=2)
        o_inter = jnp.einsum('bhik,bhkv->bhiv', qc * jnp.exp(cum), state)
        decay = jnp.exp(jnp.where(causal[:, :, None], cum[:, :, :, None, :] - cum[:, :, None, :, :], NEG))
        attn = jnp.einsum('bhik,bhjk,bhijk->bhij', qc, kc, decay)
        o_intra = jnp.einsum('bhij,bhjv->bhiv', attn, vc)
        last = cum[:, :, -1:, :]
        state = jnp.exp(last[:, :, 0, :])[..., None] * state + jnp.einsum('bhjk,bhjv->bhkv', kc * jnp.exp(last - cum), vc)
        return state, o_inter + o_intra

    state0 = jnp.zeros((b, GLA_HEADS, GLA_DK, GLA_DV), f32)
    _, o = lax.scan(step, state0, (q, k, v, g))
    o = o.transpose(1, 0, 3, 2, 4).reshape(b, s, GLA_HEADS, GLA_DV)
    o = rmsnorm(o, norm_g).astype(gr.dtype).reshape(b, s, GLA_HEADS * GLA_DV)
    return jax.nn.silu(gr) * o


def setup_inputs(seed: int = 0) -> dict:
    key = jax.random.key(seed)
    ks = jax.random.split(key, 20)
    f32 = jnp.float32

    def nrm(k, shape, scale):
        return jax.random.normal(k, shape, f32) * scale

    def gain(k, shape):
        return 1.0 + 0.05 * jax.random.normal(k, shape, f32)

    return {
        'x': nrm(ks[0], (BATCH, SEQ, D_MODEL), 1.0),
        'positions': jnp.broadcast_to(jnp.arange(SEQ, dtype=jnp.int32)[None, :], (BATCH, SEQ)),
        'attn_norm_g': gain(ks[1], (DEPTH, D_MODEL)),
        'w_in': nrm(ks[2], (DEPTH, D_MODEL, IN_TOTAL), D_MODEL ** -0.5),
        'mla_q_norm_g': gain(ks[3], (DEPTH, MLA_Q_RANK)),
        'mla_w_q_up': nrm(ks[4], (DEPTH, MLA_Q_RANK, MLA_HEADS * (MLA_NOPE + MLA_ROPE)), MLA_Q_RANK ** -0.5),
        'mla_kv_norm_g': gain(ks[5], (DEPTH, MLA_KV_RANK)),
        'mla_w_kv_up': nrm(ks[6], (DEPTH, MLA_KV_RANK, MLA_HEADS * (MLA_NOPE + MLA_V)), MLA_KV_RANK ** -0.5),
        'conv_w': nrm(ks[7], (DEPTH, CONV_K, CONV_WIDTH), CONV_K ** -0.5),
        'gla_w_gate_up': nrm(ks[8], (DEPTH, GLA_GATE_RANK, GLA_HEADS * GLA_DK), GLA_GATE_RANK ** -0.5),
        'gla_b_gate': nrm(ks[9], (DEPTH, GLA_HEADS * GLA_DK), 0.1),
        'gla_norm_g': gain(ks[10], (DEPTH, GLA_DV)),
        'w_branch': nrm(ks[11], (DEPTH, N_BRANCH, BRANCH_WIDTH, D_MODEL), BRANCH_WIDTH ** -0.5),
        'w_out': nrm(ks[12], (DEPTH, D_MODEL, D_MODEL), D_MODEL ** -0.5),
        'ffn_norm_g': gain(ks[13], (DEPTH, D_MODEL)),
        'w_ffn_gate': nrm(ks[14], (DEPTH, D_MODEL, D_FF), D_MODEL ** -0.5),
        'w_ffn_up': nrm(ks[15], (DEPTH, D_MODEL, D_FF), D_MODEL ** -0.5),
        'w_ffn_down': nrm(ks[16], (DEPTH, D_FF, D_MODEL), D_FF ** -0.5),
        'final_norm_g': gain(ks[17], (D_MODEL,)),
    }


def reference(x, positions, attn_norm_g, w_in, mla_q_norm_g, mla_w_q_up, mla_kv_norm_g, mla_w_kv_up,
              conv_w, gla_w_gate_up, gla_b_gate, gla_norm_g, w_branch, w_out, ffn_norm_g,
              w_ffn_gate, w_ffn_up, w_ffn_down, final_norm_g):
    b, s, d = x.shape
    split_points = [int(p) for p in np.cumsum(IN_SIZES)[:-1]]
    for l in range(DEPTH):
        h = rmsnorm(x, attn_norm_g[l])
        (cq, ckv, krope, cb, cc, cx, dq, dk, dv, iq, ik, iw,
         gq, gk, gv, glr, gr, gate_pre) = jnp.split(h @ w_in[l], split_points, axis=-1)

        o_mla = mla_branch(cq, ckv, krope, positions, mla_q_norm_g[l], mla_w_q_up[l],
                           mla_kv_norm_g[l], mla_w_kv_up[l])
        o_conv = shortconv_branch(cb, cc, cx, conv_w[l])
        o_dsa = dsa_branch(dq, dk, dv, iq, ik, iw)
        o_gla = gla_branch(gq, gk, gv, glr, gr, gla_w_gate_up[l], gla_b_gate[l], gla_norm_g[l])

        gates = jax.nn.sigmoid(gate_pre).reshape(b, s, N_BRANCH, d)
        branches = (o_mla, o_conv, o_dsa, o_gla)
        merged = gates[:, :, 0, :] * (branches[0] @ w_branch[l, 0])
        for n in range(1, N_BRANCH):
            merged = merged + gates[:, :, n, :] * (branches[n] @ w_branch[l, n])
        x = x + merged @ w_out[l]

        h2 = rmsnorm(x, ffn_norm_g[l])
        x = x + (jax.nn.silu(h2 @ w_ffn_gate[l]) * (h2 @ w_ffn_up[l])) @ w_ffn_down[l]
    return rmsnorm(x, final_norm_g)
```

```python
import numpy as np
from contextlib import ExitStack, contextmanager
import concourse.bass as bass
import concourse.mybir as mybir
from concourse.bass_utils import run_bass_kernel_spmd

F32 = mybir.dt.float32
BF16 = mybir.dt.bfloat16
I32 = mybir.dt.int32
AF = mybir.ActivationFunctionType
ALU = mybir.AluOpType
AX = mybir.AxisListType

D = 1024
DFF = 2816
NLAYER = 4
INTOT = 6744
EPS = 1e-6
EPOCH = 30000
NBIS = 16

C_CQ, C_CKV, C_KROPE = 0, 256, 384
C_CB, C_CC, C_CX = 416, 672, 928
C_DQ, C_DK, C_DV = 1184, 1440, 1504
C_IQ, C_IK, C_IW = 1568, 1824, 1856
C_GQ, C_GK, C_GV, C_GLR, C_GR = 1864, 1992, 2120, 2376, 2392
C_GATE = 2648


class Buf:
    def __init__(self, name, t=None):
        self.name = name
        self.t = t
        self.last_write = None
        self.readers = {}
        self.wsem = None
        self.rsem = None

    def __getitem__(self, idx):
        return self.t[idx]


class Sched:
    COMPUTE = ("pe", "act", "dve", "pool")
    SEMCAP = 30000

    def __init__(self, nc):
        self.nc = nc
        self.engs = list(self.COMPUTE) + ["sp"]
        self.ops = {e: [] for e in self.engs}
        self.cnt = {e: 0 for e in self.COMPUTE}
        self.sems = {}
        self.sem_names = []
        self.waited = {e: {} for e in self.engs}
        self.nops = 0
        self.semval = {}
        self.free_dma = []
        self.ndma_sems = 0

    def _sem(self, name):
        if name not in self.sems:
            self.sems[name] = None
            self.sem_names.append(name)
        return name

    def _dma_sem(self):
        if self.free_dma:
            return self.free_dma.pop()
        self.ndma_sems += 1
        name = self._sem(f"d_{self.ndma_sems}")
        self.semval[name] = 0
        return name

    def release(self, bufs):
        for b in bufs:
            for nm in (b.wsem, b.rsem):
                if nm is not None and self.semval[nm] < self.SEMCAP:
                    self.free_dma.append(nm)
            b.wsem = b.rsem = None

    def _compute_token(self, e):
        self.cnt[e] += 1
        k = self.cnt[e]
        ep = (k - 1) // EPOCH
        return (self._sem(f"c_{e}_{ep}"), (k - 1) % EPOCH + 1, e)

    def _need(self, e, tok, waits):
        if tok is None:
            return
        sem, val, src = tok
        if src == e and e == "pe":
            return
        w = self.waited[e]
        if w.get(sem, 0) >= val:
            return
        w[sem] = val
        waits.append((sem, val))

    def _deps(self, e, reads, writes):
        waits = []
        for b in reads:
            self._need(e, b.last_write, waits)
        for b in writes:
            self._need(e, b.last_write, waits)
            for r in b.readers.values():
                self._need(e, r, waits)
        return waits

    def _mark(self, tok, reads, writes):
        for b in reads:
            old = b.readers.get(tok[0])
            if old is None or old[1] < tok[1]:
                b.readers[tok[0]] = tok
        for b in writes:
            b.last_write = tok
            b.readers = {}

    def op(self, e, method, args, kwargs, reads=(), writes=()):
        waits = self._deps(e, reads, writes)
        tok = self._compute_token(e)
        self._mark(tok, reads, writes)
        self.ops[e].append((waits, method, args, kwargs, (tok[0], 1)))
        self.nops += 1
        return tok

    def dma(self, q, out_ap, in_ap, reads=(), writes=(), **kw):
        waits = self._deps(q, reads, writes)
        if writes:
            b = writes[0]
            if b.wsem is None:
                b.wsem = self._dma_sem()
            nm = b.wsem
        else:
            b = reads[0]
            if b.rsem is None:
                b.rsem = self._dma_sem()
            nm = b.rsem
        self.semval[nm] += 16
        tok = (nm, self.semval[nm], "dma")
        self._mark(tok, reads, writes)
        kw = dict(kw)
        kw["out"] = out_ap
        kw["in_"] = in_ap
        self.ops[q].append((waits, "dma_start", (), kw, (nm, 16)))
        self.nops += 1
        return tok

    def wait_all(self, e, bufs):
        waits = self._deps(e, (), bufs)
        if waits:
            self.ops[e].append((waits, None, None, None, None))

    def barrier(self):
        toks = []
        for e in self.COMPUTE:
            k = self.cnt[e]
            if k > 0:
                ep = (k - 1) // EPOCH
                toks.append((f"c_{e}_{ep}", (k - 1) % EPOCH + 1, e))
        for nm, v in self.semval.items():
            if v > 0:
                toks.append((nm, v, "dma"))
        for e in self.engs:
            waits = []
            for tok in toks:
                self._need(e, tok, waits)
            if waits:
                self.ops[e].append((waits, None, None, None, None))

    def emit(self, stack):
        nc = self.nc
        if not hasattr(self, "sem_stack"):
            self.sem_stack = stack
        for name in self.sem_names:
            if self.sems[name] is None:
                self.sems[name] = self.sem_stack.enter_context(nc.semaphore(name))
        if not any(self.ops[e] for e in self.engs):
            return
        with nc.Block() as block:
            amap = {"pe": block.tensor, "act": block.scalar, "dve": block.vector,
                    "pool": block.gpsimd, "sp": block.sync}
            for e in self.engs:
                ops = self.ops[e]
                if not ops:
                    continue

                def body(eng, ops=ops):
                    for waits, method, args, kwargs, inc in ops:
                        for sem, val in waits:
                            eng.wait_ge(self.sems[sem], val)
                        if method is not None:
                            ins = getattr(eng, method)(*args, **kwargs)
                            ins.then_inc(self.sems[inc[0]], inc[1])

                amap[e](body)
                self.ops[e] = []


class Rot:
    def __init__(self, bufs):
        self.bufs = bufs
        self.i = 0

    def next(self):
        b = self.bufs[self.i % len(self.bufs)]
        self.i += 1
        return b


def host_consts():
    j = np.arange(128)[:, None]
    i = np.arange(128)[None, :]
    same = (j // 64) == (i // 64)
    c = {}
    c["c_ident"] = np.eye(128, dtype=np.float32)
    c["c_tri"] = np.where(same & (j <= i), -1.0 / 16.0, 0.0).astype(np.float32)
    c["c_rev"] = np.where(same & (j > i), -1.0 / 16.0, 0.0).astype(np.float32)
    c["c_gmask"] = np.where(same & (j <= i), 1.0, 0.0).astype(np.float32)
    c["c_cmt"] = np.where(j > i, -30000.0, 0.0).astype(np.float32)
    c["c_cmq"] = np.ascontiguousarray(c["c_cmt"].T)
    rot = np.zeros((32, 32), np.float32)
    for fp in range(16):
        rot[fp + 16, fp] = -1.0
    for fp in range(16, 32):
        rot[fp - 16, fp] = 1.0
    c["c_rot"] = rot
    half = 16
    invf = (np.float32(10000.0) ** (-np.arange(half, dtype=np.float32) / np.float32(half))).astype(np.float32)
    c["c_invf"] = np.concatenate([invf, invf]).reshape(32, 1).astype(np.float32)
    return c


class Builder:
    def __init__(self, S, nl, dbg=(), stop_after=None, phases=None):
        self.S = S
        self.NT = S // 128
        self.NB = S // 512
        self.nl = nl
        self.dbg = set(dbg)
        self.stop_after = stop_after
        self.phases = phases
        self.nc = bass.Bass("TRN2", target_bir_lowering=False)
        self.s = Sched(self.nc)
        self.uid = 0
        self.live = []

    def din(self, name, shape, dt=F32):
        return self.nc.dram_tensor(name, list(shape), dt, kind="ExternalInput").ap()

    def dscr(self, name, shape, dt=BF16):
        kind = "ExternalOutput" if name in self.dbg else "Internal"
        ap = self.nc.dram_tensor(name, list(shape), dt, kind=kind).ap()
        return Buf(name, ap)

    def sb(self, st, name, shape, dt):
        self.uid += 1
        nm = f"{name}_{self.uid}"
        b = Buf(nm, st.enter_context(self.nc.sbuf_tensor(nm, list(shape), dt)))
        self.live.append(b)
        return b

    def psb(self, st, name, dt=F32):
        self.uid += 1
        nm = f"{name}_{self.uid}"
        shape = [128, 512] if dt == F32 else [128, 1024]
        b = Buf(nm, st.enter_context(self.nc.psum_tensor(nm, shape, dt)))
        self.live.append(b)
        return b

    @contextmanager
    def scope(self):
        st = ExitStack()
        mark = len(self.live)
        yield st
        self.s.barrier()
        self.s.release(self.live[mark:])
        del self.live[mark:]
        self.s.emit(self.top)
        st.close()

    def mm(self, out, lhsT, rhs, start, stop, reads, writes):
        self.s.op("pe", "matmul", (out,), dict(lhsT=lhsT, rhs=rhs, start=start, stop=stop), reads, writes)

    def tr(self, out, in_, reads, writes):
        self.s.op("pe", "transpose", (), dict(out=out, in_=in_, identity=self.identb[:]), list(reads) + [self.identb], writes)

    def act(self, out, in_, func, reads, writes, **kw):
        self.s.op("act", "activation", (), dict(out=out, in_=in_, func=func, **kw), reads, writes)

    def tt(self, eng, out, in0, in1, op, reads, writes):
        self.s.op(eng, "tensor_tensor", (), dict(out=out, in0=in0, in1=in1, op=op), reads, writes)

    def ts(self, eng, out, in0, s1, s2, op0, op1=None, reads=(), writes=(), accum_out=None):
        kw = dict(out=out, in0=in0, scalar1=s1, scalar2=s2, op0=op0)
        if op1 is not None:
            kw["op1"] = op1
        if accum_out is not None:
            kw["accum_out"] = accum_out
        self.s.op(eng, "tensor_scalar", (), kw, reads, writes)

    def stt(self, out, in0, scalar, in1, op0, op1, reads, writes):
        self.s.op("dve", "scalar_tensor_tensor", (), dict(out=out, in0=in0, scalar=scalar, in1=in1, op0=op0, op1=op1), reads, writes)

    def cp(self, eng, out, in_, reads, writes):
        self.s.op(eng, "tensor_copy", (), dict(out=out, in_=in_), reads, writes)

    def ms(self, eng, ap, val, writes):
        self.s.op(eng, "memset", (ap, val), {}, (), writes)

    def recip(self, out, in_, reads, writes):
        self.s.op("dve", "reciprocal", (), dict(out=out, in_=in_), reads, writes)

    def build(self):
        nc, s, S, NT = self.nc, self.s, self.S, self.NT
        self.x_in = Buf("x", self.din("x", [S, D]))
        self.pos = Buf("pos", self.din("pos", [S], I32))
        self.w_in = Buf("w_in", self.din("w_in", [NLAYER, D, INTOT]))
        self.w_qup = Buf("w_qup", self.din("w_qup", [NLAYER, 256, 384]))
        self.w_kvup = Buf("w_kvup", self.din("w_kvup", [NLAYER, 128, 512]))
        self.w_br = Buf("w_br", self.din("w_br", [NLAYER, 4, 256, D]))
        self.w_out = Buf("w_out", self.din("w_out", [NLAYER, D, D]))
        self.w_fg = Buf("w_fg", self.din("w_fg", [NLAYER, D, DFF]))
        self.w_fu = Buf("w_fu", self.din("w_fu", [NLAYER, D, DFF]))
        self.w_fd = Buf("w_fd", self.din("w_fd", [NLAYER, DFF, D]))
        self.g_attn = Buf("g_attn", self.din("g_attn", [128, NLAYER, 8]))
        self.g_ffn = Buf("g_ffn", self.din("g_ffn", [128, NLAYER, 8]))
        self.g_final = Buf("g_final", self.din("g_final", [D]))
        self.g_q = Buf("g_q", self.din("g_q", [128, NLAYER, 2]))
        self.g_kv = Buf("g_kv", self.din("g_kv", [128, NLAYER]))
        self.conv_w = Buf("conv_w", self.din("conv_w", [128, NLAYER, 2, 3]))
        self.gla_wg = Buf("gla_wg", self.din("gla_wg", [NLAYER, 16, 128]))
        self.gla_bg = Buf("gla_bg", self.din("gla_bg", [NLAYER, 1, 128]))
        self.gla_ng = Buf("gla_ng", self.din("gla_ng", [NLAYER, 64]))
        cin = {k: Buf(k, self.din(k, list(v.shape))) for k, v in host_consts().items()}
        self.y_out = Buf("y", nc.dram_tensor("y", [S, D], F32, kind="ExternalOutput").ap())
        self.xres = self.dscr("xres", [S, D], F32)
        self.cosT = self.dscr("cosT", [32, S], F32)
        self.sinT = self.dscr("sinT", [32, S], F32)
        self.cqT = self.dscr("cqT", [256, S])
        self.ckvT = self.dscr("ckvT", [128, S])
        self.kropeT = self.dscr("kropeT", [32, S])
        self.cbT = self.dscr("cbT", [256, S])
        self.ccT = self.dscr("ccT", [256, S])
        self.cxT = self.dscr("cxT", [256, S])
        self.dqT = self.dscr("dqT", [64, 4, S])
        self.dkT = self.dscr("dkT", [64, S])
        self.iqT = self.dscr("iqT", [32, 8, S])
        self.ikT = self.dscr("ikT", [32, S])
        self.gqT = self.dscr("gqT", [32, 4, S])
        self.gkT = self.dscr("gkT", [32, 4, S])
        self.glrT = self.dscr("glrT", [17, S])
        self.gateT = self.dscr("gateT", [4096, S])
        self.dvaug = self.dscr("dvaug", [S, 65])
        self.iw = self.dscr("iw", [S, 8], F32)
        self.gk = self.dscr("gk", [S, 128])
        self.gv = self.dscr("gv", [S, 256])
        self.grs = self.dscr("grs", [S, 256])
        self.qT = self.dscr("qT", [4, 96, S])
        self.kT = self.dscr("kT", [4, 96, S])
        self.vaug = self.dscr("vaug", [S, 4, 65])
        self.oT = self.dscr("oT", [4, 256, S])
        self.dsc = self.dscr("dsc", [S, S], F32) if "dsc" in self.dbg else None

        with ExitStack() as top:
            self.top = top
            cst = {}
            for k in ("c_ident", "c_tri", "c_rev", "c_gmask", "c_cmt", "c_cmq"):
                cst[k] = self.sb(top, k, [128, 128], F32)
                s.dma("sp", cst[k][:], cin[k][:], reads=[cin[k]], writes=[cst[k]])
            self.identf = cst["c_ident"]
            self.tri = cst["c_tri"]
            self.rev = cst["c_rev"]
            self.identb = self.sb(top, "identb", [128, 128], BF16)
            self.gmaskb = self.sb(top, "gmaskb", [128, 128], BF16)
            self.cmtb = self.sb(top, "cmtb", [128, 128], BF16)
            self.cmqb = self.sb(top, "cmqb", [128, 128], BF16)
            self.i4b = self.sb(top, "i4b", [128, 512], BF16)
            self.onesb = self.sb(top, "onesb", [128, 128], BF16)
            self.epsb = self.sb(top, "epsb", [128, 1], F32)
            self.rotb = self.sb(top, "rotb", [32, 32], BF16)
            rotf = self.sb(top, "rotf", [32, 32], F32)
            self.invf = self.sb(top, "invf", [32, 1], F32)
            s.dma("sp", rotf[:], cin["c_rot"][:], reads=[cin["c_rot"]], writes=[rotf])
            s.dma("sp", self.invf[:], cin["c_invf"][:], reads=[cin["c_invf"]], writes=[self.invf])
            self.cp("dve", self.identb[:], self.identf[:], [self.identf], [self.identb])
            self.cp("dve", self.gmaskb[:], cst["c_gmask"][:], [cst["c_gmask"]], [self.gmaskb])
            self.cp("dve", self.cmtb[:], cst["c_cmt"][:], [cst["c_cmt"]], [self.cmtb])
            self.cp("dve", self.cmqb[:], cst["c_cmq"][:], [cst["c_cmq"]], [self.cmqb])
            self.cp("dve", self.rotb[:], rotf[:], [rotf], [self.rotb])
            for h in range(4):
                self.cp("dve", self.i4b[:, h * 128:(h + 1) * 128], self.identf[:], [self.identf], [self.i4b])
            self.ms("dve", self.onesb[:], 1.0, [self.onesb])
            self.ms("dve", self.epsb[:], EPS, [self.epsb])
            self.gattn = self.sb(top, "gattn", [128, NLAYER, 8], F32)
            self.gffn = self.sb(top, "gffn", [128, NLAYER, 8], F32)
            self.gq = self.sb(top, "gq", [128, NLAYER, 2], F32)
            self.gkv = self.sb(top, "gkv", [128, NLAYER], F32)
            self.convw = self.sb(top, "convw", [128, NLAYER, 2, 3], F32)
            for dst, srcb in ((self.gattn, self.g_attn), (self.gffn, self.g_ffn), (self.gq, self.g_q),
                              (self.gkv, self.g_kv), (self.convw, self.conv_w)):
                s.dma("sp", dst[:], srcb[:], reads=[srcb], writes=[dst])
            s.emit(top)

            allph = "RABCEDFGH"
            ph = self.phases if self.phases is not None else allph
            if "R" in ph:
                self.phase_rope()
            done = False
            for l in range(self.nl):
                for name, fn in (("A", self.phase_A), ("B", self.phase_B), ("C", self.phase_C), ("E", self.phase_E),
                                 ("D", self.phase_D), ("F", self.phase_F), ("G", self.phase_G), ("H", self.phase_H)):
                    if name in ph:
                        fn(l)
                    if self.stop_after == (l, name):
                        done = True
                        break
                if done:
                    break
            if not done:
                self.phase_final()
            outs = [self.y_out] + [getattr(self, n) for n in self.dbg if getattr(self, n, None) is not None]
            s.wait_all("sp", outs)
            s.wait_all("pool", outs)
            s.barrier()
            s.emit(top)
        return nc

    def phase_rope(self):
        s, S = self.s, self.S
        W = min(S, 1024)
        with self.scope() as st:
            pi = self.sb(st, "pi", [32, W], I32)
            pf = self.sb(st, "pf", [32, W], F32)
            pk = self.sb(st, "pk", [32, W], I32)
            pkf = self.sb(st, "pkf", [32, W], F32)
            pc = self.sb(st, "pc", [32, W], F32)
            pa = self.sb(st, "pa", [32, W], F32)
            res = self.sb(st, "res", [32, W], F32)
            for c0 in range(0, S, W):
                s.dma("sp", pi[:], self.pos[c0:c0 + W].partition_broadcast(32), reads=[self.pos], writes=[pi])
                self.cp("dve", pf[:], pi[:], [pi], [pf])
                self.ts("dve", pf[:], pf[:], self.invf[:, 0:1], None, ALU.mult, reads=[pf, self.invf], writes=[pf])
                for shift, dst in ((0.0, self.sinT), (0.25, self.cosT)):
                    self.ts("dve", pa[:], pf[:], float(1.0 / (2 * np.pi)), shift, ALU.mult, ALU.add, reads=[pf], writes=[pa])
                    self.cp("dve", pk[:], pa[:], [pa], [pk])
                    self.cp("dve", pkf[:], pk[:], [pk], [pkf])
                    self.tt("dve", pa[:], pa[:], pkf[:], ALU.subtract, [pa, pkf], [pa])
                    self.ts("dve", pc[:], pa[:], 0.5, None, ALU.is_gt, reads=[pa], writes=[pc])
                    self.tt("dve", pa[:], pa[:], pc[:], ALU.subtract, [pa, pc], [pa])
                    self.ts("dve", pc[:], pa[:], -0.5, None, ALU.is_lt, reads=[pa], writes=[pc])
                    self.tt("dve", pa[:], pa[:], pc[:], ALU.add, [pa, pc], [pa])
                    self.act(res[:], pa[:], AF.Sin, [pa], [res], scale=float(2 * np.pi))
                    s.dma("pool", dst[:, c0:c0 + W], res[:], reads=[res], writes=[dst])

    def norm_tiles(self, st):
        return dict(
            xt=Rot([self.sb(st, "xt", [128, D], F32) for _ in range(3)]),
            xs=Rot([self.sb(st, "xs", [128, D], BF16) for _ in range(2)]),
            junk=self.sb(st, "junk", [128, D], BF16),
            ss=Rot([self.sb(st, "ss", [128, 1], F32) for _ in range(3)]),
            rs=Rot([self.sb(st, "rs", [128, 1], F32) for _ in range(3)]),
            psT=Rot([self.psb(st, "psT", BF16) for _ in range(2)]))

    def norm_to_hT(self, nt, src, g_tile, l, hT, tok0, ntiles):
        s = self.s
        for t in range(ntiles):
            xt, xs, ss, rs = nt["xt"].next(), nt["xs"].next(), nt["ss"].next(), nt["rs"].next()
            junk = nt["junk"]
            r0 = tok0 + t * 128
            s.dma("sp", xt[:], src[r0:r0 + 128, :], reads=[src], writes=[xt])
            self.act(junk[:], xt[:], AF.Square, [xt], [junk, ss], accum_out=ss[:])
            self.act(rs[:], ss[:], AF.Sqrt, [ss, self.epsb], [rs], scale=1.0 / D, bias=self.epsb[:, 0:1])
            self.recip(rs[:], rs[:], [rs], [rs])
            self.ts("dve", xs[:], xt[:], rs[:, 0:1], None, ALU.mult, reads=[xt, rs], writes=[xs])
            pT = nt["psT"].next()
            for k in range(8):
                self.tr(pT[:, k * 128:(k + 1) * 128], xs[:, k * 128:(k + 1) * 128], [xs], [pT])
            self.tt("dve", hT[:, :, t * 128:(t + 1) * 128], pT[:, :].rearrange("p (k q) -> p k q", k=8),
                    g_tile[:, l, :].unsqueeze(2).to_broadcast([128, 8, 128]), ALU.mult, [pT, g_tile], [hT])

    def phase_A(self, l):
        s, S, NT, NB = self.s, self.S, self.NT, self.NB
        src = self.x_in if l == 0 else self.xres
        with self.scope() as st:
            hT = self.sb(st, "hT", [128, 8, S], BF16)
            with self.scope() as st1:
                ones_row = self.sb(st1, "ones_row", [1, S], BF16)
                self.ms("dve", ones_row[:], 1.0, [ones_row])
                s.dma("pool", self.glrT[16:17, :], ones_row[:], reads=[ones_row], writes=[self.glrT])
                nt = self.norm_tiles(st1)
                self.norm_to_hT(nt, src, self.gattn, l, hT, 0, NT)
            ps = Rot([self.psb(st, "psA") for _ in range(4)])
            wst_r = Rot([self.sb(st, "wst", [128, 8, 256], F32) for _ in range(2)])
            wbf_r = Rot([self.sb(st, "wbf", [128, 8, 256], BF16) for _ in range(2)])
            stg_r = Rot([self.sb(st, "stg", [128, S], BF16) for _ in range(3)])
            evac = [0]

            def load_w(c0, n):
                wst, wbf = wst_r.next(), wbf_r.next()
                s.dma("sp", wst[:, :, 0:n], self.w_in[l, :, c0:c0 + n].rearrange("(k p) n -> p k n", p=128),
                      reads=[self.w_in], writes=[wst])
                self.cp("pool", wbf[:, :, 0:n], wst[:, :, 0:n], [wst], [wbf])
                return wbf

            def fm_group(wbf, n, outs, func):
                stg = stg_r.next()
                for b in range(NB):
                    p = ps.next()
                    for k in range(8):
                        self.mm(p[0:n, :], wbf[:, k, 0:n], hT[:, k, b * 512:(b + 1) * 512], k == 0, k == 7, [wbf, hT], [p])
                    evac[0] += 1
                    if func != AF.Copy or evac[0] % 2 == 0:
                        self.act(stg[0:n, b * 512:(b + 1) * 512], p[0:n, :], func, [p], [stg])
                    else:
                        self.cp("dve", stg[0:n, b * 512:(b + 1) * 512], p[0:n, :], [p], [stg])
                for (r0, r1, dbuf, dap) in outs:
                    s.dma("pool", dap, stg[r0:r1, :], reads=[stg], writes=[dbuf])

            fm = []
            for g in range(2):
                fm.append((C_CQ + 128 * g, 128, [(0, 128, self.cqT, self.cqT[128 * g:128 * (g + 1), :])], AF.Copy))
            fm.append((C_CKV, 128, [(0, 128, self.ckvT, self.ckvT[:, :])], AF.Copy))
            fm.append((C_KROPE, 32, [(0, 32, self.kropeT, self.kropeT[:, :])], AF.Copy))
            for c0, dst in ((C_CB, self.cbT), (C_CC, self.ccT), (C_CX, self.cxT)):
                for g in range(2):
                    fm.append((c0 + 128 * g, 128, [(0, 128, dst, dst[128 * g:128 * (g + 1), :])], AF.Copy))
            for g in range(2):
                fm.append((C_DQ + 128 * g, 128, [(64 * j, 64 * (j + 1), self.dqT, self.dqT[:, 2 * g + j, :]) for j in range(2)], AF.Copy))
            fm.append((C_DK, 64, [(0, 64, self.dkT, self.dkT[:, :])], AF.Copy))
            for g in range(2):
                fm.append((C_IQ + 128 * g, 128, [(32 * j, 32 * (j + 1), self.iqT, self.iqT[:, 4 * g + j, :]) for j in range(4)], AF.Copy))
            fm.append((C_IK, 32, [(0, 32, self.ikT, self.ikT[:, :])], AF.Copy))
            fm.append((C_GQ, 128, [(32 * j, 32 * (j + 1), self.gqT, self.gqT[:, j, :]) for j in range(4)], AF.Copy))
            fm.append((C_GK, 128, [(32 * j, 32 * (j + 1), self.gkT, self.gkT[:, j, :]) for j in range(4)], AF.Copy))
            fm.append((C_GLR, 16, [(0, 16, self.glrT, self.glrT[0:16, :])], AF.Copy))
            for g in range(32):
                fm.append((C_GATE + 128 * g, 128, [(0, 128, self.gateT, self.gateT[128 * g:128 * (g + 1), :])], AF.Sigmoid))
            nxt = load_w(fm[0][0], fm[0][1])
            for i, (c0, n, outs, func) in enumerate(fm):
                wbf = nxt
                if i + 1 < len(fm):
                    nxt = load_w(fm[i + 1][0], fm[i + 1][1])
                fm_group(wbf, n, outs, func)
            s.emit(self.top)

            tms = [(C_DV, 64, self.dvaug, BF16, AF.Copy, True), (C_IW, 8, self.iw, F32, AF.Copy, False),
                   (C_GK, 128, self.gk, BF16, AF.Copy, False), (C_GV, 256, self.gv, BF16, AF.Copy, False),
                   (C_GR, 256, self.grs, BF16, AF.Silu, False)]
            for (c0, n, dst, dt, func, ones_col) in tms:
                wbf = load_w(c0, n)
                nn = n + (1 if ones_col else 0)
                tstg = self.sb(st, "tstg", [128, NT, nn], dt)
                if ones_col:
                    self.ms("dve", tstg[:, :, n:n + 1], 1.0, [tstg])
                for t in range(NT):
                    p = ps.next()
                    for k in range(8):
                        self.mm(p[:, 0:n], hT[:, k, t * 128:(t + 1) * 128], wbf[:, k, 0:n], k == 0, k == 7, [wbf, hT], [p])
                    self.act(tstg[:, t, 0:n], p[:, 0:n], func, [p], [tstg])
                s.dma("pool", dst[:, :].rearrange("(t p) n -> p t n", p=128), tstg[:], reads=[tstg], writes=[dst])

    def phase_B(self, l):
        s, S, NT, NB = self.s, self.S, self.NT, self.NB
        with self.scope() as st:
            wqf = self.sb(st, "wqf", [128, 2, 384], F32)
            wqb = self.sb(st, "wqb", [128, 2, 384], BF16)
            wkf = self.sb(st, "wkf", [128, 512], F32)
            wkb = self.sb(st, "wkb", [128, 512], BF16)
            s.dma("sp", wqf[:], self.w_qup[l].rearrange("(k p) n -> p k n", p=128), reads=[self.w_qup], writes=[wqf])
            s.dma("sp", wkf[:], self.w_kvup[l], reads=[self.w_kvup], writes=[wkf])
            for k in range(2):
                self.ts("dve", wqb[:, k, :], wqf[:, k, :], self.gq[:, l, k:k + 1], None, ALU.mult, reads=[wqf, self.gq], writes=[wqb])
            self.ts("dve", wkb[:], wkf[:], self.gkv[:, l:l + 1], None, ALU.mult, reads=[wkf, self.gkv], writes=[wkb])
            ps = Rot([self.psb(st, "psB") for _ in range(6)])
            cq_r = Rot([self.sb(st, "cqb", [128, 2, 512], BF16) for _ in range(2)])
            ckv_r = Rot([self.sb(st, "ckvb", [128, 512], BF16) for _ in range(2)])
            kr_r = Rot([self.sb(st, "krb", [32, 512], BF16) for _ in range(2)])
            cos_r = Rot([self.sb(st, "cosb", [32, 512], F32) for _ in range(2)])
            sin_r = Rot([self.sb(st, "sinb", [32, 512], F32) for _ in range(2)])
            sq_r = Rot([self.sb(st, "sqb", [128, 2, 512], BF16) for _ in range(2)])
            sqk_r = Rot([self.sb(st, "sqk", [128, 512], BF16) for _ in range(2)])
            rq_r = Rot([self.sb(st, "rq", [128, 512], F32) for _ in range(2)])
            rk_r = Rot([self.sb(st, "rk", [128, 512], F32) for _ in range(2)])
            rkt_r = Rot([self.sb(st, "rkt", [128, 4], F32) for _ in range(2)])
            qo_r = Rot([self.sb(st, "qo", [64, 512], BF16) for _ in range(3)])
            xr_r = Rot([self.sb(st, "xr", [32, 512], BF16) for _ in range(3)])
            t1_r = Rot([self.sb(st, "t1", [32, 512], F32) for _ in range(3)])
            t2_r = Rot([self.sb(st, "t2", [32, 512], F32) for _ in range(3)])
            ro_r = Rot([self.sb(st, "ro", [32, 512], BF16) for _ in range(3)])
            vst_r = Rot([self.sb(st, "vst", [128, 4, 4, 65], BF16) for _ in range(2)])
            for v in vst_r.bufs:
                self.ms("dve", v[:, :, :, 64:65], 1.0, [v])

            def rope(xr, cosb, sinb, dsts):
                p = ps.next()
                self.mm(p[0:32, :], self.rotb[:], xr[:], True, True, [self.rotb, xr], [p])
                t1, t2, ro = t1_r.next(), t2_r.next(), ro_r.next()
                self.tt("pool", t1[:], xr[:], cosb[:], ALU.mult, [xr, cosb], [t1])
                self.tt("dve", t2[:], p[0:32, :], sinb[:], ALU.mult, [p, sinb], [t2])
                self.tt("dve", ro[:], t1[:], t2[:], ALU.add, [t1, t2], [ro])
                for dbuf, dap in dsts:
                    s.dma("pool", dap, ro[:], reads=[ro], writes=[dbuf])

            for b in range(NB):
                c0, c1 = b * 512, (b + 1) * 512
                cq, ckv, kr, cosb, sinb = cq_r.next(), ckv_r.next(), kr_r.next(), cos_r.next(), sin_r.next()
                s.dma("sp", cq[:], self.cqT[:, c0:c1].rearrange("(k p) t -> p k t", p=128), reads=[self.cqT], writes=[cq])
                s.dma("sp", ckv[:], self.ckvT[:, c0:c1], reads=[self.ckvT], writes=[ckv])
                s.dma("sp", kr[:], self.kropeT[:, c0:c1], reads=[self.kropeT], writes=[kr])
                s.dma("sp", cosb[:], self.cosT[:, c0:c1], reads=[self.cosT], writes=[cosb])
                s.dma("sp", sinb[:], self.sinT[:, c0:c1], reads=[self.sinT], writes=[sinb])
                sq, sqk, rq, rk, rkt = sq_r.next(), sqk_r.next(), rq_r.next(), rk_r.next(), rkt_r.next()
                self.tt("pool", sq[:], cq[:], cq[:], ALU.mult, [cq], [sq])
                p = ps.next()
                for k in range(2):
                    self.mm(p[:, :], self.onesb[:], sq[:, k, :], k == 0, k == 1, [self.onesb, sq], [p])
                self.act(rq[:], p[:, :], AF.Sqrt, [p, self.epsb], [rq], scale=1.0 / 256, bias=self.epsb[:, 0:1])
                self.recip(rq[:], rq[:], [rq], [rq])
                self.tt("pool", sqk[:], ckv[:], ckv[:], ALU.mult, [ckv], [sqk])
                p2 = ps.next()
                self.mm(p2[:, :], self.onesb[:], sqk[:], True, True, [self.onesb, sqk], [p2])
                self.act(rk[:], p2[:, :], AF.Sqrt, [p2, self.epsb], [rk], scale=1.0 / 128, bias=self.epsb[:, 0:1])
                self.recip(rk[:], rk[:], [rk], [rk])
                p3 = ps.next()
                for tt_ in range(4):
                    self.mm(p3[:, tt_:tt_ + 1], sqk[:, tt_ * 128:(tt_ + 1) * 128], self.onesb[:, 0:1], True, True, [self.onesb, sqk], [p3])
                self.act(rkt[:], p3[:, 0:4], AF.Sqrt, [p3, self.epsb], [rkt], scale=1.0 / 128, bias=self.epsb[:, 0:1])
                self.recip(rkt[:], rkt[:], [rkt], [rkt])
                for h in range(4):
                    pn = ps.next()
                    for k in range(2):
                        self.mm(pn[0:64, :], wqb[:, k, h * 96:h * 96 + 64], cq[:, k, :], k == 0, k == 1, [wqb, cq], [pn])
                    qo = qo_r.next()
                    self.tt("dve", qo[:], pn[0:64, :], rq[0:64, :], ALU.mult, [pn, rq], [qo])
                    s.dma("pool", self.qT[h, 0:64, c0:c1], qo[:], reads=[qo], writes=[self.qT])
                    pr = ps.next()
                    for k in range(2):
                        self.mm(pr[0:32, :], wqb[:, k, h * 96 + 64:h * 96 + 96], cq[:, k, :], k == 0, k == 1, [wqb, cq], [pr])
                    xr = xr_r.next()
                    self.tt("dve", xr[:], pr[0:32, :], rq[0:32, :], ALU.mult, [pr, rq], [xr])
                    rope(xr, cosb, sinb, [(self.qT, self.qT[h, 64:96, c0:c1])])
                for h in range(4):
                    pn = ps.next()
                    self.mm(pn[0:64, :], wkb[:, h * 128:h * 128 + 64], ckv[:], True, True, [wkb, ckv], [pn])
                    qo = qo_r.next()
                    self.tt("dve", qo[:], pn[0:64, :], rk[0:64, :], ALU.mult, [pn, rk], [qo])
                    s.dma("pool", self.kT[h, 0:64, c0:c1], qo[:], reads=[qo], writes=[self.kT])
                rope(kr, cosb, sinb, [(self.kT, self.kT[h, 64:96, c0:c1]) for h in range(4)])
                vst = vst_r.next()
                for tt_ in range(4):
                    pv = ps.next()
                    self.mm(pv[:, :], ckv[:, tt_ * 128:(tt_ + 1) * 128], wkb[:], True, True, [wkb, ckv], [pv])
                    self.act(vst[:, tt_, :, 0:64], pv[:, :].rearrange("p (h c) -> p h c", h=4)[:, :, 64:128], AF.Copy, [pv, rkt], [vst],
                             scale=rkt[:, tt_:tt_ + 1])
                s.dma("pool", self.vaug[c0:c1, :, :].rearrange("(t p) h c -> p t h c", p=128), vst[:], reads=[vst], writes=[self.vaug])

    def attn_finish(self, oacc, obuf_r, oT_r, psT_r, branch, t):
        rec = self.rec_r.next()
        o = obuf_r.next()
        acc3 = oacc[:, 0:260].rearrange("p (h c) -> p h c", h=4)
        self.recip(rec[:, :], acc3[:, :, 64], [oacc], [rec])
        self.tt("dve", o[:].rearrange("p (h c) -> p h c", h=4), acc3[:, :, 0:64],
                rec[:, :].unsqueeze(2).to_broadcast([128, 4, 64]), ALU.mult, [oacc, rec], [o])
        self.to_oT(o, oT_r, psT_r, branch, t)

    def to_oT(self, o, oT_r, psT_r, branch, t):
        s = self.s
        pT = psT_r.next()
        for k in range(2):
            self.tr(pT[:, k * 128:(k + 1) * 128], o[:, k * 128:(k + 1) * 128], [o], [pT])
        oTt = oT_r.next()
        self.act(oTt[:].rearrange("p k q -> p (k q)"), pT[:, 0:256], AF.Copy, [pT], [oTt])
        s.dma("pool", self.oT[branch, :, t * 128:(t + 1) * 128].rearrange("(k p) q -> p k q", p=128), oTt[:], reads=[oTt], writes=[self.oT])

    def phase_C(self, l):
        s, S, NT = self.s, self.S, self.NT
        scale = float(96 ** -0.5)
        with self.scope() as st:
            kT = self.sb(st, "kTs", [96, 4, S], BF16)
            qT = self.sb(st, "qTs", [96, 4, S], BF16)
            va = self.sb(st, "vas", [128, NT, 4, 65], BF16)
            for h in range(4):
                s.dma("sp", kT[:, h, :], self.kT[h], reads=[self.kT], writes=[kT])
                s.dma("sp", qT[:, h, :], self.qT[h], reads=[self.qT], writes=[qT])
            s.dma("sp", va[:], self.vaug[:, :, :].rearrange("(t p) h c -> p t h c", p=128), reads=[self.vaug], writes=[va])
            ps_st = Rot([self.psb(st, "psS") for _ in range(3)])
            ps_o = Rot([self.psb(st, "psO") for _ in range(2)])
            psT_r = Rot([self.psb(st, "psT", BF16) for _ in range(1)])
            pT_r = Rot([self.sb(st, "pT", [128, 512], BF16) for _ in range(3)])
            self.rec_r = Rot([self.sb(st, "rec", [128, 4], F32) for _ in range(2)])
            obuf_r = Rot([self.sb(st, "ob", [128, 256], BF16) for _ in range(2)])
            oT_r = Rot([self.sb(st, "oTt", [128, 2, 128], BF16) for _ in range(2)])
            for t in range(NT):
                oacc = ps_o.next()
                q0, q1 = t * 128, (t + 1) * 128
                for j in range(t + 1):
                    k0, k1 = j * 128, (j + 1) * 128
                    sp = ps_st.next()
                    for h in range(4):
                        self.mm(sp[:, h * 128:(h + 1) * 128], kT[:, h, k0:k1], qT[:, h, q0:q1], h == 0, j != t, [kT, qT], [sp])
                    if j == t:
                        for h in range(4):
                            self.mm(sp[:, h * 128:(h + 1) * 128], self.identb[:], self.cmtb[:], False, True, [self.identb, self.cmtb], [sp])
                    pT = pT_r.next()
                    self.act(pT[:], sp[:, :], AF.Exp, [sp], [pT], scale=scale)
                    for h in range(4):
                        self.mm(oacc[:, h * 65:(h + 1) * 65], pT[:, h * 128:(h + 1) * 128], va[:, j, h, :], (j == 0 and h == 0), j == t, [pT, va], [oacc])
                self.attn_finish(oacc, obuf_r, oT_r, psT_r, 0, t)
                if t % 8 == 7:
                    s.emit(self.top)

    def phase_E(self, l):
        s, S = self.s, self.S
        cw = self.convw
        with self.scope() as st:
            cb = self.sb(st, "cb", [128, S], BF16)
            cc = self.sb(st, "cc", [128, S], BF16)
            cx = self.sb(st, "cx", [128, S], BF16)
            u = self.sb(st, "u", [128, S + 2], F32)
            y = self.sb(st, "y", [128, S], F32)
            o = self.sb(st, "o", [128, S], BF16)
            for c in range(2):
                r0, r1 = c * 128, (c + 1) * 128
                s.dma("sp", cb[:], self.cbT[r0:r1, :], reads=[self.cbT], writes=[cb])
                s.dma("sp", cc[:], self.ccT[r0:r1, :], reads=[self.ccT], writes=[cc])
                s.dma("sp", cx[:], self.cxT[r0:r1, :], reads=[self.cxT], writes=[cx])
                self.ms("pool", u[:, 0:2], 0.0, [u])
                self.tt("pool", u[:, 2:S + 2], cc[:], cx[:], ALU.mult, [cc, cx], [u])
                self.ts("dve", y[:], u[:, 2:S + 2], cw[:, l, c, 2:3], None, ALU.mult, reads=[u, cw], writes=[y])
                self.stt(y[:], u[:, 1:S + 1], cw[:, l, c, 1:2], y[:], ALU.mult, ALU.add, [u, cw, y], [y])
                self.stt(y[:], u[:, 0:S], cw[:, l, c, 0:1], y[:], ALU.mult, ALU.add, [u, cw, y], [y])
                self.tt("pool", o[:], cb[:], y[:], ALU.mult, [cb, y], [o])
                s.dma("pool", self.oT[1, r0:r1, :], o[:], reads=[o], writes=[self.oT])

    def phase_D(self, l):
        s, S, NT = self.s, self.S, self.NT
        cidx = float(32 ** -0.5 * 8 ** -0.5)
        scale = float(64 ** -0.5)
        with self.scope() as st:
            iqT = self.sb(st, "iqTs", [32, 8, S], BF16)
            ikT = self.sb(st, "ikTs", [32, S], BF16)
            dkT = self.sb(st, "dkTs", [64, S], BF16)
            dqT = self.sb(st, "dqTs", [64, NT, 4, 128], BF16)
            dva = self.sb(st, "dvas", [128, NT, 65], BF16)
            iw = self.sb(st, "iws", [128, NT, 8], F32)
            for h in range(8):
                s.dma("sp", iqT[:, h, :], self.iqT[:, h, :], reads=[self.iqT], writes=[iqT])
            s.dma("sp", ikT[:], self.ikT[:, :], reads=[self.ikT], writes=[ikT])
            s.dma("sp", dkT[:], self.dkT[:, :], reads=[self.dkT], writes=[dkT])
            for h in range(4):
                s.dma("sp", dqT[:, :, h, :], self.dqT[:, h, :].rearrange("d (t q) -> d t q", q=128), reads=[self.dqT], writes=[dqT])
            s.dma("sp", dva[:], self.dvaug[:, :].rearrange("(t p) c -> p t c", p=128), reads=[self.dvaug], writes=[dva])
            s.dma("sp", iw[:], self.iw[:, :].rearrange("(t p) c -> p t c", p=128), reads=[self.iw], writes=[iw])
            sc = self.sb(st, "sc", [128, S], F32)
            mb = self.sb(st, "mb", [128, S], BF16)
            junk = self.sb(st, "junkd", [128, S], BF16)
            ps_rel = Rot([self.psb(st, "psR") for _ in range(2)])
            ps_sc = Rot([self.psb(st, "psSc") for _ in range(1)])
            ps_st = Rot([self.psb(st, "psS") for _ in range(2)])
            ps_o = Rot([self.psb(st, "psO") for _ in range(2)])
            psT_r = Rot([self.psb(st, "psT", BF16) for _ in range(1)])
            rl_r = Rot([self.sb(st, "rl", [128, 512], BF16) for _ in range(4)])
            dm_r = Rot([self.sb(st, "dm", [128, 8, 128], BF16) for _ in range(2)])
            pT_r = Rot([self.sb(st, "pT", [128, 512], BF16) for _ in range(3)])
            self.rec_r = Rot([self.sb(st, "rec", [128, 4], F32) for _ in range(2)])
            obuf_r = Rot([self.sb(st, "ob", [128, 256], BF16) for _ in range(2)])
            oT_r = Rot([self.sb(st, "oTt", [128, 2, 128], BF16) for _ in range(2)])
            sm = {k: self.sb(st, k, [128, 1], F32) for k in ("mx", "w0", "lo", "mid", "cnt", "tmp", "thr")}
            for t in range(NT):
                q0, q1 = t * 128, (t + 1) * 128
                nk = (t + 1) * 128
                dm = dm_r.next()
                for h in range(8):
                    self.ts("pool", dm[:, h, :], self.identf[:], iw[:, t, h:h + 1], cidx, ALU.mult, ALU.mult, reads=[self.identf, iw], writes=[dm])
                nb = (nk + 511) // 512
                for b in range(nb):
                    k0 = b * 512
                    w = min(512, nk - k0)
                    psc = ps_sc.next()
                    diag = (b == nb - 1)
                    for h in range(8):
                        pr = ps_rel.next()
                        self.mm(pr[:, 0:w], iqT[:, h, q0:q1], ikT[:, k0:k0 + w], True, True, [iqT, ikT], [pr])
                        rl = rl_r.next()
                        self.act(rl[:, 0:w], pr[:, 0:w], AF.Relu, [pr], [rl])
                        self.mm(psc[:, 0:w], dm[:, h, :], rl[:, 0:w], h == 0, (h == 7 and not diag), [dm, rl], [psc])
                    if diag:
                        self.mm(psc[:, w - 128:w], self.identb[:], self.cmqb[:], False, True, [self.identb, self.cmqb], [psc])
                    self.cp("dve", sc[:, k0:k0 + w], psc[:, 0:w], [psc], [sc])
                if self.dsc is not None:
                    s.dma("pool", self.dsc[q0:q1, 0:nk], sc[:, 0:nk], reads=[sc], writes=[self.dsc])
                thr = sm["thr"]
                if t < 2:
                    self.ms("dve", thr[:], -10000.0, [thr])
                else:
                    mx, w0, lo, mid, cnt, tmp = (sm[k] for k in ("mx", "w0", "lo", "mid", "cnt", "tmp"))
                    s.op("dve", "tensor_reduce", (), dict(out=mx[:], in_=sc[:, 0:nk], axis=AX.X, op=ALU.max), [sc], [mx])
                    s.op("dve", "tensor_reduce", (), dict(out=lo[:], in_=sc[:, 0:nk - 128], axis=AX.X, op=ALU.min), [sc], [lo])
                    self.tt("dve", w0[:], mx[:], lo[:], ALU.subtract, [mx, lo], [w0])
                    self.ts("dve", w0[:], w0[:], 1.001, 1e-6, ALU.mult, ALU.add, reads=[w0], writes=[w0])
                    for it in range(NBIS):
                        f = float(2.0 ** -(it + 1))
                        self.stt(mid[:], w0[:], f, lo[:], ALU.mult, ALU.add, [w0, lo], [mid])
                        self.ts("dve", junk[:, 0:nk], sc[:, 0:nk], mid[:, 0:1], None, ALU.is_ge, ALU.add, reads=[sc, mid], writes=[junk, cnt], accum_out=cnt[:])
                        self.ts("dve", tmp[:], cnt[:], 255.5, f, ALU.is_ge, ALU.mult, reads=[cnt], writes=[tmp])
                        self.stt(lo[:], tmp[:], w0[:, 0:1], lo[:], ALU.mult, ALU.add, [tmp, w0, lo], [lo])
                    self.cp("dve", thr[:], lo[:], [lo], [thr])
                self.ts("dve", mb[:, 0:nk], sc[:, 0:nk], thr[:, 0:1], -30000.0, ALU.is_lt, ALU.mult, reads=[sc, thr], writes=[mb])
                oacc = ps_o.next()
                for j in range(t + 1):
                    k0, k1 = j * 128, (j + 1) * 128
                    sp = ps_st.next()
                    self.mm(sp[:, :], dkT[:, k0:k1], dqT[:, t, :, :].rearrange("d h q -> d (h q)"), True, False, [dkT, dqT], [sp])
                    self.mm(sp[:, :], mb[:, k0:k1], self.i4b[:], False, True, [mb, self.i4b], [sp])
                    pT = pT_r.next()
                    self.act(pT[:], sp[:, :], AF.Exp, [sp], [pT], scale=scale)
                    for h in range(4):
                        self.mm(oacc[:, h * 65:(h + 1) * 65], pT[:, h * 128:(h + 1) * 128], dva[:, j, :], (j == 0 and h == 0), j == t, [pT, dva], [oacc])
                self.attn_finish(oacc, obuf_r, oT_r, psT_r, 2, t)
                if t % 4 == 3:
                    s.emit(self.top)

    def phase_F(self, l):
        s, S, NT = self.s, self.S, self.NT
        with self.scope() as st:
            gqT = self.sb(st, "gqTs", [32, 4, S], BF16)
            gkT = self.sb(st, "gkTs", [32, 4, S], BF16)
            gk = self.sb(st, "gks", [128, NT, 128], BF16)
            gv = self.sb(st, "gvs", [128, NT, 256], BF16)
            grs = self.sb(st, "grss", [128, NT, 256], BF16)
            glrT = self.sb(st, "glrTs", [17, S], BF16)
            wgf = self.sb(st, "wgf", [17, 128], F32)
            wgb = self.sb(st, "wgb", [17, 128], BF16)
            ngb = self.sb(st, "ngb", [128, 64], F32)
            s.dma("sp", gqT[:], self.gqT[:, :, :], reads=[self.gqT], writes=[gqT])
            s.dma("sp", gkT[:], self.gkT[:, :, :], reads=[self.gkT], writes=[gkT])
            s.dma("sp", gk[:], self.gk[:, :].rearrange("(t p) c -> p t c", p=128), reads=[self.gk], writes=[gk])
            s.dma("sp", gv[:], self.gv[:, :].rearrange("(t p) c -> p t c", p=128), reads=[self.gv], writes=[gv])
            s.dma("sp", grs[:], self.grs[:, :].rearrange("(t p) c -> p t c", p=128), reads=[self.grs], writes=[grs])
            s.dma("sp", glrT[:], self.glrT[:, :], reads=[self.glrT], writes=[glrT])
            s.dma("sp", wgf[0:16, :], self.gla_wg[l], reads=[self.gla_wg], writes=[wgf])
            s.dma("sp", wgf[16:17, :], self.gla_bg[l], reads=[self.gla_bg], writes=[wgf])
            s.dma("sp", ngb[:], self.gla_ng[l].partition_broadcast(128), reads=[self.gla_ng], writes=[ngb])
            self.cp("dve", wgb[:], wgf[:], [wgf], [wgb])
            state = self.sb(st, "state", [32, 4, 64], F32)
            stateb = self.sb(st, "stateb", [32, 4, 64], BF16)
            self.ms("dve", state[:], 0.0, [state])
            self.ms("dve", stateb[:], 0.0, [stateb])
            ps_z = Rot([self.psb(st, "psZ") for _ in range(1)])
            ps_c = Rot([self.psb(st, "psC") for _ in range(2)])
            ps_a = Rot([self.psb(st, "psA") for _ in range(1)])
            ps_o = Rot([self.psb(st, "psO") for _ in range(2)])
            ps_u = Rot([self.psb(st, "psU") for _ in range(1)])
            psT_r = Rot([self.psb(st, "psT", BF16) for _ in range(1)])
            ez_r = Rot([self.sb(st, "ez", [128, 128], F32) for _ in range(2)])
            sp_r = Rot([self.sb(st, "sp", [128, 128], F32) for _ in range(2)])
            ec_r = Rot([self.sb(st, "ec", [32, 4, 128], F32) for _ in range(2)])
            en_r = Rot([self.sb(st, "en", [32, 4, 128], F32) for _ in range(2)])
            er_r = Rot([self.sb(st, "er", [128, 128], F32) for _ in range(2)])
            qt_r = Rot([self.sb(st, "qt", [32, 4, 128], BF16) for _ in range(2)])
            kt_r = Rot([self.sb(st, "kt", [32, 4, 128], BF16) for _ in range(2)])
            kh_r = Rot([self.sb(st, "kh", [128, 128], BF16) for _ in range(2)])
            am_r = Rot([self.sb(st, "am", [128, 4, 128], BF16) for _ in range(2)])
            on_r = Rot([self.sb(st, "on", [128, 4, 64], F32) for _ in range(2)])
            og_r = Rot([self.sb(st, "og", [128, 256], BF16) for _ in range(2)])
            ss_r = Rot([self.sb(st, "ssg", [128, 4], F32) for _ in range(2)])
            junk = self.sb(st, "junkg", [128, 64], F32)
            oT_r = Rot([self.sb(st, "oTt", [128, 2, 128], BF16) for _ in range(2)])
            qscale = float(32 ** -0.5)
            for t in range(NT):
                c0, c1 = t * 128, (t + 1) * 128
                pz = ps_z.next()
                self.mm(pz[:, 0:128], glrT[:, c0:c1], wgb[:], True, True, [glrT, wgb], [pz])
                ez, sp = ez_r.next(), sp_r.next()
                self.act(ez[:], pz[:, 0:128], AF.Exp, [pz], [ez], scale=-1.0)
                self.act(sp[:], ez[:], AF.Ln, [ez], [sp], bias=1.0)
                pc = ps_c.next()
                for h in range(4):
                    self.mm(pc[0:32, h * 128:(h + 1) * 128], sp[:, h * 32:(h + 1) * 32], self.tri[:], True, True, [sp, self.tri], [pc])
                prv = ps_c.next()
                self.mm(prv[:, 0:128], self.rev[:], sp[:], True, True, [sp, self.rev], [prv])
                ec, en, er = ec_r.next(), en_r.next(), er_r.next()
                self.act(ec[:].rearrange("p h i -> p (h i)"), pc[0:32, :], AF.Exp, [pc], [ec])
                self.act(en[:].rearrange("p h i -> p (h i)"), pc[0:32, :], AF.Exp, [pc], [en], scale=-1.0)
                self.act(er[:], prv[:, 0:128], AF.Exp, [prv], [er])
                qt, kt, kh = qt_r.next(), kt_r.next(), kh_r.next()
                self.stt(qt[:], gqT[:, :, c0:c1], qscale, ec[:], ALU.mult, ALU.mult, [gqT, ec], [qt])
                self.tt("pool", kt[:], gkT[:, :, c0:c1], en[:], ALU.mult, [gkT, en], [kt])
                self.tt("pool", kh[:], gk[:, t, :], er[:], ALU.mult, [gk, er], [kh])
                pa = ps_a.next()
                for h in range(4):
                    self.mm(pa[:, h * 128:(h + 1) * 128], kt[:, h, :], qt[:, h, :], True, True, [kt, qt], [pa])
                am = am_r.next()
                self.tt("dve", am[:], pa[:, :].rearrange("p (h i) -> p h i", h=4), self.gmaskb[:].unsqueeze(1).to_broadcast([128, 4, 128]),
                        ALU.mult, [pa, self.gmaskb], [am])
                po = ps_o.next()
                for h in range(4):
                    self.mm(po[:, h * 64:(h + 1) * 64], am[:, h, :], gv[:, t, h * 64:(h + 1) * 64], h == 0, False, [am, gv], [po])
                for c in range(2):
                    i0, i1 = c * 64, (c + 1) * 64
                    for h in range(4):
                        self.mm(po[i0:i1, h * 64:(h + 1) * 64], qt[:, h, i0:i1], stateb[:, h, :], False, True, [qt, stateb], [po])
                    pu = ps_u.next()
                    for h in range(4):
                        self.mm(pu[0:32, h * 64:(h + 1) * 64], kh[i0:i1, h * 32:(h + 1) * 32], gv[i0:i1, t, h * 64:(h + 1) * 64], True, True, [kh, gv], [pu])
                    self.tt("dve", state[:], state[:], ec[:, :, i1 - 1:i1].to_broadcast([32, 4, 64]), ALU.mult, [state, ec], [state])
                    self.tt("dve", state[:], state[:], pu[0:32, 0:256].rearrange("p (h v) -> p h v", h=4), ALU.add, [state, pu], [state])
                    self.cp("dve", stateb[:], state[:], [state], [stateb])
                ss, on, og = ss_r.next(), on_r.next(), og_r.next()
                for h in range(4):
                    self.act(junk[:], po[:, h * 64:(h + 1) * 64], AF.Square, [po], [junk, ss], accum_out=ss[:, h:h + 1])
                self.act(ss[:], ss[:], AF.Sqrt, [ss, self.epsb], [ss], scale=1.0 / 64, bias=self.epsb[:, 0:1])
                self.recip(ss[:], ss[:], [ss], [ss])
                self.tt("dve", on[:], po[:, 0:256].rearrange("p (h v) -> p h v", h=4), ss[:].unsqueeze(2).to_broadcast([128, 4, 64]),
                        ALU.mult, [po, ss], [on])
                self.tt("pool", on[:], on[:], ngb[:].unsqueeze(1).to_broadcast([128, 4, 64]), ALU.mult, [on, ngb], [on])
                self.tt("pool", og[:], on[:].rearrange("p h v -> p (h v)"), grs[:, t, :], ALU.mult, [on, grs], [og])
                self.to_oT(og, oT_r, psT_r, 3, t)

    def phase_G(self, l):
        s, S, NT, NB = self.s, self.S, self.NT, self.NB
        src = self.x_in if l == 0 else self.xres
        with self.scope() as st:
            wbb = self.sb(st, "wbb", [128, 4, 2, D], BF16)
            wob = self.sb(st, "wob", [128, 8, D], BF16)
            with self.scope() as st1:
                wst_r = Rot([self.sb(st1, "wstg", [128, 2, D], F32) for _ in range(2)])
                for n in range(4):
                    wst = wst_r.next()
                    s.dma("sp", wst[:], self.w_br[l, n].rearrange("(k p) c -> p k c", p=128), reads=[self.w_br], writes=[wst])
                    self.cp("pool", wbb[:, n, :, :], wst[:], [wst], [wbb])
                for kk in range(4):
                    wst = wst_r.next()
                    s.dma("sp", wst[:], self.w_out[l, kk * 256:(kk + 1) * 256, :].rearrange("(k p) c -> p k c", p=128), reads=[self.w_out], writes=[wst])
                    self.cp("pool", wob[:, 2 * kk:2 * kk + 2, :], wst[:], [wst], [wob])
            ps = Rot([self.psb(st, "psG") for _ in range(4)])
            ob_r = Rot([self.sb(st, "obk", [128, 4, 2, 512], BF16) for _ in range(2)])
            gt_r = Rot([self.sb(st, "gtk", [128, 32, 512], BF16) for _ in range(2)])
            mf_r = Rot([self.sb(st, "mf", [128, 512], F32) for _ in range(2)])
            tf_r = Rot([self.sb(st, "tf", [128, 512], F32) for _ in range(3)])
            mT_r = Rot([self.sb(st, "mT", [128, 8, 512], BF16) for _ in range(2)])
            xt_r = Rot([self.sb(st, "xtg", [128, D], F32) for _ in range(2)])
            xn_r = Rot([self.sb(st, "xng", [128, D], F32) for _ in range(2)])
            for b in range(NB):
                c0, c1 = b * 512, (b + 1) * 512
                ob, gt, mT = ob_r.next(), gt_r.next(), mT_r.next()
                for n in range(4):
                    s.dma("sp", ob[:, n, :, :], self.oT[n, :, c0:c1].rearrange("(k p) t -> p k t", p=128), reads=[self.oT], writes=[ob])
                for n in range(4):
                    s.dma("sp", gt[:, n * 8:(n + 1) * 8, :], self.gateT[n * 1024:(n + 1) * 1024, c0:c1].rearrange("(g p) t -> p g t", p=128),
                          reads=[self.gateT], writes=[gt])
                for cg in range(8):
                    mf = mf_r.next()
                    for n in range(4):
                        p = ps.next()
                        for k in range(2):
                            self.mm(p[:, :], wbb[:, n, k, cg * 128:(cg + 1) * 128], ob[:, n, k, :], k == 0, k == 1, [wbb, ob], [p])
                        if n == 0:
                            self.tt("dve", mf[:], p[:, :], gt[:, n * 8 + cg, :], ALU.mult, [p, gt], [mf])
                        else:
                            tf = tf_r.next()
                            self.tt("dve", tf[:], p[:, :], gt[:, n * 8 + cg, :], ALU.mult, [p, gt], [tf])
                            if n < 3:
                                self.tt("pool", mf[:], mf[:], tf[:], ALU.add, [mf, tf], [mf])
                            else:
                                self.tt("pool", mT[:, cg, :], mf[:], tf[:], ALU.add, [mf, tf], [mT])
                for tt_ in range(4):
                    r0 = c0 + tt_ * 128
                    xt, xn = xt_r.next(), xn_r.next()
                    s.dma("sp", xt[:], src[r0:r0 + 128, :], reads=[src], writes=[xt])
                    for half in range(2):
                        p = ps.next()
                        for cg in range(8):
                            self.mm(p[:, :], mT[:, cg, tt_ * 128:(tt_ + 1) * 128], wob[:, cg, half * 512:(half + 1) * 512], cg == 0, cg == 7, [mT, wob], [p])
                        self.tt("dve", xn[:, half * 512:(half + 1) * 512], p[:, :], xt[:, half * 512:(half + 1) * 512], ALU.add, [p, xt], [xn])
                    s.dma("pool", self.xres[r0:r0 + 128, :], xn[:], reads=[xn], writes=[self.xres])

    def phase_H(self, l):
        s, S = self.s, self.S
        SB = 1024
        NG = DFF // 128
        with self.scope() as st:
            hT = self.sb(st, "h2T", [128, 8, SB], BF16)
            aT = self.sb(st, "aT", [128, NG, SB], BF16)
            nt = self.norm_tiles(st)
            ps = Rot([self.psb(st, "psH") for _ in range(4)])
            wgs_r = Rot([self.sb(st, "wgs", [128, 8, 128], F32) for _ in range(2)])
            wus_r = Rot([self.sb(st, "wus", [128, 8, 128], F32) for _ in range(2)])
            wgb_r = Rot([self.sb(st, "wgbf", [128, 8, 128], BF16) for _ in range(2)])
            wub_r = Rot([self.sb(st, "wubf", [128, 8, 128], BF16) for _ in range(2)])
            wds_r = Rot([self.sb(st, "wds", [128, NG, 256], F32) for _ in range(1)])
            wdb_r = Rot([self.sb(st, "wdb", [128, NG, 256], BF16) for _ in range(2)])
            sg_r = Rot([self.sb(st, "sg", [128, 512], BF16) for _ in range(3)])
            xt_r = Rot([self.sb(st, "xth", [128, 256], F32) for _ in range(3)])
            xn_r = Rot([self.sb(st, "xnh", [128, 256], F32) for _ in range(3)])

            def load_gu(g):
                wgs, wus, wgb, wub = wgs_r.next(), wus_r.next(), wgb_r.next(), wub_r.next()
                s.dma("sp", wgs[:], self.w_fg[l, :, g * 128:(g + 1) * 128].rearrange("(k p) n -> p k n", p=128), reads=[self.w_fg], writes=[wgs])
                s.dma("sp", wus[:], self.w_fu[l, :, g * 128:(g + 1) * 128].rearrange("(k p) n -> p k n", p=128), reads=[self.w_fu], writes=[wus])
                self.cp("pool", wgb[:], wgs[:], [wgs], [wgb])
                self.cp("pool", wub[:], wus[:], [wus], [wub])
                return wgb, wub

            for sbk in range(S // SB):
                t0 = sbk * SB
                self.norm_to_hT(nt, self.xres, self.gffn, l, hT, t0, SB // 128)
                nxt = load_gu(0)
                for g in range(NG):
                    wgb, wub = nxt
                    if g + 1 < NG:
                        nxt = load_gu(g + 1)
                    for b in range(SB // 512):
                        pg, pu = ps.next(), ps.next()
                        for k in range(8):
                            self.mm(pg[:, :], wgb[:, k, :], hT[:, k, b * 512:(b + 1) * 512], k == 0, k == 7, [wgb, hT], [pg])
                        for k in range(8):
                            self.mm(pu[:, :], wub[:, k, :], hT[:, k, b * 512:(b + 1) * 512], k == 0, k == 7, [wub, hT], [pu])
                        sg = sg_r.next()
                        self.act(sg[:], pg[:, :], AF.Silu, [pg], [sg])
                        self.tt("dve", aT[:, g, b * 512:(b + 1) * 512], pu[:, :], sg[:], ALU.mult, [pu, sg], [aT])
                for cq in range(4):
                    wds, wdb = wds_r.next(), wdb_r.next()
                    s.dma("sp", wds[:], self.w_fd[l, :, cq * 256:(cq + 1) * 256].rearrange("(k p) n -> p k n", p=128), reads=[self.w_fd], writes=[wds])
                    self.cp("pool", wdb[:], wds[:], [wds], [wdb])
                    for tt_ in range(SB // 128):
                        r0 = t0 + tt_ * 128
                        xt, xn = xt_r.next(), xn_r.next()
                        s.dma("sp", xt[:], self.xres[r0:r0 + 128, cq * 256:(cq + 1) * 256], reads=[self.xres], writes=[xt])
                        p = ps.next()
                        for k in range(NG):
                            self.mm(p[:, 0:256], aT[:, k, tt_ * 128:(tt_ + 1) * 128], wdb[:, k, :], k == 0, k == NG - 1, [aT, wdb], [p])
                        self.tt("dve", xn[:], p[:, 0:256], xt[:], ALU.add, [p, xt], [xn])
                        s.dma("pool", self.xres[r0:r0 + 128, cq * 256:(cq + 1) * 256], xn[:], reads=[xn], writes=[self.xres])
                s.emit(self.top)

    def phase_final(self):
        s, S, NT = self.s, self.S, self.NT
        with self.scope() as st:
            gf = self.sb(st, "gf", [128, D], F32)
            s.dma("sp", gf[:], self.g_final[:].partition_broadcast(128), reads=[self.g_final], writes=[gf])
            xt_r = Rot([self.sb(st, "xtf", [128, D], F32) for _ in range(3)])
            yo_r = Rot([self.sb(st, "yof", [128, D], F32) for _ in range(3)])
            junk = self.sb(st, "junkf", [128, D], BF16)
            ss_r = Rot([self.sb(st, "ssf", [128, 1], F32) for _ in range(3)])
            for t in range(NT):
                xt, yo, ss = xt_r.next(), yo_r.next(), ss_r.next()
                s.dma("sp", xt[:], self.xres[t * 128:(t + 1) * 128, :], reads=[self.xres], writes=[xt])
                self.act(junk[:], xt[:], AF.Square, [xt], [junk, ss], accum_out=ss[:])
                self.act(ss[:], ss[:], AF.Sqrt, [ss, self.epsb], [ss], scale=1.0 / D, bias=self.epsb[:, 0:1])
                self.recip(ss[:], ss[:], [ss], [ss])
                self.stt(yo[:], xt[:], ss[:, 0:1], gf[:], ALU.mult, ALU.mult, [xt, ss, gf], [yo])
                s.dma("pool", self.y_out[t * 128:(t + 1) * 128, :], yo[:], reads=[yo], writes=[self.y_out])


def make_in_maps(inputs, S, batch_ids):
    f = lambda a: np.ascontiguousarray(np.asarray(a))
    consts = host_consts()
    shared = {
        "w_in": f(inputs["w_in"]), "w_qup": f(inputs["mla_w_q_up"]), "w_kvup": f(inputs["mla_w_kv_up"]),
        "w_br": f(inputs["w_branch"]), "w_out": f(inputs["w_out"]), "w_fg": f(inputs["w_ffn_gate"]),
        "w_fu": f(inputs["w_ffn_up"]), "w_fd": f(inputs["w_ffn_down"]),
        "g_attn": f(np.asarray(inputs["attn_norm_g"]).reshape(NLAYER, 8, 128).transpose(2, 0, 1)),
        "g_ffn": f(np.asarray(inputs["ffn_norm_g"]).reshape(NLAYER, 8, 128).transpose(2, 0, 1)),
        "g_final": f(inputs["final_norm_g"]),
        "g_q": f(np.asarray(inputs["mla_q_norm_g"]).reshape(NLAYER, 2, 128).transpose(2, 0, 1)),
        "g_kv": f(np.asarray(inputs["mla_kv_norm_g"]).transpose(1, 0)),
        "conv_w": f(np.asarray(inputs["conv_w"]).reshape(NLAYER, 3, 2, 128).transpose(3, 0, 2, 1)),
        "gla_wg": f(inputs["gla_w_gate_up"]),
        "gla_bg": f(np.asarray(inputs["gla_b_gate"]).reshape(NLAYER, 1, 128)),
        "gla_ng": f(inputs["gla_norm_g"]),
    }
    shared.update(consts)
    maps = []
    x = np.asarray(inputs["x"])
    pos = np.asarray(inputs["positions"])
    for b in batch_ids:
        m = dict(shared)
        m["x"] = f(x[b, :S])
        m["pos"] = f(pos[b, :S].astype(np.int32))
        maps.append(m)
    return maps


_NC_CACHE = {}


def kernel(**inputs):
    S = 4096
    key = ("full", S)
    if key not in _NC_CACHE:
        _NC_CACHE[key] = Builder(S, NLAYER).build()
    nc = _NC_CACHE[key]
    in_maps = make_in_maps(inputs, S, list(range(8)))
    res = run_bass_kernel_spmd(nc, in_maps, core_ids=list(range(8)))
    out = np.stack([np.asarray(r["y"]) for r in res.results], axis=0).astype(np.float32)
    return out
```

```python
import numpy as np
from contextlib import ExitStack, contextmanager
import concourse.bass as bass
import concourse.mybir as mybir
from concourse.bass_utils import run_bass_kernel_spmd

F32 = mybir.dt.float32
BF16 = mybir.dt.bfloat16
I32 = mybir.dt.int32
AF = mybir.ActivationFunctionType
ALU = mybir.AluOpType
AX = mybir.AxisListType

D = 1024
DFF = 2816
NLAYER = 4
INTOT = 6744
EPS = 1e-6
EPOCH = 30000
NBIS = 14

C_CQ, C_CKV, C_KROPE = 0, 256, 384
C_CB, C_CC, C_CX = 416, 672, 928
C_DQ, C_DK, C_DV = 1184, 1440, 1504
C_IQ, C_IK, C_IW = 1568, 1824, 1856
C_GQ, C_GK, C_GV, C_GLR, C_GR = 1864, 1992, 2120, 2376, 2392
C_GATE = 2648


class Buf:
    def __init__(self, name, t=None):
        self.name = name
        self.t = t
        self.last_write = None
        self.readers = {}
        self.wsem = None
        self.rsem = None

    def __getitem__(self, idx):
        return self.t[idx]


class Sched:
    COMPUTE = ("pe", "act", "dve", "pool")
    SEMCAP = 30000

    def __init__(self, nc):
        self.nc = nc
        self.engs = list(self.COMPUTE) + ["sp"]
        self.ops = {e: [] for e in self.engs}
        self.cnt = {e: 0 for e in self.COMPUTE}
        self.sems = {}
        self.sem_names = []
        self.waited = {e: {} for e in self.engs}
        self.nops = 0
        self.semval = {}
        self.free_dma = []
        self.ndma_sems = 0

    def _sem(self, name):
        if name not in self.sems:
            self.sems[name] = None
            self.sem_names.append(name)
        return name

    def _dma_sem(self):
        if self.free_dma:
            return self.free_dma.pop()
        self.ndma_sems += 1
        name = self._sem(f"d_{self.ndma_sems}")
        self.semval[name] = 0
        return name

    def release(self, bufs):
        for b in bufs:
            for nm in (b.wsem, b.rsem):
                if nm is not None and self.semval[nm] < self.SEMCAP:
                    self.free_dma.append(nm)
            b.wsem = b.rsem = None

    def _compute_token(self, e):
        self.cnt[e] += 1
        k = self.cnt[e]
        ep = (k - 1) // EPOCH
        return (self._sem(f"c_{e}_{ep}"), (k - 1) % EPOCH + 1, e)

    def _need(self, e, tok, waits):
        if tok is None:
            return
        sem, val, src = tok
        if src == e and e == "pe":
            return
        w = self.waited[e]
        if w.get(sem, 0) >= val:
            return
        w[sem] = val
        waits.append((sem, val))

    def _deps(self, e, reads, writes):
        waits = []
        for b in reads:
            self._need(e, b.last_write, waits)
        for b in writes:
            self._need(e, b.last_write, waits)
            for r in b.readers.values():
                self._need(e, r, waits)
        return waits

    def _mark(self, tok, reads, writes):
        for b in reads:
            old = b.readers.get(tok[0])
            if old is None or old[1] < tok[1]:
                b.readers[tok[0]] = tok
        for b in writes:
            b.last_write = tok
            b.readers = {}

    def op(self, e, method, args, kwargs, reads=(), writes=()):
        waits = self._deps(e, reads, writes)
        tok = self._compute_token(e)
        self._mark(tok, reads, writes)
        self.ops[e].append((waits, method, args, kwargs, (tok[0], 1)))
        self.nops += 1
        return tok

    def dma(self, q, out_ap, in_ap, reads=(), writes=(), **kw):
        waits = self._deps(q, reads, writes)
        if writes:
            b = writes[0]
            if b.wsem is None:
                b.wsem = self._dma_sem()
            nm = b.wsem
        else:
            b = reads[0]
            if b.rsem is None:
                b.rsem = self._dma_sem()
            nm = b.rsem
        self.semval[nm] += 16
        tok = (nm, self.semval[nm], "dma")
        self._mark(tok, reads, writes)
        kw = dict(kw)
        kw["out"] = out_ap
        kw["in_"] = in_ap
        self.ops[q].append((waits, "dma_start", (), kw, (nm, 16)))
        self.nops += 1
        return tok

    def wait_all(self, e, bufs):
        waits = self._deps(e, (), bufs)
        if waits:
            self.ops[e].append((waits, None, None, None, None))

    def barrier(self):
        toks = []
        for e in self.COMPUTE:
            k = self.cnt[e]
            if k > 0:
                ep = (k - 1) // EPOCH
                toks.append((f"c_{e}_{ep}", (k - 1) % EPOCH + 1, e))
        for nm, v in self.semval.items():
            if v > 0:
                toks.append((nm, v, "dma"))
        for e in self.engs:
            waits = []
            for tok in toks:
                self._need(e, tok, waits)
            if waits:
                self.ops[e].append((waits, None, None, None, None))

    def emit(self, stack):
        nc = self.nc
        if not hasattr(self, "sem_stack"):
            self.sem_stack = stack
        for name in self.sem_names:
            if self.sems[name] is None:
                self.sems[name] = self.sem_stack.enter_context(nc.semaphore(name))
        if not any(self.ops[e] for e in self.engs):
            return
        with nc.Block() as block:
            amap = {"pe": block.tensor, "act": block.scalar, "dve": block.vector,
                    "pool": block.gpsimd, "sp": block.sync}
            for e in self.engs:
                ops = self.ops[e]
                if not ops:
                    continue

                def body(eng, ops=ops):
                    for waits, method, args, kwargs, inc in ops:
                        for sem, val in waits:
                            eng.wait_ge(self.sems[sem], val)
                        if method is not None:
                            ins = getattr(eng, method)(*args, **kwargs)
                            ins.then_inc(self.sems[inc[0]], inc[1])

                amap[e](body)
                self.ops[e] = []


class Rot:
    def __init__(self, bufs):
        self.bufs = bufs
        self.i = 0

    def next(self):
        b = self.bufs[self.i % len(self.bufs)]
        self.i += 1
        return b


def host_consts():
    j = np.arange(128)[:, None]
    i = np.arange(128)[None, :]
    same = (j // 64) == (i // 64)
    c = {}
    c["c_ident"] = np.eye(128, dtype=np.float32)
    c["c_tri"] = np.where(same & (j <= i), -1.0 / 16.0, 0.0).astype(np.float32)
    c["c_rev"] = np.where(same & (j > i), -1.0 / 16.0, 0.0).astype(np.float32)
    c["c_gmask"] = np.where(same & (j <= i), 1.0, 0.0).astype(np.float32)
    c["c_cmt"] = np.where(j > i, -30000.0, 0.0).astype(np.float32)
    c["c_cmq"] = np.ascontiguousarray(c["c_cmt"].T)
    rot = np.zeros((32, 32), np.float32)
    for fp in range(16):
        rot[fp + 16, fp] = -1.0
    for fp in range(16, 32):
        rot[fp - 16, fp] = 1.0
    c["c_rot"] = rot
    half = 16
    invf = (np.float32(10000.0) ** (-np.arange(half, dtype=np.float32) / np.float32(half))).astype(np.float32)
    c["c_invf"] = np.concatenate([invf, invf]).reshape(32, 1).astype(np.float32)
    return c


class Builder:
    def __init__(self, S, nl, dbg=(), stop_after=None, phases=None):
        self.S = S
        self.NT = S // 128
        self.NB = S // 512
        self.nl = nl
        self.dbg = set(dbg)
        self.stop_after = stop_after
        self.phases = phases
        self.nc = bass.Bass("TRN2", target_bir_lowering=False)
        self.s = Sched(self.nc)
        self.uid = 0
        self.live = []

    def din(self, name, shape, dt=F32):
        return self.nc.dram_tensor(name, list(shape), dt, kind="ExternalInput").ap()

    def dscr(self, name, shape, dt=BF16):
        kind = "ExternalOutput" if name in self.dbg else "Internal"
        ap = self.nc.dram_tensor(name, list(shape), dt, kind=kind).ap()
        return Buf(name, ap)

    def sb(self, st, name, shape, dt):
        self.uid += 1
        nm = f"{name}_{self.uid}"
        b = Buf(nm, st.enter_context(self.nc.sbuf_tensor(nm, list(shape), dt)))
        self.live.append(b)
        return b

    def psb(self, st, name, dt=F32):
        self.uid += 1
        nm = f"{name}_{self.uid}"
        shape = [128, 512] if dt == F32 else [128, 1024]
        b = Buf(nm, st.enter_context(self.nc.psum_tensor(nm, shape, dt)))
        self.live.append(b)
        return b

    @contextmanager
    def scope(self):
        st = ExitStack()
        mark = len(self.live)
        yield st
        self.s.barrier()
        self.s.release(self.live[mark:])
        del self.live[mark:]
        self.s.emit(self.top)
        st.close()

    def mm(self, out, lhsT, rhs, start, stop, reads, writes):
        self.s.op("pe", "matmul", (out,), dict(lhsT=lhsT, rhs=rhs, start=start, stop=stop), reads, writes)

    def tr(self, out, in_, reads, writes):
        self.s.op("pe", "transpose", (), dict(out=out, in_=in_, identity=self.identb[:]), list(reads) + [self.identb], writes)

    def act(self, out, in_, func, reads, writes, **kw):
        self.s.op("act", "activation", (), dict(out=out, in_=in_, func=func, **kw), reads, writes)

    def tt(self, eng, out, in0, in1, op, reads, writes):
        self.s.op(eng, "tensor_tensor", (), dict(out=out, in0=in0, in1=in1, op=op), reads, writes)

    def ts(self, eng, out, in0, s1, s2, op0, op1=None, reads=(), writes=(), accum_out=None):
        kw = dict(out=out, in0=in0, scalar1=s1, scalar2=s2, op0=op0)
        if op1 is not None:
            kw["op1"] = op1
        if accum_out is not None:
            kw["accum_out"] = accum_out
        self.s.op(eng, "tensor_scalar", (), kw, reads, writes)

    def stt(self, out, in0, scalar, in1, op0, op1, reads, writes):
        self.s.op("dve", "scalar_tensor_tensor", (), dict(out=out, in0=in0, scalar=scalar, in1=in1, op0=op0, op1=op1), reads, writes)

    def cp(self, eng, out, in_, reads, writes):
        self.s.op(eng, "tensor_copy", (), dict(out=out, in_=in_), reads, writes)

    def ms(self, eng, ap, val, writes):
        self.s.op(eng, "memset", (ap, val), {}, (), writes)

    def recip(self, out, in_, reads, writes):
        self.s.op("dve", "reciprocal", (), dict(out=out, in_=in_), reads, writes)

    def build(self):
        nc, s, S, NT = self.nc, self.s, self.S, self.NT
        self.x_in = Buf("x", self.din("x", [S, D]))
        self.pos = Buf("pos", self.din("pos", [S], I32))
        self.w_in = Buf("w_in", self.din("w_in", [NLAYER, D, INTOT]))
        self.w_qup = Buf("w_qup", self.din("w_qup", [NLAYER, 256, 384]))
        self.w_kvup = Buf("w_kvup", self.din("w_kvup", [NLAYER, 128, 512]))
        self.w_br = Buf("w_br", self.din("w_br", [NLAYER, 4, 256, D]))
        self.w_out = Buf("w_out", self.din("w_out", [NLAYER, D, D]))
        self.w_fg = Buf("w_fg", self.din("w_fg", [NLAYER, D, DFF]))
        self.w_fu = Buf("w_fu", self.din("w_fu", [NLAYER, D, DFF]))
        self.w_fd = Buf("w_fd", self.din("w_fd", [NLAYER, DFF, D]))
        self.g_attn = Buf("g_attn", self.din("g_attn", [128, NLAYER, 8]))
        self.g_ffn = Buf("g_ffn", self.din("g_ffn", [128, NLAYER, 8]))
        self.g_final = Buf("g_final", self.din("g_final", [D]))
        self.g_q = Buf("g_q", self.din("g_q", [128, NLAYER, 2]))
        self.g_kv = Buf("g_kv", self.din("g_kv", [128, NLAYER]))
        self.conv_w = Buf("conv_w", self.din("conv_w", [128, NLAYER, 2, 3]))
        self.gla_wg = Buf("gla_wg", self.din("gla_wg", [NLAYER, 16, 128]))
        self.gla_bg = Buf("gla_bg", self.din("gla_bg", [NLAYER, 1, 128]))
        self.gla_ng = Buf("gla_ng", self.din("gla_ng", [NLAYER, 64]))
        cin = {k: Buf(k, self.din(k, list(v.shape))) for k, v in host_consts().items()}
        self.y_out = Buf("y", nc.dram_tensor("y", [S, D], F32, kind="ExternalOutput").ap())
        self.xres = self.dscr("xres", [S, D], F32)
        self.cosT = self.dscr("cosT", [32, S], F32)
        self.sinT = self.dscr("sinT", [32, S], F32)
        self.cqT = self.dscr("cqT", [256, S])
        self.ckvT = self.dscr("ckvT", [128, S])
        self.kropeT = self.dscr("kropeT", [32, S])
        self.cbT = self.dscr("cbT", [256, S])
        self.ccT = self.dscr("ccT", [256, S])
        self.cxT = self.dscr("cxT", [256, S])
        self.dqT = self.dscr("dqT", [64, 4, S])
        self.dkT = self.dscr("dkT", [64, S])
        self.iqT = self.dscr("iqT", [32, 8, S])
        self.ikT = self.dscr("ikT", [32, S])
        self.gqT = self.dscr("gqT", [32, 4, S])
        self.gkT = self.dscr("gkT", [32, 4, S])
        self.glrT = self.dscr("glrT", [17, S])
        self.gateT = self.dscr("gateT", [4096, S])
        self.dvaug = self.dscr("dvaug", [S, 65])
        self.iw = self.dscr("iw", [S, 8], F32)
        self.gk = self.dscr("gk", [S, 128])
        self.gv = self.dscr("gv", [S, 256])
        self.grs = self.dscr("grs", [S, 256])
        self.qT = self.dscr("qT", [4, 96, S])
        self.kT = self.dscr("kT", [4, 96, S])
        self.vaug = self.dscr("vaug", [S, 4, 65])
        self.oT = self.dscr("oT", [4, 256, S])
        self.dsc = self.dscr("dsc", [S, S], F32) if "dsc" in self.dbg else None

        with ExitStack() as top:
            self.top = top
            cst = {}
            for k in ("c_ident", "c_tri", "c_rev", "c_gmask", "c_cmt", "c_cmq"):
                cst[k] = self.sb(top, k, [128, 128], F32)
                s.dma("sp", cst[k][:], cin[k][:], reads=[cin[k]], writes=[cst[k]])
            self.identf = cst["c_ident"]
            self.tri = cst["c_tri"]
            self.rev = cst["c_rev"]
            self.identb = self.sb(top, "identb", [128, 128], BF16)
            self.gmaskb = self.sb(top, "gmaskb", [128, 128], BF16)
            self.cmtb = self.sb(top, "cmtb", [128, 128], BF16)
            self.cmqb = self.sb(top, "cmqb", [128, 128], BF16)
            self.i4b = self.sb(top, "i4b", [128, 512], BF16)
            self.onesb = self.sb(top, "onesb", [128, 128], BF16)
            self.epsb = self.sb(top, "epsb", [128, 1], F32)
            self.rotb = self.sb(top, "rotb", [32, 32], BF16)
            rotf = self.sb(top, "rotf", [32, 32], F32)
            self.invf = self.sb(top, "invf", [32, 1], F32)
            s.dma("sp", rotf[:], cin["c_rot"][:], reads=[cin["c_rot"]], writes=[rotf])
            s.dma("sp", self.invf[:], cin["c_invf"][:], reads=[cin["c_invf"]], writes=[self.invf])
            self.cp("dve", self.identb[:], self.identf[:], [self.identf], [self.identb])
            self.cp("dve", self.gmaskb[:], cst["c_gmask"][:], [cst["c_gmask"]], [self.gmaskb])
            self.cp("dve", self.cmtb[:], cst["c_cmt"][:], [cst["c_cmt"]], [self.cmtb])
            self.cp("dve", self.cmqb[:], cst["c_cmq"][:], [cst["c_cmq"]], [self.cmqb])
            self.cp("dve", self.rotb[:], rotf[:], [rotf], [self.rotb])
            for h in range(4):
                self.cp("dve", self.i4b[:, h * 128:(h + 1) * 128], self.identf[:], [self.identf], [self.i4b])
            self.ms("dve", self.onesb[:], 1.0, [self.onesb])
            self.ms("dve", self.epsb[:], EPS, [self.epsb])
            self.gattn = self.sb(top, "gattn", [128, NLAYER, 8], F32)
            self.gffn = self.sb(top, "gffn", [128, NLAYER, 8], F32)
            self.gq = self.sb(top, "gq", [128, NLAYER, 2], F32)
            self.gkv = self.sb(top, "gkv", [128, NLAYER], F32)
            self.convw = self.sb(top, "convw", [128, NLAYER, 2, 3], F32)
            for dst, srcb in ((self.gattn, self.g_attn), (self.gffn, self.g_ffn), (self.gq, self.g_q),
                              (self.gkv, self.g_kv), (self.convw, self.conv_w)):
                s.dma("sp", dst[:], srcb[:], reads=[srcb], writes=[dst])
            s.emit(top)

            allph = "RABCEDFGH"
            ph = self.phases if self.phases is not None else allph
            if "R" in ph:
                self.phase_rope()
            done = False
            for l in range(self.nl):
                for name, fn in (("A", self.phase_A), ("B", self.phase_B), ("C", self.phase_C), ("E", self.phase_E),
                                 ("D", self.phase_D), ("F", self.phase_F), ("G", self.phase_G), ("H", self.phase_H)):
                    if name in ph:
                        fn(l)
                    if self.stop_after == (l, name):
                        done = True
                        break
                if done:
                    break
            if not done:
                self.phase_final()
            outs = [self.y_out] + [getattr(self, n) for n in self.dbg if getattr(self, n, None) is not None]
            s.wait_all("sp", outs)
            s.wait_all("pool", outs)
            s.barrier()
            s.emit(top)
        return nc

    def phase_rope(self):
        s, S = self.s, self.S
        W = min(S, 1024)
        with self.scope() as st:
            pi = self.sb(st, "pi", [32, W], I32)
            pf = self.sb(st, "pf", [32, W], F32)
            pk = self.sb(st, "pk", [32, W], I32)
            pkf = self.sb(st, "pkf", [32, W], F32)
            pc = self.sb(st, "pc", [32, W], F32)
            pa = self.sb(st, "pa", [32, W], F32)
            res = self.sb(st, "res", [32, W], F32)
            for c0 in range(0, S, W):
                s.dma("sp", pi[:], self.pos[c0:c0 + W].partition_broadcast(32), reads=[self.pos], writes=[pi])
                self.cp("dve", pf[:], pi[:], [pi], [pf])
                self.ts("dve", pf[:], pf[:], self.invf[:, 0:1], None, ALU.mult, reads=[pf, self.invf], writes=[pf])
                for shift, dst in ((0.0, self.sinT), (0.25, self.cosT)):
                    self.ts("dve", pa[:], pf[:], float(1.0 / (2 * np.pi)), shift, ALU.mult, ALU.add, reads=[pf], writes=[pa])
                    self.cp("dve", pk[:], pa[:], [pa], [pk])
                    self.cp("dve", pkf[:], pk[:], [pk], [pkf])
                    self.tt("dve", pa[:], pa[:], pkf[:], ALU.subtract, [pa, pkf], [pa])
                    self.ts("dve", pc[:], pa[:], 0.5, None, ALU.is_gt, reads=[pa], writes=[pc])
                    self.tt("dve", pa[:], pa[:], pc[:], ALU.subtract, [pa, pc], [pa])
                    self.ts("dve", pc[:], pa[:], -0.5, None, ALU.is_lt, reads=[pa], writes=[pc])
                    self.tt("dve", pa[:], pa[:], pc[:], ALU.add, [pa, pc], [pa])
                    self.act(res[:], pa[:], AF.Sin, [pa], [res], scale=float(2 * np.pi))
                    s.dma("pool", dst[:, c0:c0 + W], res[:], reads=[res], writes=[dst])

    def norm_tiles(self, st):
        return dict(
            xt=Rot([self.sb(st, "xt", [128, D], F32) for _ in range(3)]),
            xs=Rot([self.sb(st, "xs", [128, D], BF16) for _ in range(2)]),
            junk=self.sb(st, "junk", [128, D], BF16),
            ss=Rot([self.sb(st, "ss", [128, 1], F32) for _ in range(3)]),
            rs=Rot([self.sb(st, "rs", [128, 1], F32) for _ in range(3)]),
            psT=Rot([self.psb(st, "psT", BF16) for _ in range(2)]))

    def norm_to_hT(self, nt, src, g_tile, l, hT, tok0, ntiles):
        s = self.s
        for t in range(ntiles):
            xt, xs, ss, rs = nt["xt"].next(), nt["xs"].next(), nt["ss"].next(), nt["rs"].next()
            junk = nt["junk"]
            r0 = tok0 + t * 128
            s.dma("sp", xt[:], src[r0:r0 + 128, :], reads=[src], writes=[xt])
            self.act(junk[:], xt[:], AF.Square, [xt], [junk, ss], accum_out=ss[:])
            self.act(rs[:], ss[:], AF.Sqrt, [ss, self.epsb], [rs], scale=1.0 / D, bias=self.epsb[:, 0:1])
            self.recip(rs[:], rs[:], [rs], [rs])
            self.ts("dve", xs[:], xt[:], rs[:, 0:1], None, ALU.mult, reads=[xt, rs], writes=[xs])
            pT = nt["psT"].next()
            for k in range(8):
                self.tr(pT[:, k * 128:(k + 1) * 128], xs[:, k * 128:(k + 1) * 128], [xs], [pT])
            self.tt("dve", hT[:, :, t * 128:(t + 1) * 128], pT[:, :].rearrange("p (k q) -> p k q", k=8),
                    g_tile[:, l, :].unsqueeze(2).to_broadcast([128, 8, 128]), ALU.mult, [pT, g_tile], [hT])

    def phase_A(self, l):
        s, S, NT, NB = self.s, self.S, self.NT, self.NB
        src = self.x_in if l == 0 else self.xres
        with self.scope() as st:
            hT = self.sb(st, "hT", [128, 8, S], BF16)
            with self.scope() as st1:
                ones_row = self.sb(st1, "ones_row", [1, S], BF16)
                self.ms("dve", ones_row[:], 1.0, [ones_row])
                s.dma("pool", self.glrT[16:17, :], ones_row[:], reads=[ones_row], writes=[self.glrT])
                nt = self.norm_tiles(st1)
                self.norm_to_hT(nt, src, self.gattn, l, hT, 0, NT)
            ps = Rot([self.psb(st, "psA") for _ in range(4)])
            wbf_r = Rot([self.sb(st, "wbf", [128, 8, 256], BF16) for _ in range(3)])
            stg_r = Rot([self.sb(st, "stg", [128, S], BF16) for _ in range(3)])
            evac = [0]

            def load_w(c0, n):
                wbf = wbf_r.next()
                s.dma("pool", wbf[:, :, 0:n], self.w_in[l, :, c0:c0 + n].rearrange("(k p) n -> p k n", p=128),
                      reads=[self.w_in], writes=[wbf])
                return wbf

            def fm_group(wbf, n, outs, func):
                stg = stg_r.next()
                for b in range(NB):
                    p = ps.next()
                    for k in range(8):
                        self.mm(p[0:n, :], wbf[:, k, 0:n], hT[:, k, b * 512:(b + 1) * 512], k == 0, k == 7, [wbf, hT], [p])
                    evac[0] += 1
                    if func != AF.Copy or evac[0] % 2 == 0:
                        self.act(stg[0:n, b * 512:(b + 1) * 512], p[0:n, :], func, [p], [stg])
                    else:
                        self.cp("dve", stg[0:n, b * 512:(b + 1) * 512], p[0:n, :], [p], [stg])
                for (r0, r1, dbuf, dap) in outs:
                    s.dma("pool", dap, stg[r0:r1, :], reads=[stg], writes=[dbuf])

            fm = []
            for g in range(2):
                fm.append((C_CQ + 128 * g, 128, [(0, 128, self.cqT, self.cqT[128 * g:128 * (g + 1), :])], AF.Copy))
            fm.append((C_CKV, 128, [(0, 128, self.ckvT, self.ckvT[:, :])], AF.Copy))
            fm.append((C_KROPE, 32, [(0, 32, self.kropeT, self.kropeT[:, :])], AF.Copy))
            for c0, dst in ((C_CB, self.cbT), (C_CC, self.ccT), (C_CX, self.cxT)):
                for g in range(2):
                    fm.append((c0 + 128 * g, 128, [(0, 128, dst, dst[128 * g:128 * (g + 1), :])], AF.Copy))
            for g in range(2):
                fm.append((C_DQ + 128 * g, 128, [(64 * j, 64 * (j + 1), self.dqT, self.dqT[:, 2 * g + j, :]) for j in range(2)], AF.Copy))
            fm.append((C_DK, 64, [(0, 64, self.dkT, self.dkT[:, :])], AF.Copy))
            for g in range(2):
                fm.append((C_IQ + 128 * g, 128, [(32 * j, 32 * (j + 1), self.iqT, self.iqT[:, 4 * g + j, :]) for j in range(4)], AF.Copy))
            fm.append((C_IK, 32, [(0, 32, self.ikT, self.ikT[:, :])], AF.Copy))
            fm.append((C_GQ, 128, [(32 * j, 32 * (j + 1), self.gqT, self.gqT[:, j, :]) for j in range(4)], AF.Copy))
            fm.append((C_GK, 128, [(32 * j, 32 * (j + 1), self.gkT, self.gkT[:, j, :]) for j in range(4)], AF.Copy))
            fm.append((C_GLR, 16, [(0, 16, self.glrT, self.glrT[0:16, :])], AF.Copy))
            for g in range(32):
                fm.append((C_GATE + 128 * g, 128, [(0, 128, self.gateT, self.gateT[128 * g:128 * (g + 1), :])], AF.Sigmoid))
            nxt = load_w(fm[0][0], fm[0][1])
            for i, (c0, n, outs, func) in enumerate(fm):
                wbf = nxt
                if i + 1 < len(fm):
                    nxt = load_w(fm[i + 1][0], fm[i + 1][1])
                fm_group(wbf, n, outs, func)

            tms = [(C_DV, 64, self.dvaug, BF16, AF.Copy, True), (C_IW, 8, self.iw, F32, AF.Copy, False),
                   (C_GK, 128, self.gk, BF16, AF.Copy, False), (C_GV, 256, self.gv, BF16, AF.Copy, False),
                   (C_GR, 256, self.grs, BF16, AF.Silu, False)]
            for (c0, n, dst, dt, func, ones_col) in tms:
                wbf = load_w(c0, n)
                nn = n + (1 if ones_col else 0)
                tstg = self.sb(st, "tstg", [128, NT, nn], dt)
                if ones_col:
                    self.ms("dve", tstg[:, :, n:n + 1], 1.0, [tstg])
                for t in range(NT):
                    p = ps.next()
                    for k in range(8):
                        self.mm(p[:, 0:n], hT[:, k, t * 128:(t + 1) * 128], wbf[:, k, 0:n], k == 0, k == 7, [wbf, hT], [p])
                    self.act(tstg[:, t, 0:n], p[:, 0:n], func, [p], [tstg])
                s.dma("pool", dst[:, :].rearrange("(t p) n -> p t n", p=128), tstg[:], reads=[tstg], writes=[dst])

    def phase_B(self, l):
        s, S, NT, NB = self.s, self.S, self.NT, self.NB
        with self.scope() as st:
            wqf = self.sb(st, "wqf", [128, 2, 384], F32)
            wqb = self.sb(st, "wqb", [128, 2, 384], BF16)
            wkf = self.sb(st, "wkf", [128, 512], F32)
            wkb = self.sb(st, "wkb", [128, 512], BF16)
            s.dma("sp", wqf[:], self.w_qup[l].rearrange("(k p) n -> p k n", p=128), reads=[self.w_qup], writes=[wqf])
            s.dma("sp", wkf[:], self.w_kvup[l], reads=[self.w_kvup], writes=[wkf])
            for k in range(2):
                self.ts("dve", wqb[:, k, :], wqf[:, k, :], self.gq[:, l, k:k + 1], None, ALU.mult, reads=[wqf, self.gq], writes=[wqb])
            self.ts("dve", wkb[:], wkf[:], self.gkv[:, l:l + 1], None, ALU.mult, reads=[wkf, self.gkv], writes=[wkb])
            ps = Rot([self.psb(st, "psB") for _ in range(6)])
            cq_r = Rot([self.sb(st, "cqb", [128, 2, 512], BF16) for _ in range(2)])
            ckv_r = Rot([self.sb(st, "ckvb", [128, 512], BF16) for _ in range(2)])
            kr_r = Rot([self.sb(st, "krb", [32, 512], BF16) for _ in range(2)])
            cos_r = Rot([self.sb(st, "cosb", [32, 512], F32) for _ in range(2)])
            sin_r = Rot([self.sb(st, "sinb", [32, 512], F32) for _ in range(2)])
            sq_r = Rot([self.sb(st, "sqb", [128, 2, 512], BF16) for _ in range(2)])
            sqk_r = Rot([self.sb(st, "sqk", [128, 512], BF16) for _ in range(2)])
            rq_r = Rot([self.sb(st, "rq", [128, 512], F32) for _ in range(2)])
            rk_r = Rot([self.sb(st, "rk", [128, 512], F32) for _ in range(2)])
            rkt_r = Rot([self.sb(st, "rkt", [128, 4], F32) for _ in range(2)])
            qo_r = Rot([self.sb(st, "qo", [64, 512], BF16) for _ in range(3)])
            xr_r = Rot([self.sb(st, "xr", [32, 512], BF16) for _ in range(3)])
            t1_r = Rot([self.sb(st, "t1", [32, 512], F32) for _ in range(3)])
            t2_r = Rot([self.sb(st, "t2", [32, 512], F32) for _ in range(3)])
            ro_r = Rot([self.sb(st, "ro", [32, 512], BF16) for _ in range(3)])
            vst_r = Rot([self.sb(st, "vst", [128, 4, 4, 65], BF16) for _ in range(2)])
            for v in vst_r.bufs:
                self.ms("dve", v[:, :, :, 64:65], 1.0, [v])

            def rope(xr, cosb, sinb, dsts):
                p = ps.next()
                self.mm(p[0:32, :], self.rotb[:], xr[:], True, True, [self.rotb, xr], [p])
                t1, t2, ro = t1_r.next(), t2_r.next(), ro_r.next()
                self.tt("pool", t1[:], xr[:], cosb[:], ALU.mult, [xr, cosb], [t1])
                self.tt("dve", t2[:], p[0:32, :], sinb[:], ALU.mult, [p, sinb], [t2])
                self.tt("dve", ro[:], t1[:], t2[:], ALU.add, [t1, t2], [ro])
                for dbuf, dap in dsts:
                    s.dma("pool", dap, ro[:], reads=[ro], writes=[dbuf])

            for b in range(NB):
                c0, c1 = b * 512, (b + 1) * 512
                cq, ckv, kr, cosb, sinb = cq_r.next(), ckv_r.next(), kr_r.next(), cos_r.next(), sin_r.next()
                s.dma("sp", cq[:], self.cqT[:, c0:c1].rearrange("(k p) t -> p k t", p=128), reads=[self.cqT], writes=[cq])
                s.dma("sp", ckv[:], self.ckvT[:, c0:c1], reads=[self.ckvT], writes=[ckv])
                s.dma("sp", kr[:], self.kropeT[:, c0:c1], reads=[self.kropeT], writes=[kr])
                s.dma("sp", cosb[:], self.cosT[:, c0:c1], reads=[self.cosT], writes=[cosb])
                s.dma("sp", sinb[:], self.sinT[:, c0:c1], reads=[self.sinT], writes=[sinb])
                sq, sqk, rq, rk, rkt = sq_r.next(), sqk_r.next(), rq_r.next(), rk_r.next(), rkt_r.next()
                self.tt("pool", sq[:], cq[:], cq[:], ALU.mult, [cq], [sq])
                p = ps.next()
                for k in range(2):
                    self.mm(p[:, :], self.onesb[:], sq[:, k, :], k == 0, k == 1, [self.onesb, sq], [p])
                self.act(rq[:], p[:, :], AF.Sqrt, [p, self.epsb], [rq], scale=1.0 / 256, bias=self.epsb[:, 0:1])
                self.recip(rq[:], rq[:], [rq], [rq])
                self.tt("pool", sqk[:], ckv[:], ckv[:], ALU.mult, [ckv], [sqk])
                p2 = ps.next()
                self.mm(p2[:, :], self.onesb[:], sqk[:], True, True, [self.onesb, sqk], [p2])
                self.act(rk[:], p2[:, :], AF.Sqrt, [p2, self.epsb], [rk], scale=1.0 / 128, bias=self.epsb[:, 0:1])
                self.recip(rk[:], rk[:], [rk], [rk])
                p3 = ps.next()
                for tt_ in range(4):
                    self.mm(p3[:, tt_:tt_ + 1], sqk[:, tt_ * 128:(tt_ + 1) * 128], self.onesb[:, 0:1], True, True, [self.onesb, sqk], [p3])
                self.act(rkt[:], p3[:, 0:4], AF.Sqrt, [p3, self.epsb], [rkt], scale=1.0 / 128, bias=self.epsb[:, 0:1])
                self.recip(rkt[:], rkt[:], [rkt], [rkt])
                for h in range(4):
                    pn = ps.next()
                    for k in range(2):
                        self.mm(pn[0:64, :], wqb[:, k, h * 96:h * 96 + 64], cq[:, k, :], k == 0, k == 1, [wqb, cq], [pn])
                    qo = qo_r.next()
                    self.tt("dve", qo[:], pn[0:64, :], rq[0:64, :], ALU.mult, [pn, rq], [qo])
                    s.dma("pool", self.qT[h, 0:64, c0:c1], qo[:], reads=[qo], writes=[self.qT])
                    pr = ps.next()
                    for k in range(2):
                        self.mm(pr[0:32, :], wqb[:, k, h * 96 + 64:h * 96 + 96], cq[:, k, :], k == 0, k == 1, [wqb, cq], [pr])
                    xr = xr_r.next()
                    self.tt("dve", xr[:], pr[0:32, :], rq[0:32, :], ALU.mult, [pr, rq], [xr])
                    rope(xr, cosb, sinb, [(self.qT, self.qT[h, 64:96, c0:c1])])
                for h in range(4):
                    pn = ps.next()
                    self.mm(pn[0:64, :], wkb[:, h * 128:h * 128 + 64], ckv[:], True, True, [wkb, ckv], [pn])
                    qo = qo_r.next()
                    self.tt("dve", qo[:], pn[0:64, :], rk[0:64, :], ALU.mult, [pn, rk], [qo])
                    s.dma("pool", self.kT[h, 0:64, c0:c1], qo[:], reads=[qo], writes=[self.kT])
                rope(kr, cosb, sinb, [(self.kT, self.kT[h, 64:96, c0:c1]) for h in range(4)])
                vst = vst_r.next()
                for tt_ in range(4):
                    pv = ps.next()
                    self.mm(pv[:, :], ckv[:, tt_ * 128:(tt_ + 1) * 128], wkb[:], True, True, [wkb, ckv], [pv])
                    self.act(vst[:, tt_, :, 0:64], pv[:, :].rearrange("p (h c) -> p h c", h=4)[:, :, 64:128], AF.Copy, [pv, rkt], [vst],
                             scale=rkt[:, tt_:tt_ + 1])
                s.dma("pool", self.vaug[c0:c1, :, :].rearrange("(t p) h c -> p t h c", p=128), vst[:], reads=[vst], writes=[self.vaug])

    def attn_finish(self, oacc, obuf_r, oT_r, psT_r, branch, t):
        rec = self.rec_r.next()
        o = obuf_r.next()
        acc3 = oacc[:, 0:260].rearrange("p (h c) -> p h c", h=4)
        self.recip(rec[:, :], acc3[:, :, 64], [oacc], [rec])
        self.tt("dve", o[:].rearrange("p (h c) -> p h c", h=4), acc3[:, :, 0:64],
                rec[:, :].unsqueeze(2).to_broadcast([128, 4, 64]), ALU.mult, [oacc, rec], [o])
        self.to_oT(o, oT_r, psT_r, branch, t)

    def to_oT(self, o, oT_r, psT_r, branch, t):
        s = self.s
        pT = psT_r.next()
        for k in range(2):
            self.tr(pT[:, k * 128:(k + 1) * 128], o[:, k * 128:(k + 1) * 128], [o], [pT])
        oTt = oT_r.next()
        self.act(oTt[:].rearrange("p k q -> p (k q)"), pT[:, 0:256], AF.Copy, [pT], [oTt])
        s.dma("pool", self.oT[branch, :, t * 128:(t + 1) * 128].rearrange("(k p) q -> p k q", p=128), oTt[:], reads=[oTt], writes=[self.oT])

    def phase_C(self, l):
        s, S, NT = self.s, self.S, self.NT
        scale = float(96 ** -0.5)
        with self.scope() as st:
            kT = self.sb(st, "kTs", [96, 4, S], BF16)
            qT = self.sb(st, "qTs", [96, 4, S], BF16)
            va = self.sb(st, "vas", [128, NT, 4, 65], BF16)
            for h in range(4):
                s.dma("sp", kT[:, h, :], self.kT[h], reads=[self.kT], writes=[kT])
                s.dma("sp", qT[:, h, :], self.qT[h], reads=[self.qT], writes=[qT])
            s.dma("sp", va[:], self.vaug[:, :, :].rearrange("(t p) h c -> p t h c", p=128), reads=[self.vaug], writes=[va])
            ps_st = Rot([self.psb(st, "psS") for _ in range(3)])
            ps_o = Rot([self.psb(st, "psO") for _ in range(2)])
            psT_r = Rot([self.psb(st, "psT", BF16) for _ in range(1)])
            pT_r = Rot([self.sb(st, "pT", [128, 512], BF16) for _ in range(3)])
            self.rec_r = Rot([self.sb(st, "rec", [128, 4], F32) for _ in range(2)])
            obuf_r = Rot([self.sb(st, "ob", [128, 256], BF16) for _ in range(2)])
            oT_r = Rot([self.sb(st, "oTt", [128, 2, 128], BF16) for _ in range(2)])
            for t in range(NT):
                oacc = ps_o.next()
                q0, q1 = t * 128, (t + 1) * 128
                def pv(j, pT):
                    for h in range(4):
                        self.mm(oacc[:, h * 65:(h + 1) * 65], pT[:, h * 128:(h + 1) * 128], va[:, j, h, :], (j == 0 and h == 0), j == t, [pT, va], [oacc])

                prev = None
                for j in range(t + 1):
                    k0, k1 = j * 128, (j + 1) * 128
                    sp = ps_st.next()
                    for h in range(4):
                        self.mm(sp[:, h * 128:(h + 1) * 128], kT[:, h, k0:k1], qT[:, h, q0:q1], h == 0, j != t, [kT, qT], [sp])
                    if j == t:
                        for h in range(4):
                            self.mm(sp[:, h * 128:(h + 1) * 128], self.identb[:], self.cmtb[:], False, True, [self.identb, self.cmtb], [sp])
                    pT = pT_r.next()
                    self.act(pT[:], sp[:, :], AF.Exp, [sp], [pT], scale=scale)
                    if prev is not None:
                        pv(*prev)
                    prev = (j, pT)
                pv(*prev)
                self.attn_finish(oacc, obuf_r, oT_r, psT_r, 0, t)

    def phase_E(self, l):
        s, S = self.s, self.S
        cw = self.convw
        with self.scope() as st:
            cb = self.sb(st, "cb", [128, S], BF16)
            cc = self.sb(st, "cc", [128, S], BF16)
            cx = self.sb(st, "cx", [128, S], BF16)
            u = self.sb(st, "u", [128, S + 2], F32)
            y = self.sb(st, "y", [128, S], F32)
            o = self.sb(st, "o", [128, S], BF16)
            for c in range(2):
                r0, r1 = c * 128, (c + 1) * 128
                s.dma("sp", cb[:], self.cbT[r0:r1, :], reads=[self.cbT], writes=[cb])
                s.dma("sp", cc[:], self.ccT[r0:r1, :], reads=[self.ccT], writes=[cc])
                s.dma("sp", cx[:], self.cxT[r0:r1, :], reads=[self.cxT], writes=[cx])
                self.ms("pool", u[:, 0:2], 0.0, [u])
                self.tt("pool", u[:, 2:S + 2], cc[:], cx[:], ALU.mult, [cc, cx], [u])
                self.ts("dve", y[:], u[:, 2:S + 2], cw[:, l, c, 2:3], None, ALU.mult, reads=[u, cw], writes=[y])
                self.stt(y[:], u[:, 1:S + 1], cw[:, l, c, 1:2], y[:], ALU.mult, ALU.add, [u, cw, y], [y])
                self.stt(y[:], u[:, 0:S], cw[:, l, c, 0:1], y[:], ALU.mult, ALU.add, [u, cw, y], [y])
                self.tt("pool", o[:], cb[:], y[:], ALU.mult, [cb, y], [o])
                s.dma("pool", self.oT[1, r0:r1, :], o[:], reads=[o], writes=[self.oT])

    def phase_D(self, l):
        s, S, NT = self.s, self.S, self.NT
        cidx = float(32 ** -0.5 * 8 ** -0.5)
        scale = float(64 ** -0.5)
        with self.scope() as st:
            ikT = self.sb(st, "ikTs", [32, S], BF16)
            dkT = self.sb(st, "dkTs", [64, S], BF16)
            dva = self.sb(st, "dvas", [128, NT, 65], BF16)
            iw = self.sb(st, "iws", [128, NT, 8], F32)
            s.dma("sp", ikT[:], self.ikT[:, :], reads=[self.ikT], writes=[ikT])
            s.dma("sp", dkT[:], self.dkT[:, :], reads=[self.dkT], writes=[dkT])
            s.dma("sp", dva[:], self.dvaug[:, :].rearrange("(t p) c -> p t c", p=128), reads=[self.dvaug], writes=[dva])
            s.dma("sp", iw[:], self.iw[:, :].rearrange("(t p) c -> p t c", p=128), reads=[self.iw], writes=[iw])
            iq_r = Rot([self.sb(st, "iqt", [32, 8, 128], BF16) for _ in range(3)])
            dq_r = Rot([self.sb(st, "dqt", [64, 4, 128], BF16) for _ in range(4)])
            sc_r = [self.sb(st, "sc", [128, S], F32) for _ in range(2)]
            mb_r = [self.sb(st, "mb", [128, S], BF16) for _ in range(2)]
            junk = self.sb(st, "junkd", [128, S], BF16)
            ps_rel = Rot([self.psb(st, "psR") for _ in range(2)])
            ps_sc = Rot([self.psb(st, "psSc") for _ in range(1)])
            ps_st = Rot([self.psb(st, "psS") for _ in range(2)])
            ps_o = Rot([self.psb(st, "psO") for _ in range(2)])
            psT_r = Rot([self.psb(st, "psT", BF16) for _ in range(1)])
            rl_r = Rot([self.sb(st, "rl", [128, 512], BF16) for _ in range(4)])
            dm_r = Rot([self.sb(st, "dm", [128, 8, 128], BF16) for _ in range(2)])
            pT_r = Rot([self.sb(st, "pT", [128, 512], BF16) for _ in range(3)])
            self.rec_r = Rot([self.sb(st, "rec", [128, 4], F32) for _ in range(2)])
            obuf_r = Rot([self.sb(st, "ob", [128, 256], BF16) for _ in range(2)])
            oT_r = Rot([self.sb(st, "oTt", [128, 2, 128], BF16) for _ in range(2)])
            sm = {k: self.sb(st, k, [128, 1], F32) for k in ("mx", "w0", "lo", "mid", "cnt", "tmp", "thr")}
            dq_of = {}

            def score(t):
                q0, q1 = t * 128, (t + 1) * 128
                nk = (t + 1) * 128
                sc = sc_r[t % 2]
                iq = iq_r.next()
                dq = dq_r.next()
                dq_of[t] = dq
                s.dma("sp", iq[:], self.iqT[:, :, q0:q1], reads=[self.iqT], writes=[iq])
                s.dma("sp", dq[:], self.dqT[:, :, q0:q1], reads=[self.dqT], writes=[dq])
                dm = dm_r.next()
                for h in range(8):
                    self.ts("pool", dm[:, h, :], self.identf[:], iw[:, t, h:h + 1], cidx, ALU.mult, ALU.mult, reads=[self.identf, iw], writes=[dm])
                nb = (nk + 511) // 512
                for b in range(nb):
                    k0 = b * 512
                    w = min(512, nk - k0)
                    psc = ps_sc.next()
                    diag = (b == nb - 1)
                    prev = None
                    for h in range(8):
                        pr = ps_rel.next()
                        self.mm(pr[:, 0:w], iq[:, h, :], ikT[:, k0:k0 + w], True, True, [iq, ikT], [pr])
                        rl = rl_r.next()
                        self.act(rl[:, 0:w], pr[:, 0:w], AF.Relu, [pr], [rl])
                        if prev is not None:
                            hh, prl = prev
                            self.mm(psc[:, 0:w], dm[:, hh, :], prl[:, 0:w], hh == 0, False, [dm, prl], [psc])
                        prev = (h, rl)
                    hh, prl = prev
                    self.mm(psc[:, 0:w], dm[:, hh, :], prl[:, 0:w], False, not diag, [dm, prl], [psc])
                    if diag:
                        self.mm(psc[:, w - 128:w], self.identb[:], self.cmqb[:], False, True, [self.identb, self.cmqb], [psc])
                    self.act(sc[:, k0:k0 + w], psc[:, 0:w], AF.Copy, [psc], [sc])
                if self.dsc is not None:
                    s.dma("pool", self.dsc[q0:q1, 0:nk], sc[:, 0:nk], reads=[sc], writes=[self.dsc])

            def bisect(t):
                nk = (t + 1) * 128
                sc, mb = sc_r[t % 2], mb_r[t % 2]
                thr = sm["thr"]
                if t < 2:
                    self.ms("dve", thr[:], -10000.0, [thr])
                else:
                    mx, w0, lo, mid, cnt, tmp = (sm[k] for k in ("mx", "w0", "lo", "mid", "cnt", "tmp"))
                    s.op("dve", "tensor_reduce", (), dict(out=mx[:], in_=sc[:, 0:nk], axis=AX.X, op=ALU.max), [sc], [mx])
                    s.op("dve", "tensor_reduce", (), dict(out=lo[:], in_=sc[:, 0:nk - 128], axis=AX.X, op=ALU.min), [sc], [lo])
                    self.tt("dve", w0[:], mx[:], lo[:], ALU.subtract, [mx, lo], [w0])
                    self.ts("dve", w0[:], w0[:], 1.001, 1e-6, ALU.mult, ALU.add, reads=[w0], writes=[w0])
                    for it in range(NBIS):
                        f = float(2.0 ** -(it + 1))
                        self.stt(mid[:], w0[:], f, lo[:], ALU.mult, ALU.add, [w0, lo], [mid])
                        self.ts("dve", junk[:, 0:nk], sc[:, 0:nk], mid[:, 0:1], None, ALU.is_ge, ALU.add, reads=[sc, mid], writes=[junk, cnt], accum_out=cnt[:])
                        self.ts("dve", tmp[:], cnt[:], 255.5, f, ALU.is_ge, ALU.mult, reads=[cnt], writes=[tmp])
                        self.stt(lo[:], tmp[:], w0[:, 0:1], lo[:], ALU.mult, ALU.add, [tmp, w0, lo], [lo])
                    self.cp("dve", thr[:], lo[:], [lo], [thr])
                self.ts("dve", mb[:, 0:nk], sc[:, 0:nk], thr[:, 0:1], -30000.0, ALU.is_lt, ALU.mult, reads=[sc, thr], writes=[mb])

            def attend(t):
                mb = mb_r[t % 2]
                dq = dq_of.pop(t)
                oacc = ps_o.next()
                def pv(j, pT):
                    for h in range(4):
                        self.mm(oacc[:, h * 65:(h + 1) * 65], pT[:, h * 128:(h + 1) * 128], dva[:, j, :], (j == 0 and h == 0), j == t, [pT, dva], [oacc])

                prev = None
                for j in range(t + 1):
                    k0, k1 = j * 128, (j + 1) * 128
                    sp = ps_st.next()
                    self.mm(sp[:, :], dkT[:, k0:k1], dq[:, :, :].rearrange("d h q -> d (h q)"), True, False, [dkT, dq], [sp])
                    self.mm(sp[:, :], mb[:, k0:k1], self.i4b[:], False, True, [mb, self.i4b], [sp])
                    pT = pT_r.next()
                    self.act(pT[:], sp[:, :], AF.Exp, [sp], [pT], scale=scale)
                    if prev is not None:
                        pv(*prev)
                    prev = (j, pT)
                pv(*prev)
                self.attn_finish(oacc, obuf_r, oT_r, psT_r, 2, t)

            score(0)
            if NT > 1:
                score(1)
            bisect(0)
            for t in range(NT):
                if t + 2 < NT:
                    score(t + 2)
                if t + 1 < NT:
                    bisect(t + 1)
                attend(t)

    def phase_F(self, l):
        s, S, NT = self.s, self.S, self.NT
        with self.scope() as st:
            gqT = self.sb(st, "gqTs", [32, 4, S], BF16)
            gkT = self.sb(st, "gkTs", [32, 4, S], BF16)
            gk = self.sb(st, "gks", [128, NT, 128], BF16)
            gv = self.sb(st, "gvs", [128, NT, 256], BF16)
            grs = self.sb(st, "grss", [128, NT, 256], BF16)
            glrT = self.sb(st, "glrTs", [17, S], BF16)
            wgf = self.sb(st, "wgf", [17, 128], F32)
            wgb = self.sb(st, "wgb", [17, 128], BF16)
            ngb = self.sb(st, "ngb", [128, 64], F32)
            s.dma("sp", gqT[:], self.gqT[:, :, :], reads=[self.gqT], writes=[gqT])
            s.dma("sp", gkT[:], self.gkT[:, :, :], reads=[self.gkT], writes=[gkT])
            s.dma("sp", gk[:], self.gk[:, :].rearrange("(t p) c -> p t c", p=128), reads=[self.gk], writes=[gk])
            s.dma("sp", gv[:], self.gv[:, :].rearrange("(t p) c -> p t c", p=128), reads=[self.gv], writes=[gv])
            s.dma("sp", grs[:], self.grs[:, :].rearrange("(t p) c -> p t c", p=128), reads=[self.grs], writes=[grs])
            s.dma("sp", glrT[:], self.glrT[:, :], reads=[self.glrT], writes=[glrT])
            s.dma("sp", wgf[0:16, :], self.gla_wg[l], reads=[self.gla_wg], writes=[wgf])
            s.dma("sp", wgf[16:17, :], self.gla_bg[l], reads=[self.gla_bg], writes=[wgf])
            s.dma("sp", ngb[:], self.gla_ng[l].partition_broadcast(128), reads=[self.gla_ng], writes=[ngb])
            self.cp("dve", wgb[:], wgf[:], [wgf], [wgb])
            state = self.sb(st, "state", [32, 4, 64], F32)
            stateb = self.sb(st, "stateb", [32, 4, 64], BF16)
            self.ms("dve", state[:], 0.0, [state])
            self.ms("dve", stateb[:], 0.0, [stateb])
            ps_z = Rot([self.psb(st, "psZ") for _ in range(1)])
            ps_c = Rot([self.psb(st, "psC") for _ in range(2)])
            ps_a = Rot([self.psb(st, "psA") for _ in range(1)])
            ps_o = Rot([self.psb(st, "psO") for _ in range(2)])
            ps_u = Rot([self.psb(st, "psU") for _ in range(1)])
            psT_r = Rot([self.psb(st, "psT", BF16) for _ in range(1)])
            ez_r = Rot([self.sb(st, "ez", [128, 128], F32) for _ in range(2)])
            sp_r = Rot([self.sb(st, "sp", [128, 128], F32) for _ in range(2)])
            ec_r = Rot([self.sb(st, "ec", [32, 4, 128], F32) for _ in range(2)])
            en_r = Rot([self.sb(st, "en", [32, 4, 128], F32) for _ in range(2)])
            er_r = Rot([self.sb(st, "er", [128, 128], F32) for _ in range(2)])
            qt_r = Rot([self.sb(st, "qt", [32, 4, 128], BF16) for _ in range(2)])
            kt_r = Rot([self.sb(st, "kt", [32, 4, 128], BF16) for _ in range(2)])
            kh_r = Rot([self.sb(st, "kh", [128, 128], BF16) for _ in range(2)])
            am_r = Rot([self.sb(st, "am", [128, 4, 128], BF16) for _ in range(2)])
            on_r = Rot([self.sb(st, "on", [128, 4, 64], F32) for _ in range(2)])
            og_r = Rot([self.sb(st, "og", [128, 256], BF16) for _ in range(2)])
            ss_r = Rot([self.sb(st, "ssg", [128, 4], F32) for _ in range(2)])
            junk = self.sb(st, "junkg", [128, 64], F32)
            oT_r = Rot([self.sb(st, "oTt", [128, 2, 128], BF16) for _ in range(2)])
            qscale = float(32 ** -0.5)
            for t in range(NT):
                c0, c1 = t * 128, (t + 1) * 128
                pz = ps_z.next()
                self.mm(pz[:, 0:128], glrT[:, c0:c1], wgb[:], True, True, [glrT, wgb], [pz])
                ez, sp = ez_r.next(), sp_r.next()
                self.act(ez[:], pz[:, 0:128], AF.Exp, [pz], [ez], scale=-1.0)
                self.act(sp[:], ez[:], AF.Ln, [ez], [sp], bias=1.0)
                pc = ps_c.next()
                for h in range(4):
                    self.mm(pc[0:32, h * 128:(h + 1) * 128], sp[:, h * 32:(h + 1) * 32], self.tri[:], True, True, [sp, self.tri], [pc])
                prv = ps_c.next()
                self.mm(prv[:, 0:128], self.rev[:], sp[:], True, True, [sp, self.rev], [prv])
                ec, en, er = ec_r.next(), en_r.next(), er_r.next()
                self.act(ec[:].rearrange("p h i -> p (h i)"), pc[0:32, :], AF.Exp, [pc], [ec])
                self.act(en[:].rearrange("p h i -> p (h i)"), pc[0:32, :], AF.Exp, [pc], [en], scale=-1.0)
                self.act(er[:], prv[:, 0:128], AF.Exp, [prv], [er])
                qt, kt, kh = qt_r.next(), kt_r.next(), kh_r.next()
                self.stt(qt[:], gqT[:, :, c0:c1], qscale, ec[:], ALU.mult, ALU.mult, [gqT, ec], [qt])
                self.tt("pool", kt[:], gkT[:, :, c0:c1], en[:], ALU.mult, [gkT, en], [kt])
                self.tt("pool", kh[:], gk[:, t, :], er[:], ALU.mult, [gk, er], [kh])
                pa = ps_a.next()
                for h in range(4):
                    self.mm(pa[:, h * 128:(h + 1) * 128], kt[:, h, :], qt[:, h, :], True, True, [kt, qt], [pa])
                am = am_r.next()
                self.tt("dve", am[:], pa[:, :].rearrange("p (h i) -> p h i", h=4), self.gmaskb[:].unsqueeze(1).to_broadcast([128, 4, 128]),
                        ALU.mult, [pa, self.gmaskb], [am])
                po = ps_o.next()
                for h in range(4):
                    self.mm(po[:, h * 64:(h + 1) * 64], am[:, h, :], gv[:, t, h * 64:(h + 1) * 64], h == 0, False, [am, gv], [po])
                for c in range(2):
                    i0, i1 = c * 64, (c + 1) * 64
                    for h in range(4):
                        self.mm(po[i0:i1, h * 64:(h + 1) * 64], qt[:, h, i0:i1], stateb[:, h, :], False, True, [qt, stateb], [po])
                    pu = ps_u.next()
                    for h in range(4):
                        self.mm(pu[0:32, h * 64:(h + 1) * 64], kh[i0:i1, h * 32:(h + 1) * 32], gv[i0:i1, t, h * 64:(h + 1) * 64], True, True, [kh, gv], [pu])
                    self.tt("dve", state[:], state[:], ec[:, :, i1 - 1:i1].to_broadcast([32, 4, 64]), ALU.mult, [state, ec], [state])
                    self.tt("dve", state[:], state[:], pu[0:32, 0:256].rearrange("p (h v) -> p h v", h=4), ALU.add, [state, pu], [state])
                    self.cp("dve", stateb[:], state[:], [state], [stateb])
                ss, on, og = ss_r.next(), on_r.next(), og_r.next()
                for h in range(4):
                    self.act(junk[:], po[:, h * 64:(h + 1) * 64], AF.Square, [po], [junk, ss], accum_out=ss[:, h:h + 1])
                self.act(ss[:], ss[:], AF.Sqrt, [ss, self.epsb], [ss], scale=1.0 / 64, bias=self.epsb[:, 0:1])
                self.recip(ss[:], ss[:], [ss], [ss])
                self.tt("dve", on[:], po[:, 0:256].rearrange("p (h v) -> p h v", h=4), ss[:].unsqueeze(2).to_broadcast([128, 4, 64]),
                        ALU.mult, [po, ss], [on])
                self.tt("pool", on[:], on[:], ngb[:].unsqueeze(1).to_broadcast([128, 4, 64]), ALU.mult, [on, ngb], [on])
                self.tt("pool", og[:], on[:].rearrange("p h v -> p (h v)"), grs[:, t, :], ALU.mult, [on, grs], [og])
                self.to_oT(og, oT_r, psT_r, 3, t)

    def phase_G(self, l):
        s, S, NT, NB = self.s, self.S, self.NT, self.NB
        src = self.x_in if l == 0 else self.xres
        with self.scope() as st:
            wbb = self.sb(st, "wbb", [128, 4, 2, D], BF16)
            wob = self.sb(st, "wob", [128, 8, D], BF16)
            for n in range(4):
                s.dma("pool", wbb[:, n, :, :], self.w_br[l, n].rearrange("(k p) c -> p k c", p=128), reads=[self.w_br], writes=[wbb])
            for kk in range(4):
                s.dma("pool", wob[:, 2 * kk:2 * kk + 2, :], self.w_out[l, kk * 256:(kk + 1) * 256, :].rearrange("(k p) c -> p k c", p=128),
                      reads=[self.w_out], writes=[wob])
            ps = Rot([self.psb(st, "psG") for _ in range(4)])
            ob_r = Rot([self.sb(st, "obk", [128, 4, 2, 512], BF16) for _ in range(2)])
            gt_r = Rot([self.sb(st, "gtk", [128, 32, 512], BF16) for _ in range(2)])
            mf_r = Rot([self.sb(st, "mf", [128, 512], F32) for _ in range(2)])
            tf_r = Rot([self.sb(st, "tf", [128, 512], F32) for _ in range(3)])
            mT_r = Rot([self.sb(st, "mT", [128, 8, 512], BF16) for _ in range(2)])
            xt_r = Rot([self.sb(st, "xtg", [128, D], F32) for _ in range(2)])
            xn_r = Rot([self.sb(st, "xng", [128, D], F32) for _ in range(2)])
            for b in range(NB):
                c0, c1 = b * 512, (b + 1) * 512
                ob, gt, mT = ob_r.next(), gt_r.next(), mT_r.next()
                for n in range(4):
                    s.dma("sp", ob[:, n, :, :], self.oT[n, :, c0:c1].rearrange("(k p) t -> p k t", p=128), reads=[self.oT], writes=[ob])
                for n in range(4):
                    s.dma("sp", gt[:, n * 8:(n + 1) * 8, :], self.gateT[n * 1024:(n + 1) * 1024, c0:c1].rearrange("(g p) t -> p g t", p=128),
                          reads=[self.gateT], writes=[gt])
                for cg in range(8):
                    mf = mf_r.next()
                    for n in range(4):
                        p = ps.next()
                        for k in range(2):
                            self.mm(p[:, :], wbb[:, n, k, cg * 128:(cg + 1) * 128], ob[:, n, k, :], k == 0, k == 1, [wbb, ob], [p])
                        if n == 0:
                            self.tt("dve", mf[:], p[:, :], gt[:, n * 8 + cg, :], ALU.mult, [p, gt], [mf])
                        else:
                            tf = tf_r.next()
                            self.tt("dve", tf[:], p[:, :], gt[:, n * 8 + cg, :], ALU.mult, [p, gt], [tf])
                            if n < 3:
                                self.tt("pool", mf[:], mf[:], tf[:], ALU.add, [mf, tf], [mf])
                            else:
                                self.tt("pool", mT[:, cg, :], mf[:], tf[:], ALU.add, [mf, tf], [mT])
                for tt_ in range(4):
                    r0 = c0 + tt_ * 128
                    xt, xn = xt_r.next(), xn_r.next()
                    s.dma("sp", xt[:], src[r0:r0 + 128, :], reads=[src], writes=[xt])
                    for half in range(2):
                        p = ps.next()
                        for cg in range(8):
                            self.mm(p[:, :], mT[:, cg, tt_ * 128:(tt_ + 1) * 128], wob[:, cg, half * 512:(half + 1) * 512], cg == 0, cg == 7, [mT, wob], [p])
                        self.tt("dve", xn[:, half * 512:(half + 1) * 512], p[:, :], xt[:, half * 512:(half + 1) * 512], ALU.add, [p, xt], [xn])
                    s.dma("pool", self.xres[r0:r0 + 128, :], xn[:], reads=[xn], writes=[self.xres])

    def phase_H(self, l):
        s, S = self.s, self.S
        SB = 1024
        NG = DFF // 128
        with self.scope() as st:
            hT = self.sb(st, "h2T", [128, 8, SB], BF16)
            aT = self.sb(st, "aT", [128, NG, SB], BF16)
            nt = self.norm_tiles(st)
            ps = Rot([self.psb(st, "psH") for _ in range(4)])
            wgb_r = Rot([self.sb(st, "wgbf", [128, 8, 128], BF16) for _ in range(3)])
            wub_r = Rot([self.sb(st, "wubf", [128, 8, 128], BF16) for _ in range(3)])
            wdb_r = Rot([self.sb(st, "wdb", [128, NG, 512], BF16) for _ in range(2)])
            sg_r = Rot([self.sb(st, "sg", [128, 512], BF16) for _ in range(3)])
            xt_r = Rot([self.sb(st, "xth", [128, 512], F32) for _ in range(3)])
            xn_r = Rot([self.sb(st, "xnh", [128, 512], F32) for _ in range(3)])

            def load_gu(g):
                wgb, wub = wgb_r.next(), wub_r.next()
                s.dma("pool", wgb[:], self.w_fg[l, :, g * 128:(g + 1) * 128].rearrange("(k p) n -> p k n", p=128), reads=[self.w_fg], writes=[wgb])
                s.dma("pool", wub[:], self.w_fu[l, :, g * 128:(g + 1) * 128].rearrange("(k p) n -> p k n", p=128), reads=[self.w_fu], writes=[wub])
                return wgb, wub

            def load_d(cq):
                wdb = wdb_r.next()
                for kk in range(2):
                    s.dma("pool", wdb[:, kk * 11:(kk + 1) * 11, :], self.w_fd[l, kk * 1408:(kk + 1) * 1408, cq * 512:(cq + 1) * 512].rearrange("(k p) n -> p k n", p=128),
                          reads=[self.w_fd], writes=[wdb])
                return wdb

            for sbk in range(S // SB):
                t0 = sbk * SB
                self.norm_to_hT(nt, self.xres, self.gffn, l, hT, t0, SB // 128)
                nxt = load_gu(0)
                for g in range(NG):
                    wgb, wub = nxt
                    if g + 1 < NG:
                        nxt = load_gu(g + 1)
                    for b in range(SB // 512):
                        pg, pu = ps.next(), ps.next()
                        for k in range(8):
                            self.mm(pg[:, :], wgb[:, k, :], hT[:, k, b * 512:(b + 1) * 512], k == 0, k == 7, [wgb, hT], [pg])
                        for k in range(8):
                            self.mm(pu[:, :], wub[:, k, :], hT[:, k, b * 512:(b + 1) * 512], k == 0, k == 7, [wub, hT], [pu])
                        sg = sg_r.next()
                        self.act(sg[:], pg[:, :], AF.Silu, [pg], [sg])
                        self.tt("dve", aT[:, g, b * 512:(b + 1) * 512], pu[:, :], sg[:], ALU.mult, [pu, sg], [aT])
                nxtd = load_d(0)
                for cq in range(2):
                    wdb = nxtd
                    if cq + 1 < 2:
                        nxtd = load_d(cq + 1)
                    for tt_ in range(SB // 128):
                        r0 = t0 + tt_ * 128
                        xt, xn = xt_r.next(), xn_r.next()
                        s.dma("sp", xt[:], self.xres[r0:r0 + 128, cq * 512:(cq + 1) * 512], reads=[self.xres], writes=[xt])
                        p = ps.next()
                        for k in range(NG):
                            self.mm(p[:, :], aT[:, k, tt_ * 128:(tt_ + 1) * 128], wdb[:, k, :], k == 0, k == NG - 1, [aT, wdb], [p])
                        self.tt("dve", xn[:], p[:, :], xt[:], ALU.add, [p, xt], [xn])
                        s.dma("pool", self.xres[r0:r0 + 128, cq * 512:(cq + 1) * 512], xn[:], reads=[xn], writes=[self.xres])

    def phase_final(self):
        s, S, NT = self.s, self.S, self.NT
        with self.scope() as st:
            gf = self.sb(st, "gf", [128, D], F32)
            s.dma("sp", gf[:], self.g_final[:].partition_broadcast(128), reads=[self.g_final], writes=[gf])
            xt_r = Rot([self.sb(st, "xtf", [128, D], F32) for _ in range(3)])
            yo_r = Rot([self.sb(st, "yof", [128, D], F32) for _ in range(3)])
            junk = self.sb(st, "junkf", [128, D], BF16)
            ss_r = Rot([self.sb(st, "ssf", [128, 1], F32) for _ in range(3)])
            for t in range(NT):
                xt, yo, ss = xt_r.next(), yo_r.next(), ss_r.next()
                s.dma("sp", xt[:], self.xres[t * 128:(t + 1) * 128, :], reads=[self.xres], writes=[xt])
                self.act(junk[:], xt[:], AF.Square, [xt], [junk, ss], accum_out=ss[:])
                self.act(ss[:], ss[:], AF.Sqrt, [ss, self.epsb], [ss], scale=1.0 / D, bias=self.epsb[:, 0:1])
                self.recip(ss[:], ss[:], [ss], [ss])
                self.stt(yo[:], xt[:], ss[:, 0:1], gf[:], ALU.mult, ALU.mult, [xt, ss, gf], [yo])
                s.dma("pool", self.y_out[t * 128:(t + 1) * 128, :], yo[:], reads=[yo], writes=[self.y_out])


def make_in_maps(inputs, S, batch_ids):
    f = lambda a: np.ascontiguousarray(np.asarray(a))
    consts = host_consts()
    shared = {
        "w_in": f(inputs["w_in"]), "w_qup": f(inputs["mla_w_q_up"]), "w_kvup": f(inputs["mla_w_kv_up"]),
        "w_br": f(inputs["w_branch"]), "w_out": f(inputs["w_out"]), "w_fg": f(inputs["w_ffn_gate"]),
        "w_fu": f(inputs["w_ffn_up"]), "w_fd": f(inputs["w_ffn_down"]),
        "g_attn": f(np.asarray(inputs["attn_norm_g"]).reshape(NLAYER, 8, 128).transpose(2, 0, 1)),
        "g_ffn": f(np.asarray(inputs["ffn_norm_g"]).reshape(NLAYER, 8, 128).transpose(2, 0, 1)),
        "g_final": f(inputs["final_norm_g"]),
        "g_q": f(np.asarray(inputs["mla_q_norm_g"]).reshape(NLAYER, 2, 128).transpose(2, 0, 1)),
        "g_kv": f(np.asarray(inputs["mla_kv_norm_g"]).transpose(1, 0)),
        "conv_w": f(np.asarray(inputs["conv_w"]).reshape(NLAYER, 3, 2, 128).transpose(3, 0, 2, 1)),
        "gla_wg": f(inputs["gla_w_gate_up"]),
        "gla_bg": f(np.asarray(inputs["gla_b_gate"]).reshape(NLAYER, 1, 128)),
        "gla_ng": f(inputs["gla_norm_g"]),
    }
    shared.update(consts)
    maps = []
    x = np.asarray(inputs["x"])
    pos = np.asarray(inputs["positions"])
    for b in batch_ids:
        m = dict(shared)
        m["x"] = f(x[b, :S])
        m["pos"] = f(pos[b, :S].astype(np.int32))
        maps.append(m)
    return maps


_NC_CACHE = {}


def kernel(**inputs):
    S = 4096
    key = ("full", S)
    if key not in _NC_CACHE:
        _NC_CACHE[key] = Builder(S, NLAYER).build()
    nc = _NC_CACHE[key]
    in_maps = make_in_maps(inputs, S, list(range(8)))
    res = run_bass_kernel_spmd(nc, in_maps, core_ids=list(range(8)))
    out = np.stack([np.asarray(r["y"]) for r in res.results], axis=0).astype(np.float32)
    return out
```

```python
import numpy as np
from contextlib import ExitStack, contextmanager
import concourse.bass as bass
import concourse.mybir as mybir
from concourse.bass_utils import run_bass_kernel_spmd

F32 = mybir.dt.float32
BF16 = mybir.dt.bfloat16
I32 = mybir.dt.int32
AF = mybir.ActivationFunctionType
ALU = mybir.AluOpType
AX = mybir.AxisListType

D = 1024
DFF = 2816
NLAYER = 4
INTOT = 6744
EPS = 1e-6
EPOCH = 30000
NBIS = 12

C_CQ, C_CKV, C_KROPE = 0, 256, 384
C_CB, C_CC, C_CX = 416, 672, 928
C_DQ, C_DK, C_DV = 1184, 1440, 1504
C_IQ, C_IK, C_IW = 1568, 1824, 1856
C_GQ, C_GK, C_GV, C_GLR, C_GR = 1864, 1992, 2120, 2376, 2392
C_GATE = 2648


class Buf:
    def __init__(self, name, t=None):
        self.name = name
        self.t = t
        self.last_write = None
        self.readers = {}
        self.wsem = None
        self.rsem = None

    def __getitem__(self, idx):
        return self.t[idx]


class Sched:
    COMPUTE = ("pe", "act", "dve", "pool")
    SEMCAP = 30000

    def __init__(self, nc):
        self.nc = nc
        self.engs = list(self.COMPUTE) + ["sp"]
        self.ops = {e: [] for e in self.engs}
        self.cnt = {e: 0 for e in self.COMPUTE}
        self.sems = {}
        self.sem_names = []
        self.waited = {e: {} for e in self.engs}
        self.nops = 0
        self.semval = {}
        self.free_dma = []
        self.ndma_sems = 0

    def _sem(self, name):
        if name not in self.sems:
            self.sems[name] = None
            self.sem_names.append(name)
        return name

    def _dma_sem(self):
        if self.free_dma:
            return self.free_dma.pop()
        self.ndma_sems += 1
        name = self._sem(f"d_{self.ndma_sems}")
        self.semval[name] = 0
        return name

    def release(self, bufs):
        for b in bufs:
            for nm in (b.wsem, b.rsem):
                if nm is not None and self.semval[nm] < self.SEMCAP:
                    self.free_dma.append(nm)
            b.wsem = b.rsem = None

    def _compute_token(self, e):
        self.cnt[e] += 1
        k = self.cnt[e]
        ep = (k - 1) // EPOCH
        return (self._sem(f"c_{e}_{ep}"), (k - 1) % EPOCH + 1, e)

    def _need(self, e, tok, waits):
        if tok is None:
            return
        sem, val, src = tok
        if src == e and e == "pe":
            return
        w = self.waited[e]
        if w.get(sem, 0) >= val:
            return
        w[sem] = val
        waits.append((sem, val))

    def _deps(self, e, reads, writes):
        waits = []
        for b in reads:
            self._need(e, b.last_write, waits)
        for b in writes:
            self._need(e, b.last_write, waits)
            for r in b.readers.values():
                self._need(e, r, waits)
        return waits

    def _mark(self, tok, reads, writes):
        for b in reads:
            old = b.readers.get(tok[0])
            if old is None or old[1] < tok[1]:
                b.readers[tok[0]] = tok
        for b in writes:
            b.last_write = tok
            b.readers = {}

    def op(self, e, method, args, kwargs, reads=(), writes=()):
        waits = self._deps(e, reads, writes)
        tok = self._compute_token(e)
        self._mark(tok, reads, writes)
        self.ops[e].append((waits, method, args, kwargs, (tok[0], 1)))
        self.nops += 1
        return tok

    def dma(self, q, out_ap, in_ap, reads=(), writes=(), **kw):
        waits = self._deps(q, reads, writes)
        if writes:
            b = writes[0]
            if b.wsem is None:
                b.wsem = self._dma_sem()
            nm = b.wsem
        else:
            b = reads[0]
            if b.rsem is None:
                b.rsem = self._dma_sem()
            nm = b.rsem
        self.semval[nm] += 16
        tok = (nm, self.semval[nm], "dma")
        self._mark(tok, reads, writes)
        kw = dict(kw)
        kw["out"] = out_ap
        kw["in_"] = in_ap
        self.ops[q].append((waits, "dma_start", (), kw, (nm, 16)))
        self.nops += 1
        return tok

    def wait_all(self, e, bufs):
        waits = self._deps(e, (), bufs)
        if waits:
            self.ops[e].append((waits, None, None, None, None))

    def barrier(self):
        toks = []
        for e in self.COMPUTE:
            k = self.cnt[e]
            if k > 0:
                ep = (k - 1) // EPOCH
                toks.append((f"c_{e}_{ep}", (k - 1) % EPOCH + 1, e))
        for nm, v in self.semval.items():
            if v > 0:
                toks.append((nm, v, "dma"))
        for e in self.engs:
            waits = []
            for tok in toks:
                self._need(e, tok, waits)
            if waits:
                self.ops[e].append((waits, None, None, None, None))

    def emit(self, stack):
        nc = self.nc
        if not hasattr(self, "sem_stack"):
            self.sem_stack = stack
        for name in self.sem_names:
            if self.sems[name] is None:
                self.sems[name] = self.sem_stack.enter_context(nc.semaphore(name))
        if not any(self.ops[e] for e in self.engs):
            return
        with nc.Block() as block:
            amap = {"pe": block.tensor, "act": block.scalar, "dve": block.vector,
                    "pool": block.gpsimd, "sp": block.sync}
            for e in self.engs:
                ops = self.ops[e]
                if not ops:
                    continue

                def body(eng, ops=ops):
                    for waits, method, args, kwargs, inc in ops:
                        for sem, val in waits:
                            eng.wait_ge(self.sems[sem], val)
                        if method is not None:
                            ins = getattr(eng, method)(*args, **kwargs)
                            ins.then_inc(self.sems[inc[0]], inc[1])

                amap[e](body)
                self.ops[e] = []


class Rot:
    def __init__(self, bufs):
        self.bufs = bufs
        self.i = 0

    def next(self):
        b = self.bufs[self.i % len(self.bufs)]
        self.i += 1
        return b


def host_consts():
    j = np.arange(128)[:, None]
    i = np.arange(128)[None, :]
    same = (j // 64) == (i // 64)
    c = {}
    c["c_ident"] = np.eye(128, dtype=np.float32)
    c["c_tri"] = np.where(same & (j <= i), -1.0 / 16.0, 0.0).astype(np.float32)
    c["c_rev"] = np.where(same & (j > i), -1.0 / 16.0, 0.0).astype(np.float32)
    c["c_gmask"] = np.where(same & (j <= i), 1.0, 0.0).astype(np.float32)
    c["c_cmt"] = np.where(j > i, -30000.0, 0.0).astype(np.float32)
    c["c_cmq"] = np.ascontiguousarray(c["c_cmt"].T)
    rot = np.zeros((32, 32), np.float32)
    for fp in range(16):
        rot[fp + 16, fp] = -1.0
    for fp in range(16, 32):
        rot[fp - 16, fp] = 1.0
    c["c_rot"] = rot
    half = 16
    invf = (np.float32(10000.0) ** (-np.arange(half, dtype=np.float32) / np.float32(half))).astype(np.float32)
    c["c_invf"] = np.concatenate([invf, invf]).reshape(32, 1).astype(np.float32)
    return c


class Builder:
    def __init__(self, S, nl, dbg=(), stop_after=None, phases=None):
        self.S = S
        self.NT = S // 128
        self.NB = S // 512
        self.nl = nl
        self.dbg = set(dbg)
        self.stop_after = stop_after
        self.phases = phases
        self.nc = bass.Bass("TRN2", target_bir_lowering=False)
        self.s = Sched(self.nc)
        self.uid = 0
        self.live = []

    def din(self, name, shape, dt=F32):
        return self.nc.dram_tensor(name, list(shape), dt, kind="ExternalInput").ap()

    def dscr(self, name, shape, dt=BF16):
        kind = "ExternalOutput" if name in self.dbg else "Internal"
        ap = self.nc.dram_tensor(name, list(shape), dt, kind=kind).ap()
        return Buf(name, ap)

    def sb(self, st, name, shape, dt):
        self.uid += 1
        nm = f"{name}_{self.uid}"
        b = Buf(nm, st.enter_context(self.nc.sbuf_tensor(nm, list(shape), dt)))
        self.live.append(b)
        return b

    def psb(self, st, name, dt=F32):
        self.uid += 1
        nm = f"{name}_{self.uid}"
        shape = [128, 512] if dt == F32 else [128, 1024]
        b = Buf(nm, st.enter_context(self.nc.psum_tensor(nm, shape, dt)))
        self.live.append(b)
        return b

    @contextmanager
    def scope(self):
        st = ExitStack()
        mark = len(self.live)
        yield st
        self.s.barrier()
        self.s.release(self.live[mark:])
        del self.live[mark:]
        self.s.emit(self.top)
        st.close()

    def mm(self, out, lhsT, rhs, start, stop, reads, writes):
        self.s.op("pe", "matmul", (out,), dict(lhsT=lhsT, rhs=rhs, start=start, stop=stop), reads, writes)

    def tr(self, out, in_, reads, writes):
        self.s.op("pe", "transpose", (), dict(out=out, in_=in_, identity=self.identb[:]), list(reads) + [self.identb], writes)

    def act(self, out, in_, func, reads, writes, **kw):
        self.s.op("act", "activation", (), dict(out=out, in_=in_, func=func, **kw), reads, writes)

    def tt(self, eng, out, in0, in1, op, reads, writes):
        self.s.op(eng, "tensor_tensor", (), dict(out=out, in0=in0, in1=in1, op=op), reads, writes)

    def ts(self, eng, out, in0, s1, s2, op0, op1=None, reads=(), writes=(), accum_out=None):
        kw = dict(out=out, in0=in0, scalar1=s1, scalar2=s2, op0=op0)
        if op1 is not None:
            kw["op1"] = op1
        if accum_out is not None:
            kw["accum_out"] = accum_out
        self.s.op(eng, "tensor_scalar", (), kw, reads, writes)

    def stt(self, out, in0, scalar, in1, op0, op1, reads, writes):
        self.s.op("dve", "scalar_tensor_tensor", (), dict(out=out, in0=in0, scalar=scalar, in1=in1, op0=op0, op1=op1), reads, writes)

    def cp(self, eng, out, in_, reads, writes):
        self.s.op(eng, "tensor_copy", (), dict(out=out, in_=in_), reads, writes)

    def ms(self, eng, ap, val, writes):
        self.s.op(eng, "memset", (ap, val), {}, (), writes)

    def recip(self, out, in_, reads, writes):
        self.s.op("dve", "reciprocal", (), dict(out=out, in_=in_), reads, writes)

    def build(self):
        nc, s, S, NT = self.nc, self.s, self.S, self.NT
        self.x_in = Buf("x", self.din("x", [S, D]))
        self.pos = Buf("pos", self.din("pos", [S], I32))
        self.w_in = Buf("w_in", self.din("w_in", [NLAYER, D, INTOT]))
        self.w_qup = Buf("w_qup", self.din("w_qup", [NLAYER, 256, 384]))
        self.w_kvup = Buf("w_kvup", self.din("w_kvup", [NLAYER, 128, 512]))
        self.w_br = Buf("w_br", self.din("w_br", [NLAYER, 4, 256, D]))
        self.w_out = Buf("w_out", self.din("w_out", [NLAYER, D, D]))
        self.w_fg = Buf("w_fg", self.din("w_fg", [NLAYER, D, DFF]))
        self.w_fu = Buf("w_fu", self.din("w_fu", [NLAYER, D, DFF]))
        self.w_fd = Buf("w_fd", self.din("w_fd", [NLAYER, DFF, D]))
        self.g_attn = Buf("g_attn", self.din("g_attn", [128, NLAYER, 8]))
        self.g_ffn = Buf("g_ffn", self.din("g_ffn", [128, NLAYER, 8]))
        self.g_final = Buf("g_final", self.din("g_final", [D]))
        self.g_q = Buf("g_q", self.din("g_q", [128, NLAYER, 2]))
        self.g_kv = Buf("g_kv", self.din("g_kv", [128, NLAYER]))
        self.conv_w = Buf("conv_w", self.din("conv_w", [128, NLAYER, 2, 3]))
        self.gla_wg = Buf("gla_wg", self.din("gla_wg", [NLAYER, 16, 128]))
        self.gla_bg = Buf("gla_bg", self.din("gla_bg", [NLAYER, 1, 128]))
        self.gla_ng = Buf("gla_ng", self.din("gla_ng", [NLAYER, 64]))
        cin = {k: Buf(k, self.din(k, list(v.shape))) for k, v in host_consts().items()}
        self.y_out = Buf("y", nc.dram_tensor("y", [S, D], F32, kind="ExternalOutput").ap())
        self.xres = self.dscr("xres", [S, D], F32)
        self.cosT = self.dscr("cosT", [32, S], F32)
        self.sinT = self.dscr("sinT", [32, S], F32)
        self.cqT = self.dscr("cqT", [256, S])
        self.ckvT = self.dscr("ckvT", [128, S])
        self.kropeT = self.dscr("kropeT", [32, S])
        self.cbT = self.dscr("cbT", [256, S])
        self.ccT = self.dscr("ccT", [256, S])
        self.cxT = self.dscr("cxT", [256, S])
        self.dqT = self.dscr("dqT", [128, 4, S])
        self.dkT = self.dscr("dkT", [128, S])
        self.iqT = self.dscr("iqT", [128, 8, S])
        self.ikT = self.dscr("ikT", [128, S])
        self.gqT = self.dscr("gqT", [32, 4, S])
        self.gkT = self.dscr("gkT", [32, 4, S])
        self.glrT = self.dscr("glrT", [17, S])
        self.gateT = self.dscr("gateT", [4096, S])
        self.dvaug = self.dscr("dvaug", [S, 65])
        self.iw = self.dscr("iw", [S, 8], F32)
        self.gk = self.dscr("gk", [S, 128])
        self.gv = self.dscr("gv", [S, 256])
        self.grs = self.dscr("grs", [S, 256])
        self.qT = self.dscr("qT", [4, 96, S])
        self.kT = self.dscr("kT", [4, 96, S])
        self.vaug = self.dscr("vaug", [S, 4, 65])
        self.oT = self.dscr("oT", [4, 256, S])
        self.dsc = self.dscr("dsc", [S, S], F32) if "dsc" in self.dbg else None

        with ExitStack() as top:
            self.top = top
            cst = {}
            for k in ("c_ident", "c_tri", "c_rev", "c_gmask", "c_cmt", "c_cmq"):
                cst[k] = self.sb(top, k, [128, 128], F32)
                s.dma("sp", cst[k][:], cin[k][:], reads=[cin[k]], writes=[cst[k]])
            self.identf = cst["c_ident"]
            self.tri = cst["c_tri"]
            self.rev = cst["c_rev"]
            self.identb = self.sb(top, "identb", [128, 128], BF16)
            self.gmaskb = self.sb(top, "gmaskb", [128, 128], BF16)
            self.cmtb = self.sb(top, "cmtb", [128, 128], BF16)
            self.cmqb = self.sb(top, "cmqb", [128, 128], BF16)
            self.i4b = self.sb(top, "i4b", [128, 512], BF16)
            self.onesb = self.sb(top, "onesb", [128, 128], BF16)
            self.epsb = self.sb(top, "epsb", [128, 1], F32)
            self.rotb = self.sb(top, "rotb", [32, 32], BF16)
            rotf = self.sb(top, "rotf", [32, 32], F32)
            self.invf = self.sb(top, "invf", [32, 1], F32)
            s.dma("sp", rotf[:], cin["c_rot"][:], reads=[cin["c_rot"]], writes=[rotf])
            s.dma("sp", self.invf[:], cin["c_invf"][:], reads=[cin["c_invf"]], writes=[self.invf])
            self.cp("dve", self.identb[:], self.identf[:], [self.identf], [self.identb])
            self.cp("dve", self.gmaskb[:], cst["c_gmask"][:], [cst["c_gmask"]], [self.gmaskb])
            self.cp("dve", self.cmtb[:], cst["c_cmt"][:], [cst["c_cmt"]], [self.cmtb])
            self.cp("dve", self.cmqb[:], cst["c_cmq"][:], [cst["c_cmq"]], [self.cmqb])
            self.cp("dve", self.rotb[:], rotf[:], [rotf], [self.rotb])
            for h in range(4):
                self.cp("dve", self.i4b[:, h * 128:(h + 1) * 128], self.identf[:], [self.identf], [self.i4b])
            self.ms("dve", self.onesb[:], 1.0, [self.onesb])
            self.ms("dve", self.epsb[:], EPS, [self.epsb])
            self.gattn = self.sb(top, "gattn", [128, NLAYER, 8], F32)
            self.gffn = self.sb(top, "gffn", [128, NLAYER, 8], F32)
            self.gq = self.sb(top, "gq", [128, NLAYER, 2], F32)
            self.gkv = self.sb(top, "gkv", [128, NLAYER], F32)
            self.convw = self.sb(top, "convw", [128, NLAYER, 2, 3], F32)
            for dst, srcb in ((self.gattn, self.g_attn), (self.gffn, self.g_ffn), (self.gq, self.g_q),
                              (self.gkv, self.g_kv), (self.convw, self.conv_w)):
                s.dma("sp", dst[:], srcb[:], reads=[srcb], writes=[dst])
            s.emit(top)

            allph = "RABCEDFGH"
            ph = self.phases if self.phases is not None else allph
            if "R" in ph:
                self.phase_rope()
            done = False
            for l in range(self.nl):
                for name, fn in (("A", self.phase_A), ("B", self.phase_B), ("E", self.phase_E),
                                 ("D", self.phase_D), ("F", self.phase_F), ("G", self.phase_G), ("H", self.phase_H)):
                    if name in ph:
                        fn(l)
                    if self.stop_after == (l, name):
                        done = True
                        break
                if done:
                    break
            if not done:
                self.phase_final()
            outs = [self.y_out] + [getattr(self, n) for n in self.dbg if getattr(self, n, None) is not None]
            s.wait_all("sp", outs)
            s.wait_all("pool", outs)
            s.barrier()
            s.emit(top)
        return nc

    def phase_rope(self):
        s, S = self.s, self.S
        W = min(S, 1024)
        with self.scope() as st:
            pi = self.sb(st, "pi", [32, W], I32)
            pf = self.sb(st, "pf", [32, W], F32)
            pk = self.sb(st, "pk", [32, W], I32)
            pkf = self.sb(st, "pkf", [32, W], F32)
            pc = self.sb(st, "pc", [32, W], F32)
            pa = self.sb(st, "pa", [32, W], F32)
            res = self.sb(st, "res", [32, W], F32)
            for c0 in range(0, S, W):
                s.dma("sp", pi[:], self.pos[c0:c0 + W].partition_broadcast(32), reads=[self.pos], writes=[pi])
                self.cp("dve", pf[:], pi[:], [pi], [pf])
                self.ts("dve", pf[:], pf[:], self.invf[:, 0:1], None, ALU.mult, reads=[pf, self.invf], writes=[pf])
                for shift, dst in ((0.0, self.sinT), (0.25, self.cosT)):
                    self.ts("dve", pa[:], pf[:], float(1.0 / (2 * np.pi)), shift, ALU.mult, ALU.add, reads=[pf], writes=[pa])
                    self.cp("dve", pk[:], pa[:], [pa], [pk])
                    self.cp("dve", pkf[:], pk[:], [pk], [pkf])
                    self.tt("dve", pa[:], pa[:], pkf[:], ALU.subtract, [pa, pkf], [pa])
                    self.ts("dve", pc[:], pa[:], 0.5, None, ALU.is_gt, reads=[pa], writes=[pc])
                    self.tt("dve", pa[:], pa[:], pc[:], ALU.subtract, [pa, pc], [pa])
                    self.ts("dve", pc[:], pa[:], -0.5, None, ALU.is_lt, reads=[pa], writes=[pc])
                    self.tt("dve", pa[:], pa[:], pc[:], ALU.add, [pa, pc], [pa])
                    self.act(res[:], pa[:], AF.Sin, [pa], [res], scale=float(2 * np.pi))
                    s.dma("pool", dst[:, c0:c0 + W], res[:], reads=[res], writes=[dst])

    def norm_tiles(self, st):
        return dict(
            xt=Rot([self.sb(st, "xt", [128, D], F32) for _ in range(3)]),
            xs=Rot([self.sb(st, "xs", [128, D], BF16) for _ in range(2)]),
            junk=self.sb(st, "junk", [128, D], BF16),
            ss=Rot([self.sb(st, "ss", [128, 1], F32) for _ in range(3)]),
            rs=Rot([self.sb(st, "rs", [128, 1], F32) for _ in range(3)]),
            psT=Rot([self.psb(st, "psT", BF16) for _ in range(2)]))

    def norm_to_hT(self, nt, src, g_tile, l, hT, tok0, ntiles):
        s = self.s
        for t in range(ntiles):
            xt, xs, ss, rs = nt["xt"].next(), nt["xs"].next(), nt["ss"].next(), nt["rs"].next()
            junk = nt["junk"]
            r0 = tok0 + t * 128
            s.dma("sp", xt[:], src[r0:r0 + 128, :], reads=[src], writes=[xt])
            self.act(junk[:], xt[:], AF.Square, [xt], [junk, ss], accum_out=ss[:])
            self.act(rs[:], ss[:], AF.Sqrt, [ss, self.epsb], [rs], scale=1.0 / D, bias=self.epsb[:, 0:1])
            self.recip(rs[:], rs[:], [rs], [rs])
            self.ts("dve", xs[:], xt[:], rs[:, 0:1], None, ALU.mult, reads=[xt, rs], writes=[xs])
            pT = nt["psT"].next()
            for k in range(8):
                self.tr(pT[:, k * 128:(k + 1) * 128], xs[:, k * 128:(k + 1) * 128], [xs], [pT])
            self.tt("dve", hT[:, :, t * 128:(t + 1) * 128], pT[:, :].rearrange("p (k q) -> p k q", k=8),
                    g_tile[:, l, :].unsqueeze(2).to_broadcast([128, 8, 128]), ALU.mult, [pT, g_tile], [hT])

    def phase_A(self, l):
        s, S, NT, NB = self.s, self.S, self.NT, self.NB
        src = self.x_in if l == 0 else self.xres
        with self.scope() as st:
            hT = self.sb(st, "hT", [128, 8, S], BF16)
            with self.scope() as st1:
                ones_row = self.sb(st1, "ones_row", [1, S], BF16)
                self.ms("dve", ones_row[:], 1.0, [ones_row])
                s.dma("pool", self.glrT[16:17, :], ones_row[:], reads=[ones_row], writes=[self.glrT])
                nt = self.norm_tiles(st1)
                self.norm_to_hT(nt, src, self.gattn, l, hT, 0, NT)
            ps = Rot([self.psb(st, "psA") for _ in range(4)])
            wbf_r = Rot([self.sb(st, "wbf", [128, 8, 256], BF16) for _ in range(3)])
            stg_r = Rot([self.sb(st, "stg", [128, S], BF16) for _ in range(4)])
            evac = [0]

            def load_w(c0, n):
                wbf = wbf_r.next()
                s.dma("pool", wbf[:, :, 0:n], self.w_in[l, :, c0:c0 + n].rearrange("(k p) n -> p k n", p=128),
                      reads=[self.w_in], writes=[wbf])
                return wbf

            def fm_group(wbf, n, outs, func, lo_outs=None):
                stg = stg_r.next()
                stl = stg_r.next() if lo_outs else None
                for b in range(NB):
                    p = ps.next()
                    for k in range(8):
                        self.mm(p[0:n, :], wbf[:, k, 0:n], hT[:, k, b * 512:(b + 1) * 512], k == 0, k == 7, [wbf, hT], [p])
                    evac[0] += 1
                    if lo_outs:
                        self.act(stg[0:n, b * 512:(b + 1) * 512], p[0:n, :], func, [p], [stg])
                        self.tt("dve", stl[0:n, b * 512:(b + 1) * 512], p[0:n, :], stg[0:n, b * 512:(b + 1) * 512], ALU.subtract, [p, stg], [stl])
                    elif func != AF.Copy or evac[0] % 2 == 0:
                        self.act(stg[0:n, b * 512:(b + 1) * 512], p[0:n, :], func, [p], [stg])
                    else:
                        self.cp("dve", stg[0:n, b * 512:(b + 1) * 512], p[0:n, :], [p], [stg])
                for (r0, r1, dbuf, dap) in outs:
                    s.dma("pool", dap, stg[r0:r1, :], reads=[stg], writes=[dbuf])
                for (r0, r1, dbuf, dap) in (lo_outs or []):
                    s.dma("pool", dap, stl[r0:r1, :], reads=[stl], writes=[dbuf])

            fm = []
            for g in range(2):
                fm.append((C_CQ + 128 * g, 128, [(0, 128, self.cqT, self.cqT[128 * g:128 * (g + 1), :])], AF.Copy))
            fm.append((C_CKV, 128, [(0, 128, self.ckvT, self.ckvT[:, :])], AF.Copy))
            fm.append((C_KROPE, 32, [(0, 32, self.kropeT, self.kropeT[:, :])], AF.Copy))
            for c0, dst in ((C_CB, self.cbT), (C_CC, self.ccT), (C_CX, self.cxT)):
                for g in range(2):
                    fm.append((c0 + 128 * g, 128, [(0, 128, dst, dst[128 * g:128 * (g + 1), :])], AF.Copy))
            for g in range(2):
                fm.append((C_DQ + 128 * g, 128, [(64 * j, 64 * (j + 1), self.dqT, self.dqT[64 * r:64 * (r + 1), 2 * g + j, :]) for j in range(2) for r in range(2)], AF.Copy))
            fm.append((C_DK, 64, [(0, 64, self.dkT, self.dkT[0:64, :])], AF.Copy, [(0, 64, self.dkT, self.dkT[64:128, :])]))
            for g in range(2):
                fm.append((C_IQ + 128 * g, 128, [(32 * j, 32 * (j + 1), self.iqT, self.iqT[32 * r:32 * (r + 1), 4 * g + j, :]) for j in range(4) for r in (0, 1)], AF.Copy,
                           [(32 * j, 32 * (j + 1), self.iqT, self.iqT[32 * r:32 * (r + 1), 4 * g + j, :]) for j in range(4) for r in (2, 3)]))
            fm.append((C_IK, 32, [(0, 32, self.ikT, self.ikT[32 * r:32 * (r + 1), :]) for r in (0, 2)], AF.Copy,
                       [(0, 32, self.ikT, self.ikT[32 * r:32 * (r + 1), :]) for r in (1, 3)]))
            fm.append((C_GQ, 128, [(32 * j, 32 * (j + 1), self.gqT, self.gqT[:, j, :]) for j in range(4)], AF.Copy))
            fm.append((C_GK, 128, [(32 * j, 32 * (j + 1), self.gkT, self.gkT[:, j, :]) for j in range(4)], AF.Copy))
            fm.append((C_GLR, 16, [(0, 16, self.glrT, self.glrT[0:16, :])], AF.Copy))
            for g in range(32):
                fm.append((C_GATE + 128 * g, 128, [(0, 128, self.gateT, self.gateT[128 * g:128 * (g + 1), :])], AF.Sigmoid))
            nxt = load_w(fm[0][0], fm[0][1])
            for i, ent in enumerate(fm):
                c0, n, outs, func = ent[:4]
                wbf = nxt
                if i + 1 < len(fm):
                    nxt = load_w(fm[i + 1][0], fm[i + 1][1])
                fm_group(wbf, n, outs, func, ent[4] if len(ent) > 4 else None)

            tms = [(C_DV, 64, self.dvaug, BF16, AF.Copy, True), (C_IW, 8, self.iw, F32, AF.Copy, False),
                   (C_GK, 128, self.gk, BF16, AF.Copy, False), (C_GV, 256, self.gv, BF16, AF.Copy, False),
                   (C_GR, 256, self.grs, BF16, AF.Silu, False)]
            for (c0, n, dst, dt, func, ones_col) in tms:
                wbf = load_w(c0, n)
                nn = n + (1 if ones_col else 0)
                tstg = self.sb(st, "tstg", [128, NT, nn], dt)
                if ones_col:
                    self.ms("dve", tstg[:, :, n:n + 1], 1.0, [tstg])
                for t in range(NT):
                    p = ps.next()
                    for k in range(8):
                        self.mm(p[:, 0:n], hT[:, k, t * 128:(t + 1) * 128], wbf[:, k, 0:n], k == 0, k == 7, [wbf, hT], [p])
                    self.act(tstg[:, t, 0:n], p[:, 0:n], func, [p], [tstg])
                s.dma("pool", dst[:, :].rearrange("(t p) n -> p t n", p=128), tstg[:], reads=[tstg], writes=[dst])

    def phase_B(self, l):
        s, S, NT, NB = self.s, self.S, self.NT, self.NB
        with self.scope() as st:
            wqf = self.sb(st, "wqf", [128, 2, 384], F32)
            wqb = self.sb(st, "wqb", [128, 2, 384], BF16)
            wkf = self.sb(st, "wkf", [128, 512], F32)
            wkb = self.sb(st, "wkb", [128, 512], BF16)
            s.dma("sp", wqf[:], self.w_qup[l].rearrange("(k p) n -> p k n", p=128), reads=[self.w_qup], writes=[wqf])
            s.dma("sp", wkf[:], self.w_kvup[l], reads=[self.w_kvup], writes=[wkf])
            for k in range(2):
                self.ts("dve", wqb[:, k, :], wqf[:, k, :], self.gq[:, l, k:k + 1], None, ALU.mult, reads=[wqf, self.gq], writes=[wqb])
            self.ts("dve", wkb[:], wkf[:], self.gkv[:, l:l + 1], None, ALU.mult, reads=[wkf, self.gkv], writes=[wkb])
            ps = Rot([self.psb(st, "psB") for _ in range(6)])
            cq_r = Rot([self.sb(st, "cqb", [128, 2, 512], BF16) for _ in range(2)])
            ckv_r = Rot([self.sb(st, "ckvb", [128, 512], BF16) for _ in range(2)])
            kr_r = Rot([self.sb(st, "krb", [32, 512], BF16) for _ in range(2)])
            cos_r = Rot([self.sb(st, "cosb", [32, 512], F32) for _ in range(2)])
            sin_r = Rot([self.sb(st, "sinb", [32, 512], F32) for _ in range(2)])
            sq_r = Rot([self.sb(st, "sqb", [128, 2, 512], BF16) for _ in range(2)])
            sqk_r = Rot([self.sb(st, "sqk", [128, 512], BF16) for _ in range(2)])
            rq_r = Rot([self.sb(st, "rq", [128, 512], F32) for _ in range(2)])
            rk_r = Rot([self.sb(st, "rk", [128, 512], F32) for _ in range(2)])
            rkt_r = Rot([self.sb(st, "rkt", [128, 4], F32) for _ in range(2)])
            qo_r = Rot([self.sb(st, "qo", [64, 512], BF16) for _ in range(3)])
            xr_r = Rot([self.sb(st, "xr", [32, 512], BF16) for _ in range(3)])
            t1_r = Rot([self.sb(st, "t1", [32, 512], F32) for _ in range(3)])
            t2_r = Rot([self.sb(st, "t2", [32, 512], F32) for _ in range(3)])
            ro_r = Rot([self.sb(st, "ro", [32, 512], BF16) for _ in range(3)])
            vst_r = Rot([self.sb(st, "vst", [128, 4, 4, 65], BF16) for _ in range(2)])
            for v in vst_r.bufs:
                self.ms("dve", v[:, :, :, 64:65], 1.0, [v])

            def rope(xr, cosb, sinb, dsts):
                p = ps.next()
                self.mm(p[0:32, :], self.rotb[:], xr[:], True, True, [self.rotb, xr], [p])
                t1, t2, ro = t1_r.next(), t2_r.next(), ro_r.next()
                self.tt("pool", t1[:], xr[:], cosb[:], ALU.mult, [xr, cosb], [t1])
                self.tt("dve", t2[:], p[0:32, :], sinb[:], ALU.mult, [p, sinb], [t2])
                self.tt("dve", ro[:], t1[:], t2[:], ALU.add, [t1, t2], [ro])
                for dbuf, dap in dsts:
                    s.dma("pool", dap, ro[:], reads=[ro], writes=[dbuf])

            for b in range(NB):
                c0, c1 = b * 512, (b + 1) * 512
                cq, ckv, kr, cosb, sinb = cq_r.next(), ckv_r.next(), kr_r.next(), cos_r.next(), sin_r.next()
                s.dma("sp", cq[:], self.cqT[:, c0:c1].rearrange("(k p) t -> p k t", p=128), reads=[self.cqT], writes=[cq])
                s.dma("sp", ckv[:], self.ckvT[:, c0:c1], reads=[self.ckvT], writes=[ckv])
                s.dma("sp", kr[:], self.kropeT[:, c0:c1], reads=[self.kropeT], writes=[kr])
                s.dma("sp", cosb[:], self.cosT[:, c0:c1], reads=[self.cosT], writes=[cosb])
                s.dma("sp", sinb[:], self.sinT[:, c0:c1], reads=[self.sinT], writes=[sinb])
                sq, sqk, rq, rk, rkt = sq_r.next(), sqk_r.next(), rq_r.next(), rk_r.next(), rkt_r.next()
                self.tt("pool", sq[:], cq[:], cq[:], ALU.mult, [cq], [sq])
                p = ps.next()
                for k in range(2):
                    self.mm(p[:, :], self.onesb[:], sq[:, k, :], k == 0, k == 1, [self.onesb, sq], [p])
                self.act(rq[:], p[:, :], AF.Sqrt, [p, self.epsb], [rq], scale=1.0 / 256, bias=self.epsb[:, 0:1])
                self.recip(rq[:], rq[:], [rq], [rq])
                self.tt("pool", sqk[:], ckv[:], ckv[:], ALU.mult, [ckv], [sqk])
                p2 = ps.next()
                self.mm(p2[:, :], self.onesb[:], sqk[:], True, True, [self.onesb, sqk], [p2])
                self.act(rk[:], p2[:, :], AF.Sqrt, [p2, self.epsb], [rk], scale=1.0 / 128, bias=self.epsb[:, 0:1])
                self.recip(rk[:], rk[:], [rk], [rk])
                p3 = ps.next()
                for tt_ in range(4):
                    self.mm(p3[:, tt_:tt_ + 1], sqk[:, tt_ * 128:(tt_ + 1) * 128], self.onesb[:, 0:1], True, True, [self.onesb, sqk], [p3])
                self.act(rkt[:], p3[:, 0:4], AF.Sqrt, [p3, self.epsb], [rkt], scale=1.0 / 128, bias=self.epsb[:, 0:1])
                self.recip(rkt[:], rkt[:], [rkt], [rkt])
                for h in range(4):
                    pn = ps.next()
                    for k in range(2):
                        self.mm(pn[0:64, :], wqb[:, k, h * 96:h * 96 + 64], cq[:, k, :], k == 0, k == 1, [wqb, cq], [pn])
                    qo = qo_r.next()
                    self.tt("dve", qo[:], pn[0:64, :], rq[0:64, :], ALU.mult, [pn, rq], [qo])
                    s.dma("pool", self.qT[h, 0:64, c0:c1], qo[:], reads=[qo], writes=[self.qT])
                    pr = ps.next()
                    for k in range(2):
                        self.mm(pr[0:32, :], wqb[:, k, h * 96 + 64:h * 96 + 96], cq[:, k, :], k == 0, k == 1, [wqb, cq], [pr])
                    xr = xr_r.next()
                    self.tt("dve", xr[:], pr[0:32, :], rq[0:32, :], ALU.mult, [pr, rq], [xr])
                    rope(xr, cosb, sinb, [(self.qT, self.qT[h, 64:96, c0:c1])])
                for h in range(4):
                    pn = ps.next()
                    self.mm(pn[0:64, :], wkb[:, h * 128:h * 128 + 64], ckv[:], True, True, [wkb, ckv], [pn])
                    qo = qo_r.next()
                    self.tt("dve", qo[:], pn[0:64, :], rk[0:64, :], ALU.mult, [pn, rk], [qo])
                    s.dma("pool", self.kT[h, 0:64, c0:c1], qo[:], reads=[qo], writes=[self.kT])
                rope(kr, cosb, sinb, [(self.kT, self.kT[h, 64:96, c0:c1]) for h in range(4)])
                vst = vst_r.next()
                for tt_ in range(4):
                    pv = ps.next()
                    self.mm(pv[:, :], ckv[:, tt_ * 128:(tt_ + 1) * 128], wkb[:], True, True, [wkb, ckv], [pv])
                    self.act(vst[:, tt_, :, 0:64], pv[:, :].rearrange("p (h c) -> p h c", h=4)[:, :, 64:128], AF.Copy, [pv, rkt], [vst],
                             scale=rkt[:, tt_:tt_ + 1])
                s.dma("pool", self.vaug[c0:c1, :, :].rearrange("(t p) h c -> p t h c", p=128), vst[:], reads=[vst], writes=[self.vaug])

    def attn_finish(self, oacc, obuf_r, oT_r, psT_r, branch, t):
        rec = self.rec_r.next()
        o = obuf_r.next()
        acc3 = oacc[:, 0:260].rearrange("p (h c) -> p h c", h=4)
        self.recip(rec[:, :], acc3[:, :, 64], [oacc], [rec])
        self.tt("dve", o[:].rearrange("p (h c) -> p h c", h=4), acc3[:, :, 0:64],
                rec[:, :].unsqueeze(2).to_broadcast([128, 4, 64]), ALU.mult, [oacc, rec], [o])
        self.to_oT(o, oT_r, psT_r, branch, t)

    def to_oT(self, o, oT_r, psT_r, branch, t):
        s = self.s
        pT = psT_r.next()
        for k in range(2):
            self.tr(pT[:, k * 128:(k + 1) * 128], o[:, k * 128:(k + 1) * 128], [o], [pT])
        oTt = oT_r.next()
        self.act(oTt[:].rearrange("p k q -> p (k q)"), pT[:, 0:256], AF.Copy, [pT], [oTt])
        s.dma("pool", self.oT[branch, :, t * 128:(t + 1) * 128].rearrange("(k p) q -> p k q", p=128), oTt[:], reads=[oTt], writes=[self.oT])

    def phase_C(self, l):
        s, S, NT = self.s, self.S, self.NT
        scale = float(96 ** -0.5)
        with self.scope() as st:
            kT = self.sb(st, "kTs", [96, 4, S], BF16)
            qT = self.sb(st, "qTs", [96, 4, S], BF16)
            va = self.sb(st, "vas", [128, NT, 4, 65], BF16)
            for h in range(4):
                s.dma("sp", kT[:, h, :], self.kT[h], reads=[self.kT], writes=[kT])
                s.dma("sp", qT[:, h, :], self.qT[h], reads=[self.qT], writes=[qT])
            s.dma("sp", va[:], self.vaug[:, :, :].rearrange("(t p) h c -> p t h c", p=128), reads=[self.vaug], writes=[va])
            ps_st = Rot([self.psb(st, "psS") for _ in range(3)])
            ps_o = Rot([self.psb(st, "psO") for _ in range(2)])
            psT_r = Rot([self.psb(st, "psT", BF16) for _ in range(1)])
            pT_r = Rot([self.sb(st, "pT", [128, 512], BF16) for _ in range(3)])
            self.rec_r = Rot([self.sb(st, "rec", [128, 4], F32) for _ in range(2)])
            obuf_r = Rot([self.sb(st, "ob", [128, 256], BF16) for _ in range(2)])
            oT_r = Rot([self.sb(st, "oTt", [128, 2, 128], BF16) for _ in range(2)])
            for t in range(NT):
                oacc = ps_o.next()
                q0, q1 = t * 128, (t + 1) * 128
                def pv(j, pT):
                    for h in range(4):
                        self.mm(oacc[:, h * 65:(h + 1) * 65], pT[:, h * 128:(h + 1) * 128], va[:, j, h, :], (j == 0 and h == 0), j == t, [pT, va], [oacc])

                prev = None
                for j in range(t + 1):
                    k0, k1 = j * 128, (j + 1) * 128
                    sp = ps_st.next()
                    for h in range(4):
                        self.mm(sp[:, h * 128:(h + 1) * 128], kT[:, h, k0:k1], qT[:, h, q0:q1], h == 0, j != t, [kT, qT], [sp])
                    if j == t:
                        for h in range(4):
                            self.mm(sp[:, h * 128:(h + 1) * 128], self.identb[:], self.cmtb[:], False, True, [self.identb, self.cmtb], [sp])
                    pT = pT_r.next()
                    self.act(pT[:], sp[:, :], AF.Exp, [sp], [pT], scale=scale)
                    if prev is not None:
                        pv(*prev)
                    prev = (j, pT)
                pv(*prev)
                self.attn_finish(oacc, obuf_r, oT_r, psT_r, 0, t)

    def phase_E(self, l):
        s, S = self.s, self.S
        cw = self.convw
        with self.scope() as st:
            cb = self.sb(st, "cb", [128, S], BF16)
            cc = self.sb(st, "cc", [128, S], BF16)
            cx = self.sb(st, "cx", [128, S], BF16)
            u = self.sb(st, "u", [128, S + 2], F32)
            y = self.sb(st, "y", [128, S], F32)
            o = self.sb(st, "o", [128, S], BF16)
            for c in range(2):
                r0, r1 = c * 128, (c + 1) * 128
                s.dma("sp", cb[:], self.cbT[r0:r1, :], reads=[self.cbT], writes=[cb])
                s.dma("sp", cc[:], self.ccT[r0:r1, :], reads=[self.ccT], writes=[cc])
                s.dma("sp", cx[:], self.cxT[r0:r1, :], reads=[self.cxT], writes=[cx])
                self.ms("pool", u[:, 0:2], 0.0, [u])
                self.tt("pool", u[:, 2:S + 2], cc[:], cx[:], ALU.mult, [cc, cx], [u])
                self.ts("dve", y[:], u[:, 2:S + 2], cw[:, l, c, 2:3], None, ALU.mult, reads=[u, cw], writes=[y])
                self.stt(y[:], u[:, 1:S + 1], cw[:, l, c, 1:2], y[:], ALU.mult, ALU.add, [u, cw, y], [y])
                self.stt(y[:], u[:, 0:S], cw[:, l, c, 0:1], y[:], ALU.mult, ALU.add, [u, cw, y], [y])
                self.tt("pool", o[:], cb[:], y[:], ALU.mult, [cb, y], [o])
                s.dma("pool", self.oT[1, r0:r1, :], o[:], reads=[o], writes=[self.oT])

    def phase_D(self, l):
        s, S, NT = self.s, self.S, self.NT
        cidx = float(32 ** -0.5 * 8 ** -0.5)
        scale = float(64 ** -0.5)
        mscale = float(96 ** -0.5)
        with self.scope() as st:
            ikT = self.sb(st, "ikTs", [128, S], BF16)
            dkT = self.sb(st, "dkTs", [128, S], BF16)
            dva = self.sb(st, "dvas", [128, NT, 65], BF16)
            iw = self.sb(st, "iws", [128, NT, 8], F32)
            kT = self.sb(st, "kTs", [96, 4, S], BF16)
            va = self.sb(st, "vas", [128, NT, 4, 65], BF16)
            s.dma("sp", ikT[:], self.ikT[:, :], reads=[self.ikT], writes=[ikT])
            s.dma("sp", dkT[:], self.dkT[:, :], reads=[self.dkT], writes=[dkT])
            s.dma("sp", dva[:], self.dvaug[:, :].rearrange("(t p) c -> p t c", p=128), reads=[self.dvaug], writes=[dva])
            s.dma("sp", iw[:], self.iw[:, :].rearrange("(t p) c -> p t c", p=128), reads=[self.iw], writes=[iw])
            for h in range(4):
                s.dma("sp", kT[:, h, :], self.kT[h], reads=[self.kT], writes=[kT])
            s.dma("sp", va[:], self.vaug[:, :, :].rearrange("(t p) h c -> p t h c", p=128), reads=[self.vaug], writes=[va])
            iq_r = Rot([self.sb(st, "iqt", [128, 8, 128], BF16) for _ in range(3)])
            dq_r = Rot([self.sb(st, "dqt", [128, 4, 128], BF16) for _ in range(4)])
            mq_r = Rot([self.sb(st, "mqt", [96, 4, 128], BF16) for _ in range(3)])
            sc_r = [self.sb(st, "sc", [128, S], F32) for _ in range(2)]
            mb_r = [self.sb(st, "mb", [128, S], BF16) for _ in range(2)]
            junk = self.sb(st, "junkd", [128, S], BF16)
            ps_rel = Rot([self.psb(st, "psR") for _ in range(2)])
            ps_sc = Rot([self.psb(st, "psSc") for _ in range(1)])
            ps_st = Rot([self.psb(st, "psS") for _ in range(2)])
            ps_od = Rot([self.psb(st, "psOd") for _ in range(1)])
            ps_om = Rot([self.psb(st, "psOm") for _ in range(1)])
            psT_r = Rot([self.psb(st, "psT", BF16) for _ in range(1)])
            rl_r = Rot([self.sb(st, "rl", [128, 512], BF16) for _ in range(4)])
            dm_r = Rot([self.sb(st, "dm", [128, 8, 128], BF16) for _ in range(2)])
            pT_r = Rot([self.sb(st, "pT", [128, 512], BF16) for _ in range(4)])
            self.rec_r = Rot([self.sb(st, "rec", [128, 4], F32) for _ in range(2)])
            obuf_r = Rot([self.sb(st, "ob", [128, 256], BF16) for _ in range(2)])
            oT_r = Rot([self.sb(st, "oTt", [128, 2, 128], BF16) for _ in range(2)])
            sm = {k: self.sb(st, k, [128, 1], F32) for k in ("mx", "w0", "lo", "mid", "cnt", "tmp", "thr")}
            dq_of = {}

            def score(t):
                q0, q1 = t * 128, (t + 1) * 128
                nk = (t + 1) * 128
                sc = sc_r[t % 2]
                iq = iq_r.next()
                dq = dq_r.next()
                dq_of[t] = dq
                s.dma("sp", iq[:], self.iqT[:, :, q0:q1], reads=[self.iqT], writes=[iq])
                s.dma("sp", dq[:], self.dqT[:, :, q0:q1], reads=[self.dqT], writes=[dq])
                dm = dm_r.next()
                for h in range(8):
                    self.ts("pool", dm[:, h, :], self.identf[:], iw[:, t, h:h + 1], cidx, ALU.mult, ALU.mult, reads=[self.identf, iw], writes=[dm])
                nb = (nk + 511) // 512
                for b in range(nb):
                    k0 = b * 512
                    w = min(512, nk - k0)
                    psc = ps_sc.next()
                    diag = (b == nb - 1)
                    prev = None
                    for h in range(8):
                        pr = ps_rel.next()
                        self.mm(pr[:, 0:w], iq[:, h, :], ikT[:, k0:k0 + w], True, True, [iq, ikT], [pr])
                        rl = rl_r.next()
                        self.act(rl[:, 0:w], pr[:, 0:w], AF.Relu, [pr], [rl])
                        if prev is not None:
                            hh, prl = prev
                            self.mm(psc[:, 0:w], dm[:, hh, :], prl[:, 0:w], hh == 0, False, [dm, prl], [psc])
                        prev = (h, rl)
                    hh, prl = prev
                    self.mm(psc[:, 0:w], dm[:, hh, :], prl[:, 0:w], False, not diag, [dm, prl], [psc])
                    if diag:
                        self.mm(psc[:, w - 128:w], self.identb[:], self.cmqb[:], False, True, [self.identb, self.cmqb], [psc])
                    self.act(sc[:, k0:k0 + w], psc[:, 0:w], AF.Copy, [psc], [sc])
                if self.dsc is not None:
                    s.dma("pool", self.dsc[q0:q1, 0:nk], sc[:, 0:nk], reads=[sc], writes=[self.dsc])

            def bisect(t):
                nk = (t + 1) * 128
                sc, mb = sc_r[t % 2], mb_r[t % 2]
                thr = sm["thr"]
                if t < 2:
                    self.ms("dve", thr[:], -10000.0, [thr])
                else:
                    mx, w0, lo, mid, cnt, tmp = (sm[k] for k in ("mx", "w0", "lo", "mid", "cnt", "tmp"))
                    s.op("dve", "tensor_reduce", (), dict(out=mx[:], in_=sc[:, 0:nk], axis=AX.X, op=ALU.max), [sc], [mx])
                    s.op("dve", "tensor_reduce", (), dict(out=lo[:], in_=sc[:, 0:nk - 128], axis=AX.X, op=ALU.min), [sc], [lo])
                    self.tt("dve", w0[:], mx[:], lo[:], ALU.subtract, [mx, lo], [w0])
                    self.ts("dve", w0[:], w0[:], 1.001, 1e-6, ALU.mult, ALU.add, reads=[w0], writes=[w0])
                    for it in range(NBIS):
                        f = float(2.0 ** -(it + 1))
                        self.stt(mid[:], w0[:], f, lo[:], ALU.mult, ALU.add, [w0, lo], [mid])
                        self.ts("dve", junk[:, 0:nk], sc[:, 0:nk], mid[:, 0:1], None, ALU.is_ge, ALU.add, reads=[sc, mid], writes=[junk, cnt], accum_out=cnt[:])
                        self.ts("dve", tmp[:], cnt[:], 255.5, f, ALU.is_ge, ALU.mult, reads=[cnt], writes=[tmp])
                        self.stt(lo[:], tmp[:], w0[:, 0:1], lo[:], ALU.mult, ALU.add, [tmp, w0, lo], [lo])
                    self.cp("dve", thr[:], lo[:], [lo], [thr])
                self.ts("dve", mb[:, 0:nk], sc[:, 0:nk], thr[:, 0:1], -30000.0, ALU.is_lt, ALU.mult, reads=[sc, thr], writes=[mb])

            def attend(t):
                mb = mb_r[t % 2]
                dq = dq_of.pop(t)
                oacc = ps_od.next()

                def pv(j, pT):
                    for h in range(4):
                        self.mm(oacc[:, h * 65:(h + 1) * 65], pT[:, h * 128:(h + 1) * 128], dva[:, j, :], (j == 0 and h == 0), j == t, [pT, dva], [oacc])

                prev = None
                for j in range(t + 1):
                    k0, k1 = j * 128, (j + 1) * 128
                    sp = ps_st.next()
                    self.mm(sp[:, :], dkT[:, k0:k1], dq[:, :, :].rearrange("d h q -> d (h q)"), True, False, [dkT, dq], [sp])
                    self.mm(sp[:, :], mb[:, k0:k1], self.i4b[:], False, True, [mb, self.i4b], [sp])
                    pT = pT_r.next()
                    self.act(pT[:], sp[:, :], AF.Exp, [sp], [pT], scale=scale)
                    if prev is not None:
                        pv(*prev)
                    prev = (j, pT)
                pv(*prev)
                self.attn_finish(oacc, obuf_r, oT_r, psT_r, 2, t)

            def attend_mla(t):
                q0, q1 = t * 128, (t + 1) * 128
                mq = mq_r.next()
                s.dma("sp", mq[:], self.qT[:, :, q0:q1].rearrange("h d q -> d h q"), reads=[self.qT], writes=[mq])
                oacc = ps_om.next()

                def pv(j, pT):
                    for h in range(4):
                        self.mm(oacc[:, h * 65:(h + 1) * 65], pT[:, h * 128:(h + 1) * 128], va[:, j, h, :], (j == 0 and h == 0), j == t, [pT, va], [oacc])

                prev = None
                for j in range(t + 1):
                    k0, k1 = j * 128, (j + 1) * 128
                    sp = ps_st.next()
                    for h in range(4):
                        self.mm(sp[:, h * 128:(h + 1) * 128], kT[:, h, k0:k1], mq[:, h, :], h == 0, j != t, [kT, mq], [sp])
                    if j == t:
                        for h in range(4):
                            self.mm(sp[:, h * 128:(h + 1) * 128], self.identb[:], self.cmtb[:], False, True, [self.identb, self.cmtb], [sp])
                    pT = pT_r.next()
                    self.act(pT[:], sp[:, :], AF.Exp, [sp], [pT], scale=mscale)
                    if prev is not None:
                        pv(*prev)
                    prev = (j, pT)
                pv(*prev)
                self.attn_finish(oacc, obuf_r, oT_r, psT_r, 0, t)

            score(0)
            if NT > 1:
                score(1)
            bisect(0)
            for t in range(NT):
                if t + 2 < NT:
                    score(t + 2)
                if t + 1 < NT:
                    bisect(t + 1)
                attend(t)
                attend_mla(t)

    def phase_F(self, l):
        s, S, NT = self.s, self.S, self.NT
        with self.scope() as st:
            gqT = self.sb(st, "gqTs", [32, 4, S], BF16)
            gkT = self.sb(st, "gkTs", [32, 4, S], BF16)
            gk = self.sb(st, "gks", [128, NT, 128], BF16)
            gv = self.sb(st, "gvs", [128, NT, 256], BF16)
            grs = self.sb(st, "grss", [128, NT, 256], BF16)
            glrT = self.sb(st, "glrTs", [17, S], BF16)
            wgf = self.sb(st, "wgf", [17, 128], F32)
            wgb = self.sb(st, "wgb", [17, 128], BF16)
            ngb = self.sb(st, "ngb", [128, 64], F32)
            s.dma("sp", gqT[:], self.gqT[:, :, :], reads=[self.gqT], writes=[gqT])
            s.dma("sp", gkT[:], self.gkT[:, :, :], reads=[self.gkT], writes=[gkT])
            s.dma("sp", gk[:], self.gk[:, :].rearrange("(t p) c -> p t c", p=128), reads=[self.gk], writes=[gk])
            s.dma("sp", gv[:], self.gv[:, :].rearrange("(t p) c -> p t c", p=128), reads=[self.gv], writes=[gv])
            s.dma("sp", grs[:], self.grs[:, :].rearrange("(t p) c -> p t c", p=128), reads=[self.grs], writes=[grs])
            s.dma("sp", glrT[:], self.glrT[:, :], reads=[self.glrT], writes=[glrT])
            s.dma("sp", wgf[0:16, :], self.gla_wg[l], reads=[self.gla_wg], writes=[wgf])
            s.dma("sp", wgf[16:17, :], self.gla_bg[l], reads=[self.gla_bg], writes=[wgf])
            s.dma("sp", ngb[:], self.gla_ng[l].partition_broadcast(128), reads=[self.gla_ng], writes=[ngb])
            self.cp("dve", wgb[:], wgf[:], [wgf], [wgb])
            state = self.sb(st, "state", [32, 4, 64], F32)
            stateb = self.sb(st, "stateb", [32, 4, 64], BF16)
            self.ms("dve", state[:], 0.0, [state])
            self.ms("dve", stateb[:], 0.0, [stateb])
            ps_z = Rot([self.psb(st, "psZ") for _ in range(1)])
            ps_c = Rot([self.psb(st, "psC") for _ in range(2)])
            ps_a = Rot([self.psb(st, "psA") for _ in range(1)])
            ps_o = Rot([self.psb(st, "psO") for _ in range(2)])
            ps_u = Rot([self.psb(st, "psU") for _ in range(1)])
            psT_r = Rot([self.psb(st, "psT", BF16) for _ in range(1)])
            ez_r = Rot([self.sb(st, "ez", [128, 128], F32) for _ in range(2)])
            sp_r = Rot([self.sb(st, "sp", [128, 128], F32) for _ in range(2)])
            ec_r = Rot([self.sb(st, "ec", [32, 4, 128], F32) for _ in range(2)])
            en_r = Rot([self.sb(st, "en", [32, 4, 128], F32) for _ in range(2)])
            er_r = Rot([self.sb(st, "er", [128, 128], F32) for _ in range(2)])
            qt_r = Rot([self.sb(st, "qt", [32, 4, 128], BF16) for _ in range(2)])
            kt_r = Rot([self.sb(st, "kt", [32, 4, 128], BF16) for _ in range(2)])
            kh_r = Rot([self.sb(st, "kh", [128, 128], BF16) for _ in range(2)])
            am_r = Rot([self.sb(st, "am", [128, 4, 128], BF16) for _ in range(2)])
            on_r = Rot([self.sb(st, "on", [128, 4, 64], F32) for _ in range(2)])
            og_r = Rot([self.sb(st, "og", [128, 256], BF16) for _ in range(2)])
            ss_r = Rot([self.sb(st, "ssg", [128, 4], F32) for _ in range(2)])
            junk = self.sb(st, "junkg", [128, 64], F32)
            oT_r = Rot([self.sb(st, "oTt", [128, 2, 128], BF16) for _ in range(2)])
            qscale = float(32 ** -0.5)
            def pre(t):
                c0, c1 = t * 128, (t + 1) * 128
                pz = ps_z.next()
                self.mm(pz[:, 0:128], glrT[:, c0:c1], wgb[:], True, True, [glrT, wgb], [pz])
                ez, sp = ez_r.next(), sp_r.next()
                self.act(ez[:], pz[:, 0:128], AF.Exp, [pz], [ez], scale=-1.0)
                self.act(sp[:], ez[:], AF.Ln, [ez], [sp], bias=1.0)
                pc = ps_c.next()
                for h in range(4):
                    self.mm(pc[0:32, h * 128:(h + 1) * 128], sp[:, h * 32:(h + 1) * 32], self.tri[:], True, True, [sp, self.tri], [pc])
                prv = ps_c.next()
                self.mm(prv[:, 0:128], self.rev[:], sp[:], True, True, [sp, self.rev], [prv])
                ec, en, er = ec_r.next(), en_r.next(), er_r.next()
                self.act(ec[:].rearrange("p h i -> p (h i)"), pc[0:32, :], AF.Exp, [pc], [ec])
                self.act(en[:].rearrange("p h i -> p (h i)"), pc[0:32, :], AF.Exp, [pc], [en], scale=-1.0)
                self.act(er[:], prv[:, 0:128], AF.Exp, [prv], [er])
                qt, kt, kh = qt_r.next(), kt_r.next(), kh_r.next()
                self.stt(qt[:], gqT[:, :, c0:c1], qscale, ec[:], ALU.mult, ALU.mult, [gqT, ec], [qt])
                self.tt("pool", kt[:], gkT[:, :, c0:c1], en[:], ALU.mult, [gkT, en], [kt])
                self.tt("pool", kh[:], gk[:, t, :], er[:], ALU.mult, [gk, er], [kh])
                pa = ps_a.next()
                for h in range(4):
                    self.mm(pa[:, h * 128:(h + 1) * 128], kt[:, h, :], qt[:, h, :], True, True, [kt, qt], [pa])
                am = am_r.next()
                self.tt("dve", am[:], pa[:, :].rearrange("p (h i) -> p h i", h=4), self.gmaskb[:].unsqueeze(1).to_broadcast([128, 4, 128]),
                        ALU.mult, [pa, self.gmaskb], [am])
                return ec, qt, kh, am

            def rec(t, ec, qt, kh, am):
                po = ps_o.next()
                for h in range(4):
                    self.mm(po[:, h * 64:(h + 1) * 64], am[:, h, :], gv[:, t, h * 64:(h + 1) * 64], h == 0, False, [am, gv], [po])
                for c in range(2):
                    i0, i1 = c * 64, (c + 1) * 64
                    for h in range(4):
                        self.mm(po[i0:i1, h * 64:(h + 1) * 64], qt[:, h, i0:i1], stateb[:, h, :], False, True, [qt, stateb], [po])
                    pu = ps_u.next()
                    for h in range(4):
                        self.mm(pu[0:32, h * 64:(h + 1) * 64], kh[i0:i1, h * 32:(h + 1) * 32], gv[i0:i1, t, h * 64:(h + 1) * 64], True, True, [kh, gv], [pu])
                    self.tt("dve", state[:], state[:], ec[:, :, i1 - 1:i1].to_broadcast([32, 4, 64]), ALU.mult, [state, ec], [state])
                    self.tt("dve", state[:], state[:], pu[0:32, 0:256].rearrange("p (h v) -> p h v", h=4), ALU.add, [state, pu], [state])
                    self.cp("dve", stateb[:], state[:], [state], [stateb])
                ss, on, og = ss_r.next(), on_r.next(), og_r.next()
                for h in range(4):
                    self.act(junk[:], po[:, h * 64:(h + 1) * 64], AF.Square, [po], [junk, ss], accum_out=ss[:, h:h + 1])
                self.act(ss[:], ss[:], AF.Sqrt, [ss, self.epsb], [ss], scale=1.0 / 64, bias=self.epsb[:, 0:1])
                self.recip(ss[:], ss[:], [ss], [ss])
                self.tt("dve", on[:], po[:, 0:256].rearrange("p (h v) -> p h v", h=4), ss[:].unsqueeze(2).to_broadcast([128, 4, 64]),
                        ALU.mult, [po, ss], [on])
                self.tt("pool", on[:], on[:], ngb[:].unsqueeze(1).to_broadcast([128, 4, 64]), ALU.mult, [on, ngb], [on])
                self.tt("pool", og[:], on[:].rearrange("p h v -> p (h v)"), grs[:, t, :], ALU.mult, [on, grs], [og])
                self.to_oT(og, oT_r, psT_r, 3, t)

            nxt = pre(0)
            for t in range(NT):
                cur = nxt
                if t + 1 < NT:
                    nxt = pre(t + 1)
                rec(t, *cur)

    def phase_G(self, l):
        s, S, NT, NB = self.s, self.S, self.NT, self.NB
        src = self.x_in if l == 0 else self.xres
        with self.scope() as st:
            wbb = self.sb(st, "wbb", [128, 4, 2, D], BF16)
            wob = self.sb(st, "wob", [128, 8, D], BF16)
            for n in range(4):
                s.dma("pool", wbb[:, n, :, :], self.w_br[l, n].rearrange("(k p) c -> p k c", p=128), reads=[self.w_br], writes=[wbb])
            for kk in range(4):
                s.dma("pool", wob[:, 2 * kk:2 * kk + 2, :], self.w_out[l, kk * 256:(kk + 1) * 256, :].rearrange("(k p) c -> p k c", p=128),
                      reads=[self.w_out], writes=[wob])
            ps = Rot([self.psb(st, "psG") for _ in range(4)])
            ob_r = Rot([self.sb(st, "obk", [128, 4, 2, 512], BF16) for _ in range(2)])
            gt_r = Rot([self.sb(st, "gtk", [128, 32, 512], BF16) for _ in range(2)])
            mf_r = Rot([self.sb(st, "mf", [128, 512], F32) for _ in range(2)])
            tf_r = Rot([self.sb(st, "tf", [128, 512], F32) for _ in range(3)])
            mT_r = Rot([self.sb(st, "mT", [128, 8, 512], BF16) for _ in range(2)])
            xt_r = Rot([self.sb(st, "xtg", [128, D], F32) for _ in range(2)])
            xn_r = Rot([self.sb(st, "xng", [128, D], F32) for _ in range(2)])
            for b in range(NB):
                c0, c1 = b * 512, (b + 1) * 512
                ob, gt, mT = ob_r.next(), gt_r.next(), mT_r.next()
                for n in range(4):
                    s.dma("sp", ob[:, n, :, :], self.oT[n, :, c0:c1].rearrange("(k p) t -> p k t", p=128), reads=[self.oT], writes=[ob])
                for n in range(4):
                    s.dma("sp", gt[:, n * 8:(n + 1) * 8, :], self.gateT[n * 1024:(n + 1) * 1024, c0:c1].rearrange("(g p) t -> p g t", p=128),
                          reads=[self.gateT], writes=[gt])
                for cg in range(8):
                    mf = mf_r.next()
                    for n in range(4):
                        p = ps.next()
                        for k in range(2):
                            self.mm(p[:, :], wbb[:, n, k, cg * 128:(cg + 1) * 128], ob[:, n, k, :], k == 0, k == 1, [wbb, ob], [p])
                        if n == 0:
                            self.tt("dve", mf[:], p[:, :], gt[:, n * 8 + cg, :], ALU.mult, [p, gt], [mf])
                        else:
                            tf = tf_r.next()
                            self.tt("dve", tf[:], p[:, :], gt[:, n * 8 + cg, :], ALU.mult, [p, gt], [tf])
                            if n < 3:
                                self.tt("pool", mf[:], mf[:], tf[:], ALU.add, [mf, tf], [mf])
                            else:
                                self.tt("pool", mT[:, cg, :], mf[:], tf[:], ALU.add, [mf, tf], [mT])
                for tt_ in range(4):
                    r0 = c0 + tt_ * 128
                    xt, xn = xt_r.next(), xn_r.next()
                    s.dma("sp", xt[:], src[r0:r0 + 128, :], reads=[src], writes=[xt])
                    for half in range(2):
                        p = ps.next()
                        for cg in range(8):
                            self.mm(p[:, :], mT[:, cg, tt_ * 128:(tt_ + 1) * 128], wob[:, cg, half * 512:(half + 1) * 512], cg == 0, cg == 7, [mT, wob], [p])
                        self.tt("dve", xn[:, half * 512:(half + 1) * 512], p[:, :], xt[:, half * 512:(half + 1) * 512], ALU.add, [p, xt], [xn])
                    s.dma("pool", self.xres[r0:r0 + 128, :], xn[:], reads=[xn], writes=[self.xres])

    def phase_H(self, l):
        s, S = self.s, self.S
        SB = 1024
        NG = DFF // 128
        with self.scope() as st:
            hT = self.sb(st, "h2T", [128, 8, SB], BF16)
            aT = self.sb(st, "aT", [128, NG, SB], BF16)
            nt = self.norm_tiles(st)
            ps = Rot([self.psb(st, "psH") for _ in range(4)])
            wgb_r = Rot([self.sb(st, "wgbf", [128, 8, 128], BF16) for _ in range(3)])
            wub_r = Rot([self.sb(st, "wubf", [128, 8, 128], BF16) for _ in range(3)])
            wdb_r = Rot([self.sb(st, "wdb", [128, NG, 512], BF16) for _ in range(2)])
            sg_r = Rot([self.sb(st, "sg", [128, 512], BF16) for _ in range(3)])
            xt_r = Rot([self.sb(st, "xth", [128, 512], F32) for _ in range(3)])
            xn_r = Rot([self.sb(st, "xnh", [128, 512], F32) for _ in range(3)])

            def load_gu(g):
                wgb, wub = wgb_r.next(), wub_r.next()
                s.dma("pool", wgb[:], self.w_fg[l, :, g * 128:(g + 1) * 128].rearrange("(k p) n -> p k n", p=128), reads=[self.w_fg], writes=[wgb])
                s.dma("pool", wub[:], self.w_fu[l, :, g * 128:(g + 1) * 128].rearrange("(k p) n -> p k n", p=128), reads=[self.w_fu], writes=[wub])
                return wgb, wub

            def load_d(cq):
                wdb = wdb_r.next()
                for kk in range(2):
                    s.dma("pool", wdb[:, kk * 11:(kk + 1) * 11, :], self.w_fd[l, kk * 1408:(kk + 1) * 1408, cq * 512:(cq + 1) * 512].rearrange("(k p) n -> p k n", p=128),
                          reads=[self.w_fd], writes=[wdb])
                return wdb

            for sbk in range(S // SB):
                t0 = sbk * SB
                self.norm_to_hT(nt, self.xres, self.gffn, l, hT, t0, SB // 128)
                nxt = load_gu(0)
                for g in range(NG):
                    wgb, wub = nxt
                    if g + 1 < NG:
                        nxt = load_gu(g + 1)
                    for b in range(SB // 512):
                        pg, pu = ps.next(), ps.next()
                        for k in range(8):
                            self.mm(pg[:, :], wgb[:, k, :], hT[:, k, b * 512:(b + 1) * 512], k == 0, k == 7, [wgb, hT], [pg])
                        for k in range(8):
                            self.mm(pu[:, :], wub[:, k, :], hT[:, k, b * 512:(b + 1) * 512], k == 0, k == 7, [wub, hT], [pu])
                        sg = sg_r.next()
                        self.act(sg[:], pg[:, :], AF.Silu, [pg], [sg])
                        self.tt("dve", aT[:, g, b * 512:(b + 1) * 512], pu[:, :], sg[:], ALU.mult, [pu, sg], [aT])
                nxtd = load_d(0)
                for cq in range(2):
                    wdb = nxtd
                    if cq + 1 < 2:
                        nxtd = load_d(cq + 1)
                    for tt_ in range(SB // 128):
                        r0 = t0 + tt_ * 128
                        xt, xn = xt_r.next(), xn_r.next()
                        s.dma("sp", xt[:], self.xres[r0:r0 + 128, cq * 512:(cq + 1) * 512], reads=[self.xres], writes=[xt])
                        p = ps.next()
                        for k in range(NG):
                            self.mm(p[:, :], aT[:, k, tt_ * 128:(tt_ + 1) * 128], wdb[:, k, :], k == 0, k == NG - 1, [aT, wdb], [p])
                        self.tt("dve", xn[:], p[:, :], xt[:], ALU.add, [p, xt], [xn])
                        s.dma("pool", self.xres[r0:r0 + 128, cq * 512:(cq + 1) * 512], xn[:], reads=[xn], writes=[self.xres])

    def phase_final(self):
        s, S, NT = self.s, self.S, self.NT
        with self.scope() as st:
            gf = self.sb(st, "gf", [128, D], F32)
            s.dma("sp", gf[:], self.g_final[:].partition_broadcast(128), reads=[self.g_final], writes=[gf])
            xt_r = Rot([self.sb(st, "xtf", [128, D], F32) for _ in range(3)])
            yo_r = Rot([self.sb(st, "yof", [128, D], F32) for _ in range(3)])
            junk = self.sb(st, "junkf", [128, D], BF16)
            ss_r = Rot([self.sb(st, "ssf", [128, 1], F32) for _ in range(3)])
            for t in range(NT):
                xt, yo, ss = xt_r.next(), yo_r.next(), ss_r.next()
                s.dma("sp", xt[:], self.xres[t * 128:(t + 1) * 128, :], reads=[self.xres], writes=[xt])
                self.act(junk[:], xt[:], AF.Square, [xt], [junk, ss], accum_out=ss[:])
                self.act(ss[:], ss[:], AF.Sqrt, [ss, self.epsb], [ss], scale=1.0 / D, bias=self.epsb[:, 0:1])
                self.recip(ss[:], ss[:], [ss], [ss])
                self.stt(yo[:], xt[:], ss[:, 0:1], gf[:], ALU.mult, ALU.mult, [xt, ss, gf], [yo])
                s.dma("pool", self.y_out[t * 128:(t + 1) * 128, :], yo[:], reads=[yo], writes=[self.y_out])


def make_in_maps(inputs, S, batch_ids):
    f = lambda a: np.ascontiguousarray(np.asarray(a))
    consts = host_consts()
    shared = {
        "w_in": f(inputs["w_in"]), "w_qup": f(inputs["mla_w_q_up"]), "w_kvup": f(inputs["mla_w_kv_up"]),
        "w_br": f(inputs["w_branch"]), "w_out": f(inputs["w_out"]), "w_fg": f(inputs["w_ffn_gate"]),
        "w_fu": f(inputs["w_ffn_up"]), "w_fd": f(inputs["w_ffn_down"]),
        "g_attn": f(np.asarray(inputs["attn_norm_g"]).reshape(NLAYER, 8, 128).transpose(2, 0, 1)),
        "g_ffn": f(np.asarray(inputs["ffn_norm_g"]).reshape(NLAYER, 8, 128).transpose(2, 0, 1)),
        "g_final": f(inputs["final_norm_g"]),
        "g_q": f(np.asarray(inputs["mla_q_norm_g"]).reshape(NLAYER, 2, 128).transpose(2, 0, 1)),
        "g_kv": f(np.asarray(inputs["mla_kv_norm_g"]).transpose(1, 0)),
        "conv_w": f(np.asarray(inputs["conv_w"]).reshape(NLAYER, 3, 2, 128).transpose(3, 0, 2, 1)),
        "gla_wg": f(inputs["gla_w_gate_up"]),
        "gla_bg": f(np.asarray(inputs["gla_b_gate"]).reshape(NLAYER, 1, 128)),
        "gla_ng": f(inputs["gla_norm_g"]),
    }
    shared.update(consts)
    maps = []
    x = np.asarray(inputs["x"])
    pos = np.asarray(inputs["positions"])
    for b in batch_ids:
        m = dict(shared)
        m["x"] = f(x[b, :S])
        m["pos"] = f(pos[b, :S].astype(np.int32))
        maps.append(m)
    return maps


_NC_CACHE = {}


def kernel(**inputs):
    S = 4096
    key = ("full", S)
    if key not in _NC_CACHE:
        _NC_CACHE[key] = Builder(S, NLAYER).build()
    nc = _NC_CACHE[key]
    in_maps = make_in_maps(inputs, S, list(range(8)))
    res = run_bass_kernel_spmd(nc, in_maps, core_ids=list(range(8)))
    out = np.stack([np.asarray(r["y"]) for r in res.results], axis=0).astype(np.float32)
    return out
```

```python
import numpy as np
from contextlib import ExitStack, contextmanager
import concourse.bass as bass
import concourse.mybir as mybir
from concourse.bass_utils import run_bass_kernel_spmd

F32 = mybir.dt.float32
BF16 = mybir.dt.bfloat16
I32 = mybir.dt.int32
AF = mybir.ActivationFunctionType
ALU = mybir.AluOpType
AX = mybir.AxisListType

D = 1024
DFF = 2816
NLAYER = 4
INTOT = 6744
EPS = 1e-6
EPOCH = 30000
NBIS = 12

C_CQ, C_CKV, C_KROPE = 0, 256, 384
C_CB, C_CC, C_CX = 416, 672, 928
C_DQ, C_DK, C_DV = 1184, 1440, 1504
C_IQ, C_IK, C_IW = 1568, 1824, 1856
C_GQ, C_GK, C_GV, C_GLR, C_GR = 1864, 1992, 2120, 2376, 2392
C_GATE = 2648


class Buf:
    def __init__(self, name, t=None):
        self.name = name
        self.t = t
        self.last_write = None
        self.readers = {}
        self.wsem = None
        self.rsem = None

    def __getitem__(self, idx):
        return self.t[idx]


class Sched:
    COMPUTE = ("pe", "act", "dve", "pool")
    SEMCAP = 30000

    def __init__(self, nc):
        self.nc = nc
        self.engs = list(self.COMPUTE) + ["sp"]
        self.ops = {e: [] for e in self.engs}
        self.cnt = {e: 0 for e in self.COMPUTE}
        self.sems = {}
        self.sem_names = []
        self.waited = {e: {} for e in self.engs}
        self.nops = 0
        self.semval = {}
        self.free_dma = []
        self.ndma_sems = 0

    def _sem(self, name):
        if name not in self.sems:
            self.sems[name] = None
            self.sem_names.append(name)
        return name

    def _dma_sem(self):
        if self.free_dma:
            return self.free_dma.pop()
        self.ndma_sems += 1
        name = self._sem(f"d_{self.ndma_sems}")
        self.semval[name] = 0
        return name

    def release(self, bufs):
        for b in bufs:
            for nm in (b.wsem, b.rsem):
                if nm is not None and self.semval[nm] < self.SEMCAP:
                    self.free_dma.append(nm)
            b.wsem = b.rsem = None

    def _compute_token(self, e):
        self.cnt[e] += 1
        k = self.cnt[e]
        ep = (k - 1) // EPOCH
        return (self._sem(f"c_{e}_{ep}"), (k - 1) % EPOCH + 1, e)

    def _need(self, e, tok, waits):
        if tok is None:
            return
        sem, val, src = tok
        if src == e and e == "pe":
            return
        w = self.waited[e]
        if w.get(sem, 0) >= val:
            return
        w[sem] = val
        waits.append((sem, val))

    def _deps(self, e, reads, writes):
        waits = []
        for b in reads:
            self._need(e, b.last_write, waits)
        for b in writes:
            self._need(e, b.last_write, waits)
            for r in b.readers.values():
                self._need(e, r, waits)
        return waits

    def _mark(self, tok, reads, writes):
        for b in reads:
            old = b.readers.get(tok[0])
            if old is None or old[1] < tok[1]:
                b.readers[tok[0]] = tok
        for b in writes:
            b.last_write = tok
            b.readers = {}

    def op(self, e, method, args, kwargs, reads=(), writes=()):
        waits = self._deps(e, reads, writes)
        tok = self._compute_token(e)
        self._mark(tok, reads, writes)
        self.ops[e].append((waits, method, args, kwargs, (tok[0], 1)))
        self.nops += 1
        return tok

    def dma(self, q, out_ap, in_ap, reads=(), writes=(), **kw):
        waits = self._deps(q, reads, writes)
        if writes:
            b = writes[0]
            if b.wsem is None:
                b.wsem = self._dma_sem()
            nm = b.wsem
        else:
            b = reads[0]
            if b.rsem is None:
                b.rsem = self._dma_sem()
            nm = b.rsem
        self.semval[nm] += 16
        tok = (nm, self.semval[nm], "dma")
        self._mark(tok, reads, writes)
        kw = dict(kw)
        kw["out"] = out_ap
        kw["in_"] = in_ap
        self.ops[q].append((waits, "dma_start", (), kw, (nm, 16)))
        self.nops += 1
        return tok

    def wait_all(self, e, bufs):
        waits = self._deps(e, (), bufs)
        if waits:
            self.ops[e].append((waits, None, None, None, None))

    def barrier(self):
        toks = []
        for e in self.COMPUTE:
            k = self.cnt[e]
            if k > 0:
                ep = (k - 1) // EPOCH
                toks.append((f"c_{e}_{ep}", (k - 1) % EPOCH + 1, e))
        for nm, v in self.semval.items():
            if v > 0:
                toks.append((nm, v, "dma"))
        for e in self.engs:
            waits = []
            for tok in toks:
                self._need(e, tok, waits)
            if waits:
                self.ops[e].append((waits, None, None, None, None))

    def emit(self, stack):
        nc = self.nc
        if not hasattr(self, "sem_stack"):
            self.sem_stack = stack
        for name in self.sem_names:
            if self.sems[name] is None:
                self.sems[name] = self.sem_stack.enter_context(nc.semaphore(name))
        if not any(self.ops[e] for e in self.engs):
            return
        with nc.Block() as block:
            amap = {"pe": block.tensor, "act": block.scalar, "dve": block.vector,
                    "pool": block.gpsimd, "sp": block.sync}
            for e in self.engs:
                ops = self.ops[e]
                if not ops:
                    continue

                def body(eng, ops=ops):
                    for waits, method, args, kwargs, inc in ops:
                        for sem, val in waits:
                            eng.wait_ge(self.sems[sem], val)
                        if method is not None:
                            ins = getattr(eng, method)(*args, **kwargs)
                            ins.then_inc(self.sems[inc[0]], inc[1])

                amap[e](body)
                self.ops[e] = []


class Rot:
    def __init__(self, bufs):
        self.bufs = bufs
        self.i = 0

    def next(self):
        b = self.bufs[self.i % len(self.bufs)]
        self.i += 1
        return b


def host_consts():
    j = np.arange(128)[:, None]
    i = np.arange(128)[None, :]
    same = (j // 64) == (i // 64)
    c = {}
    c["c_ident"] = np.eye(128, dtype=np.float32)
    c["c_tri"] = np.where(same & (j <= i), -1.0 / 16.0, 0.0).astype(np.float32)
    c["c_rev"] = np.where(same & (j > i), -1.0 / 16.0, 0.0).astype(np.float32)
    c["c_gmask"] = np.where(same & (j <= i), 1.0, 0.0).astype(np.float32)
    c["c_cmt"] = np.where(j > i, -30000.0, 0.0).astype(np.float32)
    c["c_cmq"] = np.ascontiguousarray(c["c_cmt"].T)
    rot = np.zeros((32, 32), np.float32)
    for fp in range(16):
        rot[fp + 16, fp] = -1.0
    for fp in range(16, 32):
        rot[fp - 16, fp] = 1.0
    c["c_rot"] = rot
    half = 16
    invf = (np.float32(10000.0) ** (-np.arange(half, dtype=np.float32) / np.float32(half))).astype(np.float32)
    c["c_invf"] = np.concatenate([invf, invf]).reshape(32, 1).astype(np.float32)
    return c


class Builder:
    def __init__(self, S, nl, dbg=(), stop_after=None, phases=None):
        self.S = S
        self.NT = S // 128
        self.NB = S // 512
        self.nl = nl
        self.dbg = set(dbg)
        self.stop_after = stop_after
        self.phases = phases
        self.nc = bass.Bass("TRN2", target_bir_lowering=False)
        self.s = Sched(self.nc)
        self.uid = 0
        self.live = []

    def din(self, name, shape, dt=F32):
        return self.nc.dram_tensor(name, list(shape), dt, kind="ExternalInput").ap()

    def dscr(self, name, shape, dt=BF16):
        kind = "ExternalOutput" if name in self.dbg else "Internal"
        ap = self.nc.dram_tensor(name, list(shape), dt, kind=kind).ap()
        return Buf(name, ap)

    def sb(self, st, name, shape, dt):
        self.uid += 1
        nm = f"{name}_{self.uid}"
        b = Buf(nm, st.enter_context(self.nc.sbuf_tensor(nm, list(shape), dt)))
        self.live.append(b)
        return b

    def psb(self, st, name, dt=F32):
        self.uid += 1
        nm = f"{name}_{self.uid}"
        shape = [128, 512] if dt == F32 else [128, 1024]
        b = Buf(nm, st.enter_context(self.nc.psum_tensor(nm, shape, dt)))
        self.live.append(b)
        return b

    @contextmanager
    def scope(self):
        st = ExitStack()
        mark = len(self.live)
        yield st
        self.s.barrier()
        self.s.release(self.live[mark:])
        del self.live[mark:]
        self.s.emit(self.top)
        st.close()

    def mm(self, out, lhsT, rhs, start, stop, reads, writes):
        self.s.op("pe", "matmul", (out,), dict(lhsT=lhsT, rhs=rhs, start=start, stop=stop), reads, writes)

    def tr(self, out, in_, reads, writes):
        self.s.op("pe", "transpose", (), dict(out=out, in_=in_, identity=self.identb[:]), list(reads) + [self.identb], writes)

    def act(self, out, in_, func, reads, writes, **kw):
        self.s.op("act", "activation", (), dict(out=out, in_=in_, func=func, **kw), reads, writes)

    def tt(self, eng, out, in0, in1, op, reads, writes):
        self.s.op(eng, "tensor_tensor", (), dict(out=out, in0=in0, in1=in1, op=op), reads, writes)

    def ts(self, eng, out, in0, s1, s2, op0, op1=None, reads=(), writes=(), accum_out=None):
        kw = dict(out=out, in0=in0, scalar1=s1, scalar2=s2, op0=op0)
        if op1 is not None:
            kw["op1"] = op1
        if accum_out is not None:
            kw["accum_out"] = accum_out
        self.s.op(eng, "tensor_scalar", (), kw, reads, writes)

    def stt(self, out, in0, scalar, in1, op0, op1, reads, writes):
        self.s.op("dve", "scalar_tensor_tensor", (), dict(out=out, in0=in0, scalar=scalar, in1=in1, op0=op0, op1=op1), reads, writes)

    def cp(self, eng, out, in_, reads, writes):
        self.s.op(eng, "tensor_copy", (), dict(out=out, in_=in_), reads, writes)

    def ms(self, eng, ap, val, writes):
        self.s.op(eng, "memset", (ap, val), {}, (), writes)

    def recip(self, out, in_, reads, writes):
        self.s.op("dve", "reciprocal", (), dict(out=out, in_=in_), reads, writes)

    def build(self):
        nc, s, S, NT = self.nc, self.s, self.S, self.NT
        self.x_in = Buf("x", self.din("x", [S, D]))
        self.pos = Buf("pos", self.din("pos", [S], I32))
        self.w_in = Buf("w_in", self.din("w_in", [NLAYER, D, INTOT]))
        self.w_qup = Buf("w_qup", self.din("w_qup", [NLAYER, 256, 384]))
        self.w_kvup = Buf("w_kvup", self.din("w_kvup", [NLAYER, 128, 512]))
        self.w_br = Buf("w_br", self.din("w_br", [NLAYER, 4, 256, D]))
        self.w_out = Buf("w_out", self.din("w_out", [NLAYER, D, D]))
        self.w_fg = Buf("w_fg", self.din("w_fg", [NLAYER, D, DFF]))
        self.w_fu = Buf("w_fu", self.din("w_fu", [NLAYER, D, DFF]))
        self.w_fd = Buf("w_fd", self.din("w_fd", [NLAYER, DFF, D]))
        self.g_attn = Buf("g_attn", self.din("g_attn", [128, NLAYER, 8]))
        self.g_ffn = Buf("g_ffn", self.din("g_ffn", [128, NLAYER, 8]))
        self.g_final = Buf("g_final", self.din("g_final", [D]))
        self.g_q = Buf("g_q", self.din("g_q", [128, NLAYER, 2]))
        self.g_kv = Buf("g_kv", self.din("g_kv", [128, NLAYER]))
        self.conv_w = Buf("conv_w", self.din("conv_w", [128, NLAYER, 2, 3]))
        self.gla_wg = Buf("gla_wg", self.din("gla_wg", [NLAYER, 16, 128]))
        self.gla_bg = Buf("gla_bg", self.din("gla_bg", [NLAYER, 1, 128]))
        self.gla_ng = Buf("gla_ng", self.din("gla_ng", [NLAYER, 64]))
        cin = {k: Buf(k, self.din(k, list(v.shape))) for k, v in host_consts().items()}
        self.y_out = Buf("y", nc.dram_tensor("y", [S, D], F32, kind="ExternalOutput").ap())
        self.xres = self.dscr("xres", [S, D], F32)
        self.cosT = self.dscr("cosT", [32, S], F32)
        self.sinT = self.dscr("sinT", [32, S], F32)
        self.cqT = self.dscr("cqT", [256, S])
        self.ckvT = self.dscr("ckvT", [128, S])
        self.kropeT = self.dscr("kropeT", [32, S])
        self.cbT = self.dscr("cbT", [256, S])
        self.ccT = self.dscr("ccT", [256, S])
        self.cxT = self.dscr("cxT", [256, S])
        self.dqT = self.dscr("dqT", [128, 4, S])
        self.dkT = self.dscr("dkT", [128, S])
        self.iqT = self.dscr("iqT", [128, 8, S])
        self.ikT = self.dscr("ikT", [128, S])
        self.gqT = self.dscr("gqT", [32, 4, S])
        self.gkT = self.dscr("gkT", [32, 4, S])
        self.glrT = self.dscr("glrT", [17, S])
        self.hTd = self.dscr("hTd", [1024, S])
        self.dvaug = self.dscr("dvaug", [S, 65])
        self.iw = self.dscr("iw", [S, 8], F32)
        self.gk = self.dscr("gk", [S, 128])
        self.gv = self.dscr("gv", [S, 256])
        self.grs = self.dscr("grs", [S, 256])
        self.qT = self.dscr("qT", [4, 96, S])
        self.kT = self.dscr("kT", [4, 96, S])
        self.vaug = self.dscr("vaug", [S, 4, 65])
        self.oT = self.dscr("oT", [4, 256, S])
        self.dsc = self.dscr("dsc", [S, S], F32) if "dsc" in self.dbg else None

        with ExitStack() as top:
            self.top = top
            cst = {}
            for k in ("c_ident", "c_tri", "c_rev", "c_gmask", "c_cmt", "c_cmq"):
                cst[k] = self.sb(top, k, [128, 128], F32)
                s.dma("sp", cst[k][:], cin[k][:], reads=[cin[k]], writes=[cst[k]])
            self.identf = cst["c_ident"]
            self.tri = cst["c_tri"]
            self.rev = cst["c_rev"]
            self.identb = self.sb(top, "identb", [128, 128], BF16)
            self.gmaskb = self.sb(top, "gmaskb", [128, 128], BF16)
            self.cmtb = self.sb(top, "cmtb", [128, 128], BF16)
            self.cmqb = self.sb(top, "cmqb", [128, 128], BF16)
            self.i4b = self.sb(top, "i4b", [128, 512], BF16)
            self.onesb = self.sb(top, "onesb", [128, 128], BF16)
            self.epsb = self.sb(top, "epsb", [128, 1], F32)
            self.rotb = self.sb(top, "rotb", [32, 32], BF16)
            rotf = self.sb(top, "rotf", [32, 32], F32)
            self.invf = self.sb(top, "invf", [32, 1], F32)
            s.dma("sp", rotf[:], cin["c_rot"][:], reads=[cin["c_rot"]], writes=[rotf])
            s.dma("sp", self.invf[:], cin["c_invf"][:], reads=[cin["c_invf"]], writes=[self.invf])
            self.cp("dve", self.identb[:], self.identf[:], [self.identf], [self.identb])
            self.cp("dve", self.gmaskb[:], cst["c_gmask"][:], [cst["c_gmask"]], [self.gmaskb])
            self.cp("dve", self.cmtb[:], cst["c_cmt"][:], [cst["c_cmt"]], [self.cmtb])
            self.cp("dve", self.cmqb[:], cst["c_cmq"][:], [cst["c_cmq"]], [self.cmqb])
            self.cp("dve", self.rotb[:], rotf[:], [rotf], [self.rotb])
            for h in range(4):
                self.cp("dve", self.i4b[:, h * 128:(h + 1) * 128], self.identf[:], [self.identf], [self.i4b])
            self.ms("dve", self.onesb[:], 1.0, [self.onesb])
            self.ms("dve", self.epsb[:], EPS, [self.epsb])
            self.gattn = self.sb(top, "gattn", [128, NLAYER, 8], F32)
            self.gffn = self.sb(top, "gffn", [128, NLAYER, 8], F32)
            self.gq = self.sb(top, "gq", [128, NLAYER, 2], F32)
            self.gkv = self.sb(top, "gkv", [128, NLAYER], F32)
            self.convw = self.sb(top, "convw", [128, NLAYER, 2, 3], F32)
            for dst, srcb in ((self.gattn, self.g_attn), (self.gffn, self.g_ffn), (self.gq, self.g_q),
                              (self.gkv, self.g_kv), (self.convw, self.conv_w)):
                s.dma("sp", dst[:], srcb[:], reads=[srcb], writes=[dst])
            s.emit(top)

            allph = "RABCEDFGH"
            ph = self.phases if self.phases is not None else allph
            if "R" in ph:
                self.phase_rope()
            done = False
            for l in range(self.nl):
                for name, fn in (("A", self.phase_A), ("B", self.phase_B), ("E", self.phase_E),
                                 ("D", self.phase_D), ("F", self.phase_F), ("G", self.phase_G), ("H", self.phase_H)):
                    if name in ph:
                        fn(l)
                    if self.stop_after == (l, name):
                        done = True
                        break
                if done:
                    break
            if not done:
                self.phase_final()
            outs = [self.y_out] + [getattr(self, n) for n in self.dbg if getattr(self, n, None) is not None]
            s.wait_all("sp", outs)
            s.wait_all("pool", outs)
            s.barrier()
            s.emit(top)
        return nc

    def phase_rope(self):
        s, S = self.s, self.S
        W = min(S, 1024)
        with self.scope() as st:
            pi = self.sb(st, "pi", [32, W], I32)
            pf = self.sb(st, "pf", [32, W], F32)
            pk = self.sb(st, "pk", [32, W], I32)
            pkf = self.sb(st, "pkf", [32, W], F32)
            pc = self.sb(st, "pc", [32, W], F32)
            pa = self.sb(st, "pa", [32, W], F32)
            res = self.sb(st, "res", [32, W], F32)
            for c0 in range(0, S, W):
                s.dma("sp", pi[:], self.pos[c0:c0 + W].partition_broadcast(32), reads=[self.pos], writes=[pi])
                self.cp("dve", pf[:], pi[:], [pi], [pf])
                self.ts("dve", pf[:], pf[:], self.invf[:, 0:1], None, ALU.mult, reads=[pf, self.invf], writes=[pf])
                for shift, dst in ((0.0, self.sinT), (0.25, self.cosT)):
                    self.ts("dve", pa[:], pf[:], float(1.0 / (2 * np.pi)), shift, ALU.mult, ALU.add, reads=[pf], writes=[pa])
                    self.cp("dve", pk[:], pa[:], [pa], [pk])
                    self.cp("dve", pkf[:], pk[:], [pk], [pkf])
                    self.tt("dve", pa[:], pa[:], pkf[:], ALU.subtract, [pa, pkf], [pa])
                    self.ts("dve", pc[:], pa[:], 0.5, None, ALU.is_gt, reads=[pa], writes=[pc])
                    self.tt("dve", pa[:], pa[:], pc[:], ALU.subtract, [pa, pc], [pa])
                    self.ts("dve", pc[:], pa[:], -0.5, None, ALU.is_lt, reads=[pa], writes=[pc])
                    self.tt("dve", pa[:], pa[:], pc[:], ALU.add, [pa, pc], [pa])
                    self.act(res[:], pa[:], AF.Sin, [pa], [res], scale=float(2 * np.pi))
                    s.dma("pool", dst[:, c0:c0 + W], res[:], reads=[res], writes=[dst])

    def norm_tiles(self, st):
        return dict(
            xt=Rot([self.sb(st, "xt", [128, D], F32) for _ in range(3)]),
            xs=Rot([self.sb(st, "xs", [128, D], BF16) for _ in range(2)]),
            junk=self.sb(st, "junk", [128, D], BF16),
            ss=Rot([self.sb(st, "ss", [128, 1], F32) for _ in range(3)]),
            rs=Rot([self.sb(st, "rs", [128, 1], F32) for _ in range(3)]),
            psT=Rot([self.psb(st, "psT", BF16) for _ in range(2)]))

    def norm_to_hT(self, nt, src, g_tile, l, hT, tok0, ntiles):
        s = self.s
        for t in range(ntiles):
            xt, xs, ss, rs = nt["xt"].next(), nt["xs"].next(), nt["ss"].next(), nt["rs"].next()
            junk = nt["junk"]
            r0 = tok0 + t * 128
            s.dma("sp", xt[:], src[r0:r0 + 128, :], reads=[src], writes=[xt])
            self.act(junk[:], xt[:], AF.Square, [xt], [junk, ss], accum_out=ss[:])
            self.act(rs[:], ss[:], AF.Sqrt, [ss, self.epsb], [rs], scale=1.0 / D, bias=self.epsb[:, 0:1])
            self.recip(rs[:], rs[:], [rs], [rs])
            self.ts("dve", xs[:], xt[:], rs[:, 0:1], None, ALU.mult, reads=[xt, rs], writes=[xs])
            pT = nt["psT"].next()
            for k in range(8):
                self.tr(pT[:, k * 128:(k + 1) * 128], xs[:, k * 128:(k + 1) * 128], [xs], [pT])
            self.tt("dve", hT[:, :, t * 128:(t + 1) * 128], pT[:, :].rearrange("p (k q) -> p k q", k=8),
                    g_tile[:, l, :].unsqueeze(2).to_broadcast([128, 8, 128]), ALU.mult, [pT, g_tile], [hT])

    def phase_A(self, l):
        s, S, NT, NB = self.s, self.S, self.NT, self.NB
        src = self.x_in if l == 0 else self.xres
        with self.scope() as st:
            hT = self.sb(st, "hT", [128, 8, S], BF16)
            with self.scope() as st1:
                ones_row = self.sb(st1, "ones_row", [1, S], BF16)
                self.ms("dve", ones_row[:], 1.0, [ones_row])
                s.dma("pool", self.glrT[16:17, :], ones_row[:], reads=[ones_row], writes=[self.glrT])
                nt = self.norm_tiles(st1)
                self.norm_to_hT(nt, src, self.gattn, l, hT, 0, NT)
            ps = Rot([self.psb(st, "psA") for _ in range(4)])
            wbf_r = Rot([self.sb(st, "wbf", [128, 8, 256], BF16) for _ in range(3)])
            stg_r = Rot([self.sb(st, "stg", [128, S], BF16) for _ in range(4)])
            evac = [0]

            def load_w(c0, n):
                wbf = wbf_r.next()
                s.dma("pool", wbf[:, :, 0:n], self.w_in[l, :, c0:c0 + n].rearrange("(k p) n -> p k n", p=128),
                      reads=[self.w_in], writes=[wbf])
                return wbf

            def fm_group(wbf, n, outs, func, lo_outs=None):
                stg = stg_r.next()
                stl = stg_r.next() if lo_outs else None
                for b in range(NB):
                    p = ps.next()
                    for k in range(8):
                        self.mm(p[0:n, :], wbf[:, k, 0:n], hT[:, k, b * 512:(b + 1) * 512], k == 0, k == 7, [wbf, hT], [p])
                    evac[0] += 1
                    if lo_outs:
                        self.act(stg[0:n, b * 512:(b + 1) * 512], p[0:n, :], func, [p], [stg])
                        self.tt("dve", stl[0:n, b * 512:(b + 1) * 512], p[0:n, :], stg[0:n, b * 512:(b + 1) * 512], ALU.subtract, [p, stg], [stl])
                    elif func != AF.Copy or evac[0] % 2 == 0:
                        self.act(stg[0:n, b * 512:(b + 1) * 512], p[0:n, :], func, [p], [stg])
                    else:
                        self.cp("dve", stg[0:n, b * 512:(b + 1) * 512], p[0:n, :], [p], [stg])
                for (r0, r1, dbuf, dap) in outs:
                    s.dma("pool", dap, stg[r0:r1, :], reads=[stg], writes=[dbuf])
                for (r0, r1, dbuf, dap) in (lo_outs or []):
                    s.dma("pool", dap, stl[r0:r1, :], reads=[stl], writes=[dbuf])

            fm = []
            for g in range(2):
                fm.append((C_CQ + 128 * g, 128, [(0, 128, self.cqT, self.cqT[128 * g:128 * (g + 1), :])], AF.Copy))
            fm.append((C_CKV, 128, [(0, 128, self.ckvT, self.ckvT[:, :])], AF.Copy))
            fm.append((C_KROPE, 32, [(0, 32, self.kropeT, self.kropeT[:, :])], AF.Copy))
            for c0, dst in ((C_CB, self.cbT), (C_CC, self.ccT), (C_CX, self.cxT)):
                for g in range(2):
                    fm.append((c0 + 128 * g, 128, [(0, 128, dst, dst[128 * g:128 * (g + 1), :])], AF.Copy))
            for g in range(2):
                fm.append((C_DQ + 128 * g, 128, [(64 * j, 64 * (j + 1), self.dqT, self.dqT[64 * r:64 * (r + 1), 2 * g + j, :]) for j in range(2) for r in range(2)], AF.Copy))
            fm.append((C_DK, 64, [(0, 64, self.dkT, self.dkT[0:64, :])], AF.Copy, [(0, 64, self.dkT, self.dkT[64:128, :])]))
            for g in range(2):
                fm.append((C_IQ + 128 * g, 128, [(32 * j, 32 * (j + 1), self.iqT, self.iqT[32 * r:32 * (r + 1), 4 * g + j, :]) for j in range(4) for r in (0, 1)], AF.Copy,
                           [(32 * j, 32 * (j + 1), self.iqT, self.iqT[32 * r:32 * (r + 1), 4 * g + j, :]) for j in range(4) for r in (2, 3)]))
            fm.append((C_IK, 32, [(0, 32, self.ikT, self.ikT[32 * r:32 * (r + 1), :]) for r in (0, 2)], AF.Copy,
                       [(0, 32, self.ikT, self.ikT[32 * r:32 * (r + 1), :]) for r in (1, 3)]))
            fm.append((C_GQ, 128, [(32 * j, 32 * (j + 1), self.gqT, self.gqT[:, j, :]) for j in range(4)], AF.Copy))
            fm.append((C_GK, 128, [(32 * j, 32 * (j + 1), self.gkT, self.gkT[:, j, :]) for j in range(4)], AF.Copy))
            fm.append((C_GLR, 16, [(0, 16, self.glrT, self.glrT[0:16, :])], AF.Copy))
            for k in range(8):
                s.dma("pool", self.hTd[k * 128:(k + 1) * 128, :], hT[:, k, :], reads=[hT], writes=[self.hTd])
            nxt = load_w(fm[0][0], fm[0][1])
            for i, ent in enumerate(fm):
                c0, n, outs, func = ent[:4]
                wbf = nxt
                if i + 1 < len(fm):
                    nxt = load_w(fm[i + 1][0], fm[i + 1][1])
                fm_group(wbf, n, outs, func, ent[4] if len(ent) > 4 else None)

            tms = [(C_DV, 64, self.dvaug, BF16, AF.Copy, True), (C_IW, 8, self.iw, F32, AF.Copy, False),
                   (C_GK, 128, self.gk, BF16, AF.Copy, False), (C_GV, 256, self.gv, BF16, AF.Copy, False),
                   (C_GR, 256, self.grs, BF16, AF.Silu, False)]
            for (c0, n, dst, dt, func, ones_col) in tms:
                wbf = load_w(c0, n)
                nn = n + (1 if ones_col else 0)
                tstg = self.sb(st, "tstg", [128, NT, nn], dt)
                if ones_col:
                    self.ms("dve", tstg[:, :, n:n + 1], 1.0, [tstg])
                for t in range(NT):
                    p = ps.next()
                    for k in range(8):
                        self.mm(p[:, 0:n], hT[:, k, t * 128:(t + 1) * 128], wbf[:, k, 0:n], k == 0, k == 7, [wbf, hT], [p])
                    self.act(tstg[:, t, 0:n], p[:, 0:n], func, [p], [tstg])
                s.dma("pool", dst[:, :].rearrange("(t p) n -> p t n", p=128), tstg[:], reads=[tstg], writes=[dst])

    def phase_B(self, l):
        s, S, NT, NB = self.s, self.S, self.NT, self.NB
        with self.scope() as st:
            wqf = self.sb(st, "wqf", [128, 2, 384], F32)
            wqb = self.sb(st, "wqb", [128, 2, 384], BF16)
            wkf = self.sb(st, "wkf", [128, 512], F32)
            wkb = self.sb(st, "wkb", [128, 512], BF16)
            s.dma("sp", wqf[:], self.w_qup[l].rearrange("(k p) n -> p k n", p=128), reads=[self.w_qup], writes=[wqf])
            s.dma("sp", wkf[:], self.w_kvup[l], reads=[self.w_kvup], writes=[wkf])
            for k in range(2):
                self.ts("dve", wqb[:, k, :], wqf[:, k, :], self.gq[:, l, k:k + 1], None, ALU.mult, reads=[wqf, self.gq], writes=[wqb])
            self.ts("dve", wkb[:], wkf[:], self.gkv[:, l:l + 1], None, ALU.mult, reads=[wkf, self.gkv], writes=[wkb])
            ps = Rot([self.psb(st, "psB") for _ in range(6)])
            cq_r = Rot([self.sb(st, "cqb", [128, 2, 512], BF16) for _ in range(2)])
            ckv_r = Rot([self.sb(st, "ckvb", [128, 512], BF16) for _ in range(2)])
            kr_r = Rot([self.sb(st, "krb", [32, 512], BF16) for _ in range(2)])
            cos_r = Rot([self.sb(st, "cosb", [32, 512], F32) for _ in range(2)])
            sin_r = Rot([self.sb(st, "sinb", [32, 512], F32) for _ in range(2)])
            sq_r = Rot([self.sb(st, "sqb", [128, 2, 512], BF16) for _ in range(2)])
            sqk_r = Rot([self.sb(st, "sqk", [128, 512], BF16) for _ in range(2)])
            rq_r = Rot([self.sb(st, "rq", [128, 512], F32) for _ in range(2)])
            rk_r = Rot([self.sb(st, "rk", [128, 512], F32) for _ in range(2)])
            rkt_r = Rot([self.sb(st, "rkt", [128, 4], F32) for _ in range(2)])
            qo_r = Rot([self.sb(st, "qo", [64, 512], BF16) for _ in range(3)])
            xr_r = Rot([self.sb(st, "xr", [32, 512], BF16) for _ in range(3)])
            t1_r = Rot([self.sb(st, "t1", [32, 512], F32) for _ in range(3)])
            t2_r = Rot([self.sb(st, "t2", [32, 512], F32) for _ in range(3)])
            ro_r = Rot([self.sb(st, "ro", [32, 512], BF16) for _ in range(3)])
            vst_r = Rot([self.sb(st, "vst", [128, 4, 4, 65], BF16) for _ in range(2)])
            for v in vst_r.bufs:
                self.ms("dve", v[:, :, :, 64:65], 1.0, [v])

            def rope(xr, cosb, sinb, dsts):
                p = ps.next()
                self.mm(p[0:32, :], self.rotb[:], xr[:], True, True, [self.rotb, xr], [p])
                t1, t2, ro = t1_r.next(), t2_r.next(), ro_r.next()
                self.tt("pool", t1[:], xr[:], cosb[:], ALU.mult, [xr, cosb], [t1])
                self.tt("dve", t2[:], p[0:32, :], sinb[:], ALU.mult, [p, sinb], [t2])
                self.tt("dve", ro[:], t1[:], t2[:], ALU.add, [t1, t2], [ro])
                for dbuf, dap in dsts:
                    s.dma("pool", dap, ro[:], reads=[ro], writes=[dbuf])

            for b in range(NB):
                c0, c1 = b * 512, (b + 1) * 512
                cq, ckv, kr, cosb, sinb = cq_r.next(), ckv_r.next(), kr_r.next(), cos_r.next(), sin_r.next()
                s.dma("sp", cq[:], self.cqT[:, c0:c1].rearrange("(k p) t -> p k t", p=128), reads=[self.cqT], writes=[cq])
                s.dma("sp", ckv[:], self.ckvT[:, c0:c1], reads=[self.ckvT], writes=[ckv])
                s.dma("sp", kr[:], self.kropeT[:, c0:c1], reads=[self.kropeT], writes=[kr])
                s.dma("sp", cosb[:], self.cosT[:, c0:c1], reads=[self.cosT], writes=[cosb])
                s.dma("sp", sinb[:], self.sinT[:, c0:c1], reads=[self.sinT], writes=[sinb])
                sq, sqk, rq, rk, rkt = sq_r.next(), sqk_r.next(), rq_r.next(), rk_r.next(), rkt_r.next()
                self.tt("pool", sq[:], cq[:], cq[:], ALU.mult, [cq], [sq])
                p = ps.next()
                for k in range(2):
                    self.mm(p[:, :], self.onesb[:], sq[:, k, :], k == 0, k == 1, [self.onesb, sq], [p])
                self.act(rq[:], p[:, :], AF.Sqrt, [p, self.epsb], [rq], scale=1.0 / 256, bias=self.epsb[:, 0:1])
                self.recip(rq[:], rq[:], [rq], [rq])
                self.tt("pool", sqk[:], ckv[:], ckv[:], ALU.mult, [ckv], [sqk])
                p2 = ps.next()
                self.mm(p2[:, :], self.onesb[:], sqk[:], True, True, [self.onesb, sqk], [p2])
                self.act(rk[:], p2[:, :], AF.Sqrt, [p2, self.epsb], [rk], scale=1.0 / 128, bias=self.epsb[:, 0:1])
                self.recip(rk[:], rk[:], [rk], [rk])
                p3 = ps.next()
                for tt_ in range(4):
                    self.mm(p3[:, tt_:tt_ + 1], sqk[:, tt_ * 128:(tt_ + 1) * 128], self.onesb[:, 0:1], True, True, [self.onesb, sqk], [p3])
                self.act(rkt[:], p3[:, 0:4], AF.Sqrt, [p3, self.epsb], [rkt], scale=1.0 / 128, bias=self.epsb[:, 0:1])
                self.recip(rkt[:], rkt[:], [rkt], [rkt])
                for h in range(4):
                    pn = ps.next()
                    for k in range(2):
                        self.mm(pn[0:64, :], wqb[:, k, h * 96:h * 96 + 64], cq[:, k, :], k == 0, k == 1, [wqb, cq], [pn])
                    qo = qo_r.next()
                    self.tt("dve", qo[:], pn[0:64, :], rq[0:64, :], ALU.mult, [pn, rq], [qo])
                    s.dma("pool", self.qT[h, 0:64, c0:c1], qo[:], reads=[qo], writes=[self.qT])
                    pr = ps.next()
                    for k in range(2):
                        self.mm(pr[0:32, :], wqb[:, k, h * 96 + 64:h * 96 + 96], cq[:, k, :], k == 0, k == 1, [wqb, cq], [pr])
                    xr = xr_r.next()
                    self.tt("dve", xr[:], pr[0:32, :], rq[0:32, :], ALU.mult, [pr, rq], [xr])
                    rope(xr, cosb, sinb, [(self.qT, self.qT[h, 64:96, c0:c1])])
                for h in range(4):
                    pn = ps.next()
                    self.mm(pn[0:64, :], wkb[:, h * 128:h * 128 + 64], ckv[:], True, True, [wkb, ckv], [pn])
                    qo = qo_r.next()
                    self.tt("dve", qo[:], pn[0:64, :], rk[0:64, :], ALU.mult, [pn, rk], [qo])
                    s.dma("pool", self.kT[h, 0:64, c0:c1], qo[:], reads=[qo], writes=[self.kT])
                rope(kr, cosb, sinb, [(self.kT, self.kT[h, 64:96, c0:c1]) for h in range(4)])
                vst = vst_r.next()
                for tt_ in range(4):
                    pv = ps.next()
                    self.mm(pv[:, :], ckv[:, tt_ * 128:(tt_ + 1) * 128], wkb[:], True, True, [wkb, ckv], [pv])
                    self.act(vst[:, tt_, :, 0:64], pv[:, :].rearrange("p (h c) -> p h c", h=4)[:, :, 64:128], AF.Copy, [pv, rkt], [vst],
                             scale=rkt[:, tt_:tt_ + 1])
                s.dma("pool", self.vaug[c0:c1, :, :].rearrange("(t p) h c -> p t h c", p=128), vst[:], reads=[vst], writes=[self.vaug])

    def attn_finish(self, oacc, obuf_r, oT_r, psT_r, branch, t):
        rec = self.rec_r.next()
        o = obuf_r.next()
        acc3 = oacc[:, 0:260].rearrange("p (h c) -> p h c", h=4)
        self.recip(rec[:, :], acc3[:, :, 64], [oacc], [rec])
        self.tt("dve", o[:].rearrange("p (h c) -> p h c", h=4), acc3[:, :, 0:64],
                rec[:, :].unsqueeze(2).to_broadcast([128, 4, 64]), ALU.mult, [oacc, rec], [o])
        self.to_oT(o, oT_r, psT_r, branch, t)

    def to_oT(self, o, oT_r, psT_r, branch, t):
        s = self.s
        pT = psT_r.next()
        for k in range(2):
            self.tr(pT[:, k * 128:(k + 1) * 128], o[:, k * 128:(k + 1) * 128], [o], [pT])
        oTt = oT_r.next()
        self.act(oTt[:].rearrange("p k q -> p (k q)"), pT[:, 0:256], AF.Copy, [pT], [oTt])
        s.dma("pool", self.oT[branch, :, t * 128:(t + 1) * 128].rearrange("(k p) q -> p k q", p=128), oTt[:], reads=[oTt], writes=[self.oT])

    def phase_C(self, l):
        s, S, NT = self.s, self.S, self.NT
        scale = float(96 ** -0.5)
        with self.scope() as st:
            kT = self.sb(st, "kTs", [96, 4, S], BF16)
            qT = self.sb(st, "qTs", [96, 4, S], BF16)
            va = self.sb(st, "vas", [128, NT, 4, 65], BF16)
            for h in range(4):
                s.dma("sp", kT[:, h, :], self.kT[h], reads=[self.kT], writes=[kT])
                s.dma("sp", qT[:, h, :], self.qT[h], reads=[self.qT], writes=[qT])
            s.dma("sp", va[:], self.vaug[:, :, :].rearrange("(t p) h c -> p t h c", p=128), reads=[self.vaug], writes=[va])
            ps_st = Rot([self.psb(st, "psS") for _ in range(3)])
            ps_o = Rot([self.psb(st, "psO") for _ in range(2)])
            psT_r = Rot([self.psb(st, "psT", BF16) for _ in range(1)])
            pT_r = Rot([self.sb(st, "pT", [128, 512], BF16) for _ in range(3)])
            self.rec_r = Rot([self.sb(st, "rec", [128, 4], F32) for _ in range(2)])
            obuf_r = Rot([self.sb(st, "ob", [128, 256], BF16) for _ in range(2)])
            oT_r = Rot([self.sb(st, "oTt", [128, 2, 128], BF16) for _ in range(2)])
            for t in range(NT):
                oacc = ps_o.next()
                q0, q1 = t * 128, (t + 1) * 128
                def pv(j, pT):
                    for h in range(4):
                        self.mm(oacc[:, h * 65:(h + 1) * 65], pT[:, h * 128:(h + 1) * 128], va[:, j, h, :], (j == 0 and h == 0), j == t, [pT, va], [oacc])

                prev = None
                for j in range(t + 1):
                    k0, k1 = j * 128, (j + 1) * 128
                    sp = ps_st.next()
                    for h in range(4):
                        self.mm(sp[:, h * 128:(h + 1) * 128], kT[:, h, k0:k1], qT[:, h, q0:q1], h == 0, j != t, [kT, qT], [sp])
                    if j == t:
                        for h in range(4):
                            self.mm(sp[:, h * 128:(h + 1) * 128], self.identb[:], self.cmtb[:], False, True, [self.identb, self.cmtb], [sp])
                    pT = pT_r.next()
                    self.act(pT[:], sp[:, :], AF.Exp, [sp], [pT], scale=scale)
                    if prev is not None:
                        pv(*prev)
                    prev = (j, pT)
                pv(*prev)
                self.attn_finish(oacc, obuf_r, oT_r, psT_r, 0, t)

    def phase_E(self, l):
        s, S = self.s, self.S
        cw = self.convw
        with self.scope() as st:
            cb = self.sb(st, "cb", [128, S], BF16)
            cc = self.sb(st, "cc", [128, S], BF16)
            cx = self.sb(st, "cx", [128, S], BF16)
            u = self.sb(st, "u", [128, S + 2], F32)
            y = self.sb(st, "y", [128, S], F32)
            o = self.sb(st, "o", [128, S], BF16)
            for c in range(2):
                r0, r1 = c * 128, (c + 1) * 128
                s.dma("sp", cb[:], self.cbT[r0:r1, :], reads=[self.cbT], writes=[cb])
                s.dma("sp", cc[:], self.ccT[r0:r1, :], reads=[self.ccT], writes=[cc])
                s.dma("sp", cx[:], self.cxT[r0:r1, :], reads=[self.cxT], writes=[cx])
                self.ms("pool", u[:, 0:2], 0.0, [u])
                self.tt("pool", u[:, 2:S + 2], cc[:], cx[:], ALU.mult, [cc, cx], [u])
                self.ts("dve", y[:], u[:, 2:S + 2], cw[:, l, c, 2:3], None, ALU.mult, reads=[u, cw], writes=[y])
                self.stt(y[:], u[:, 1:S + 1], cw[:, l, c, 1:2], y[:], ALU.mult, ALU.add, [u, cw, y], [y])
                self.stt(y[:], u[:, 0:S], cw[:, l, c, 0:1], y[:], ALU.mult, ALU.add, [u, cw, y], [y])
                self.tt("pool", o[:], cb[:], y[:], ALU.mult, [cb, y], [o])
                s.dma("pool", self.oT[1, r0:r1, :], o[:], reads=[o], writes=[self.oT])

    def phase_D(self, l):
        s, S, NT = self.s, self.S, self.NT
        cidx = float(32 ** -0.5 * 8 ** -0.5)
        scale = float(64 ** -0.5)
        mscale = float(96 ** -0.5)
        with self.scope() as st:
            ikT = self.sb(st, "ikTs", [128, S], BF16)
            dkT = self.sb(st, "dkTs", [128, S], BF16)
            dva = self.sb(st, "dvas", [128, NT, 65], BF16)
            iw = self.sb(st, "iws", [128, NT, 8], F32)
            kT = self.sb(st, "kTs", [96, 4, S], BF16)
            va = self.sb(st, "vas", [128, NT, 4, 65], BF16)
            s.dma("sp", ikT[:], self.ikT[:, :], reads=[self.ikT], writes=[ikT])
            s.dma("sp", dkT[:], self.dkT[:, :], reads=[self.dkT], writes=[dkT])
            s.dma("sp", dva[:], self.dvaug[:, :].rearrange("(t p) c -> p t c", p=128), reads=[self.dvaug], writes=[dva])
            s.dma("sp", iw[:], self.iw[:, :].rearrange("(t p) c -> p t c", p=128), reads=[self.iw], writes=[iw])
            for h in range(4):
                s.dma("sp", kT[:, h, :], self.kT[h], reads=[self.kT], writes=[kT])
            s.dma("sp", va[:], self.vaug[:, :, :].rearrange("(t p) h c -> p t h c", p=128), reads=[self.vaug], writes=[va])
            iq_r = Rot([self.sb(st, "iqt", [128, 8, 128], BF16) for _ in range(3)])
            dq_r = Rot([self.sb(st, "dqt", [128, 4, 128], BF16) for _ in range(4)])
            mq_r = Rot([self.sb(st, "mqt", [96, 4, 128], BF16) for _ in range(3)])
            sc_r = [self.sb(st, "sc", [128, S], F32) for _ in range(2)]
            mb_r = [self.sb(st, "mb", [128, S], BF16) for _ in range(2)]
            junk = self.sb(st, "junkd", [128, S], BF16)
            ps_rel = Rot([self.psb(st, "psR") for _ in range(2)])
            ps_sc = Rot([self.psb(st, "psSc") for _ in range(1)])
            ps_st = Rot([self.psb(st, "psS") for _ in range(2)])
            ps_od = Rot([self.psb(st, "psOd") for _ in range(1)])
            ps_om = Rot([self.psb(st, "psOm") for _ in range(1)])
            psT_r = Rot([self.psb(st, "psT", BF16) for _ in range(1)])
            rl_r = Rot([self.sb(st, "rl", [128, 512], BF16) for _ in range(4)])
            dm_r = Rot([self.sb(st, "dm", [128, 8, 128], BF16) for _ in range(2)])
            pT_r = Rot([self.sb(st, "pT", [128, 512], BF16) for _ in range(4)])
            self.rec_r = Rot([self.sb(st, "rec", [128, 4], F32) for _ in range(2)])
            obuf_r = Rot([self.sb(st, "ob", [128, 256], BF16) for _ in range(2)])
            oT_r = Rot([self.sb(st, "oTt", [128, 2, 128], BF16) for _ in range(2)])
            sm = {k: self.sb(st, k, [128, 1], F32) for k in ("w0", "lo", "mid", "cnt", "tmp", "thr")}
            mx_r = Rot([self.sb(st, "mxr", [128, 1], F32) for _ in range(3)])
            lo_r = Rot([self.sb(st, "lor", [128, 1], F32) for _ in range(3)])
            thc_r = Rot([self.sb(st, "thc", [128, 1], F32) for _ in range(2)])
            dq_of = {}
            mm_of = {}

            def score(t):
                q0, q1 = t * 128, (t + 1) * 128
                nk = (t + 1) * 128
                sc = sc_r[t % 2]
                iq = iq_r.next()
                dq = dq_r.next()
                dq_of[t] = dq
                s.dma("sp", iq[:], self.iqT[:, :, q0:q1], reads=[self.iqT], writes=[iq])
                s.dma("sp", dq[:], self.dqT[:, :, q0:q1], reads=[self.dqT], writes=[dq])
                dm = dm_r.next()
                for h in range(8):
                    self.ts("pool", dm[:, h, :], self.identf[:], iw[:, t, h:h + 1], cidx, ALU.mult, ALU.mult, reads=[self.identf, iw], writes=[dm])
                nb = (nk + 511) // 512
                for b in range(nb):
                    k0 = b * 512
                    w = min(512, nk - k0)
                    psc = ps_sc.next()
                    diag = (b == nb - 1)
                    prev = None
                    for h in range(8):
                        pr = ps_rel.next()
                        self.mm(pr[:, 0:w], iq[:, h, :], ikT[:, k0:k0 + w], True, True, [iq, ikT], [pr])
                        rl = rl_r.next()
                        self.act(rl[:, 0:w], pr[:, 0:w], AF.Relu, [pr], [rl])
                        if prev is not None:
                            hh, prl = prev
                            self.mm(psc[:, 0:w], dm[:, hh, :], prl[:, 0:w], hh == 0, False, [dm, prl], [psc])
                        prev = (h, rl)
                    hh, prl = prev
                    self.mm(psc[:, 0:w], dm[:, hh, :], prl[:, 0:w], False, not diag, [dm, prl], [psc])
                    if diag:
                        self.mm(psc[:, w - 128:w], self.identb[:], self.cmqb[:], False, True, [self.identb, self.cmqb], [psc])
                    self.act(sc[:, k0:k0 + w], psc[:, 0:w], AF.Copy, [psc], [sc])
                if self.dsc is not None:
                    s.dma("pool", self.dsc[q0:q1, 0:nk], sc[:, 0:nk], reads=[sc], writes=[self.dsc])

            def bisect(t):
                nk = (t + 1) * 128
                sc, mb = sc_r[t % 2], mb_r[t % 2]
                thr = sm["thr"]
                if t < 2:
                    self.ms("dve", thr[:], -10000.0, [thr])
                else:
                    w0, lo, mid, cnt, tmp = (sm[k] for k in ("w0", "lo", "mid", "cnt", "tmp"))
                    mx = mx_r.next()
                    s.op("dve", "tensor_reduce", (), dict(out=mx[:], in_=sc[:, 0:nk], axis=AX.X, op=ALU.max), [sc], [mx])
                    s.op("dve", "tensor_reduce", (), dict(out=lo[:], in_=sc[:, 0:256], axis=AX.X, op=ALU.min), [sc], [lo])
                    self.tt("dve", w0[:], mx[:], lo[:], ALU.subtract, [mx, lo], [w0])
                    self.ts("dve", w0[:], w0[:], 1.001, 1e-6, ALU.mult, ALU.add, reads=[w0], writes=[w0])
                    for it in range(NBIS):
                        f = float(2.0 ** -(it + 1))
                        self.stt(mid[:], w0[:], f, lo[:], ALU.mult, ALU.add, [w0, lo], [mid])
                        self.ts("dve", junk[:, 0:nk], sc[:, 0:nk], mid[:, 0:1], None, ALU.is_ge, ALU.add, reads=[sc, mid], writes=[junk, cnt], accum_out=cnt[:])
                        self.ts("dve", tmp[:], cnt[:], 255.5, f, ALU.is_ge, ALU.mult, reads=[cnt], writes=[tmp])
                        self.stt(lo[:], tmp[:], w0[:, 0:1], lo[:], ALU.mult, ALU.add, [tmp, w0, lo], [lo])
                    self.cp("dve", thr[:], lo[:], [lo], [thr])
                thc = thc_r.next()
                self.cp("dve", thc[:], thr[:], [thr], [thc])
                self.ts("pool", mb[:, 0:nk], sc[:, 0:nk], thc[:, 0:1], -30000.0, ALU.is_lt, ALU.mult, reads=[sc, thc], writes=[mb])

            def attend(t):
                mb = mb_r[t % 2]
                dq = dq_of.pop(t)
                oacc = ps_od.next()

                def pv(j, pT):
                    for h in range(4):
                        self.mm(oacc[:, h * 65:(h + 1) * 65], pT[:, h * 128:(h + 1) * 128], dva[:, j, :], (j == 0 and h == 0), j == t, [pT, dva], [oacc])

                prev = None
                for j in range(t + 1):
                    k0, k1 = j * 128, (j + 1) * 128
                    sp = ps_st.next()
                    self.mm(sp[:, :], dkT[:, k0:k1], dq[:, :, :].rearrange("d h q -> d (h q)"), True, False, [dkT, dq], [sp])
                    self.mm(sp[:, :], mb[:, k0:k1], self.i4b[:], False, True, [mb, self.i4b], [sp])
                    pT = pT_r.next()
                    self.act(pT[:], sp[:, :], AF.Exp, [sp], [pT], scale=scale)
                    if prev is not None:
                        pv(*prev)
                    prev = (j, pT)
                pv(*prev)
                self.attn_finish(oacc, obuf_r, oT_r, psT_r, 2, t)

            def attend_mla(t):
                q0, q1 = t * 128, (t + 1) * 128
                mq = mq_r.next()
                s.dma("sp", mq[:], self.qT[:, :, q0:q1].rearrange("h d q -> d h q"), reads=[self.qT], writes=[mq])
                oacc = ps_om.next()

                def pv(j, pT):
                    for h in range(4):
                        self.mm(oacc[:, h * 65:(h + 1) * 65], pT[:, h * 128:(h + 1) * 128], va[:, j, h, :], (j == 0 and h == 0), j == t, [pT, va], [oacc])

                prev = None
                for j in range(t + 1):
                    k0, k1 = j * 128, (j + 1) * 128
                    sp = ps_st.next()
                    for h in range(4):
                        self.mm(sp[:, h * 128:(h + 1) * 128], kT[:, h, k0:k1], mq[:, h, :], h == 0, j != t, [kT, mq], [sp])
                    if j == t:
                        for h in range(4):
                            self.mm(sp[:, h * 128:(h + 1) * 128], self.identb[:], self.cmtb[:], False, True, [self.identb, self.cmtb], [sp])
                    pT = pT_r.next()
                    self.act(pT[:], sp[:, :], AF.Exp, [sp], [pT], scale=mscale)
                    if prev is not None:
                        pv(*prev)
                    prev = (j, pT)
                pv(*prev)
                self.attn_finish(oacc, obuf_r, oT_r, psT_r, 0, t)

            score(0)
            if NT > 1:
                score(1)
            bisect(0)
            for t in range(NT):
                if t + 2 < NT:
                    score(t + 2)
                if t + 1 < NT:
                    bisect(t + 1)
                attend(t)
                attend_mla(t)

    def phase_F(self, l):
        s, S, NT = self.s, self.S, self.NT
        with self.scope() as st:
            gqT = self.sb(st, "gqTs", [32, 4, S], BF16)
            gkT = self.sb(st, "gkTs", [32, 4, S], BF16)
            gk = self.sb(st, "gks", [128, NT, 128], BF16)
            gv = self.sb(st, "gvs", [128, NT, 256], BF16)
            grs = self.sb(st, "grss", [128, NT, 256], BF16)
            glrT = self.sb(st, "glrTs", [17, S], BF16)
            wgf = self.sb(st, "wgf", [17, 128], F32)
            wgb = self.sb(st, "wgb", [17, 128], BF16)
            ngb = self.sb(st, "ngb", [128, 64], F32)
            s.dma("sp", gqT[:], self.gqT[:, :, :], reads=[self.gqT], writes=[gqT])
            s.dma("sp", gkT[:], self.gkT[:, :, :], reads=[self.gkT], writes=[gkT])
            s.dma("sp", gk[:], self.gk[:, :].rearrange("(t p) c -> p t c", p=128), reads=[self.gk], writes=[gk])
            s.dma("sp", gv[:], self.gv[:, :].rearrange("(t p) c -> p t c", p=128), reads=[self.gv], writes=[gv])
            s.dma("sp", grs[:], self.grs[:, :].rearrange("(t p) c -> p t c", p=128), reads=[self.grs], writes=[grs])
            s.dma("sp", glrT[:], self.glrT[:, :], reads=[self.glrT], writes=[glrT])
            s.dma("sp", wgf[0:16, :], self.gla_wg[l], reads=[self.gla_wg], writes=[wgf])
            s.dma("sp", wgf[16:17, :], self.gla_bg[l], reads=[self.gla_bg], writes=[wgf])
            s.dma("sp", ngb[:], self.gla_ng[l].partition_broadcast(128), reads=[self.gla_ng], writes=[ngb])
            self.cp("dve", wgb[:], wgf[:], [wgf], [wgb])
            state = self.sb(st, "state", [32, 4, 64], F32)
            stateb = self.sb(st, "stateb", [32, 4, 64], BF16)
            self.ms("dve", state[:], 0.0, [state])
            self.ms("dve", stateb[:], 0.0, [stateb])
            ps_z = Rot([self.psb(st, "psZ") for _ in range(1)])
            ps_c = Rot([self.psb(st, "psC") for _ in range(2)])
            ps_a = Rot([self.psb(st, "psA") for _ in range(1)])
            ps_o = Rot([self.psb(st, "psO") for _ in range(2)])
            ps_u = Rot([self.psb(st, "psU") for _ in range(1)])
            psT_r = Rot([self.psb(st, "psT", BF16) for _ in range(1)])
            ez_r = Rot([self.sb(st, "ez", [128, 128], F32) for _ in range(2)])
            sp_r = Rot([self.sb(st, "sp", [128, 128], F32) for _ in range(2)])
            ec_r = Rot([self.sb(st, "ec", [32, 4, 128], F32) for _ in range(2)])
            en_r = Rot([self.sb(st, "en", [32, 4, 128], F32) for _ in range(2)])
            er_r = Rot([self.sb(st, "er", [128, 128], F32) for _ in range(2)])
            qt_r = Rot([self.sb(st, "qt", [32, 4, 128], BF16) for _ in range(2)])
            kt_r = Rot([self.sb(st, "kt", [32, 4, 128], BF16) for _ in range(2)])
            kh_r = Rot([self.sb(st, "kh", [128, 128], BF16) for _ in range(2)])
            am_r = Rot([self.sb(st, "am", [128, 4, 128], BF16) for _ in range(2)])
            on_r = Rot([self.sb(st, "on", [128, 4, 64], F32) for _ in range(2)])
            og_r = Rot([self.sb(st, "og", [128, 256], BF16) for _ in range(2)])
            ss_r = Rot([self.sb(st, "ssg", [128, 4], F32) for _ in range(2)])
            junk = self.sb(st, "junkg", [128, 64], F32)
            oT_r = Rot([self.sb(st, "oTt", [128, 2, 128], BF16) for _ in range(2)])
            qscale = float(32 ** -0.5)
            def pre(t):
                c0, c1 = t * 128, (t + 1) * 128
                pz = ps_z.next()
                self.mm(pz[:, 0:128], glrT[:, c0:c1], wgb[:], True, True, [glrT, wgb], [pz])
                ez, sp = ez_r.next(), sp_r.next()
                self.act(ez[:], pz[:, 0:128], AF.Exp, [pz], [ez], scale=-1.0)
                self.act(sp[:], ez[:], AF.Ln, [ez], [sp], bias=1.0)
                pc = ps_c.next()
                for h in range(4):
                    self.mm(pc[0:32, h * 128:(h + 1) * 128], sp[:, h * 32:(h + 1) * 32], self.tri[:], True, True, [sp, self.tri], [pc])
                prv = ps_c.next()
                self.mm(prv[:, 0:128], self.rev[:], sp[:], True, True, [sp, self.rev], [prv])
                ec, en, er = ec_r.next(), en_r.next(), er_r.next()
                self.act(ec[:].rearrange("p h i -> p (h i)"), pc[0:32, :], AF.Exp, [pc], [ec])
                self.act(en[:].rearrange("p h i -> p (h i)"), pc[0:32, :], AF.Exp, [pc], [en], scale=-1.0)
                self.act(er[:], prv[:, 0:128], AF.Exp, [prv], [er])
                qt, kt, kh = qt_r.next(), kt_r.next(), kh_r.next()
                self.stt(qt[:], gqT[:, :, c0:c1], qscale, ec[:], ALU.mult, ALU.mult, [gqT, ec], [qt])
                self.tt("pool", kt[:], gkT[:, :, c0:c1], en[:], ALU.mult, [gkT, en], [kt])
                self.tt("pool", kh[:], gk[:, t, :], er[:], ALU.mult, [gk, er], [kh])
                pa = ps_a.next()
                for h in range(4):
                    self.mm(pa[:, h * 128:(h + 1) * 128], kt[:, h, :], qt[:, h, :], True, True, [kt, qt], [pa])
                am = am_r.next()
                self.tt("dve", am[:], pa[:, :].rearrange("p (h i) -> p h i", h=4), self.gmaskb[:].unsqueeze(1).to_broadcast([128, 4, 128]),
                        ALU.mult, [pa, self.gmaskb], [am])
                return ec, qt, kh, am

            def rec(t, ec, qt, kh, am):
                po = ps_o.next()
                for h in range(4):
                    self.mm(po[:, h * 64:(h + 1) * 64], am[:, h, :], gv[:, t, h * 64:(h + 1) * 64], h == 0, False, [am, gv], [po])
                for c in range(2):
                    i0, i1 = c * 64, (c + 1) * 64
                    for h in range(4):
                        self.mm(po[i0:i1, h * 64:(h + 1) * 64], qt[:, h, i0:i1], stateb[:, h, :], False, True, [qt, stateb], [po])
                    pu = ps_u.next()
                    for h in range(4):
                        self.mm(pu[0:32, h * 64:(h + 1) * 64], kh[i0:i1, h * 32:(h + 1) * 32], gv[i0:i1, t, h * 64:(h + 1) * 64], True, True, [kh, gv], [pu])
                    self.tt("dve", state[:], state[:], ec[:, :, i1 - 1:i1].to_broadcast([32, 4, 64]), ALU.mult, [state, ec], [state])
                    self.tt("dve", state[:], state[:], pu[0:32, 0:256].rearrange("p (h v) -> p h v", h=4), ALU.add, [state, pu], [state])
                    self.cp("dve", stateb[:], state[:], [state], [stateb])
                ss, on, og = ss_r.next(), on_r.next(), og_r.next()
                for h in range(4):
                    self.act(junk[:], po[:, h * 64:(h + 1) * 64], AF.Square, [po], [junk, ss], accum_out=ss[:, h:h + 1])
                self.act(ss[:], ss[:], AF.Sqrt, [ss, self.epsb], [ss], scale=1.0 / 64, bias=self.epsb[:, 0:1])
                self.recip(ss[:], ss[:], [ss], [ss])
                self.tt("dve", on[:], po[:, 0:256].rearrange("p (h v) -> p h v", h=4), ss[:].unsqueeze(2).to_broadcast([128, 4, 64]),
                        ALU.mult, [po, ss], [on])
                self.tt("pool", on[:], on[:], ngb[:].unsqueeze(1).to_broadcast([128, 4, 64]), ALU.mult, [on, ngb], [on])
                self.tt("pool", og[:], on[:].rearrange("p h v -> p (h v)"), grs[:, t, :], ALU.mult, [on, grs], [og])
                self.to_oT(og, oT_r, psT_r, 3, t)

            nxt = pre(0)
            for t in range(NT):
                cur = nxt
                if t + 1 < NT:
                    nxt = pre(t + 1)
                rec(t, *cur)

    def phase_G(self, l):
        s, S, NT, NB = self.s, self.S, self.NT, self.NB
        src = self.x_in if l == 0 else self.xres
        with self.scope() as st:
            wbb = self.sb(st, "wbb", [128, 4, 2, D], BF16)
            wob = self.sb(st, "wob", [128, 8, D], BF16)
            wgt = self.sb(st, "wgt", [128, 8, 4096], BF16)
            for n in range(4):
                s.dma("pool", wbb[:, n, :, :], self.w_br[l, n].rearrange("(k p) c -> p k c", p=128), reads=[self.w_br], writes=[wbb])
            for k in range(8):
                s.dma("pool", wgt[:, k, :], self.w_in[l, k * 128:(k + 1) * 128, C_GATE:C_GATE + 4096], reads=[self.w_in], writes=[wgt])
            for kk in range(4):
                s.dma("pool", wob[:, 2 * kk:2 * kk + 2, :], self.w_out[l, kk * 256:(kk + 1) * 256, :].rearrange("(k p) c -> p k c", p=128),
                      reads=[self.w_out], writes=[wob])
            psy = Rot([self.psb(st, "psGy") for _ in range(2)])
            psg = Rot([self.psb(st, "psGg") for _ in range(3)])
            psx = Rot([self.psb(st, "psGx") for _ in range(2)])
            ob_r = Rot([self.sb(st, "obk", [128, 4, 2, 512], BF16) for _ in range(2)])
            hb_r = Rot([self.sb(st, "hbk", [128, 8, 512], BF16) for _ in range(2)])
            gs_r = Rot([self.sb(st, "gsb", [128, 512], BF16) for _ in range(3)])
            mf_r = Rot([self.sb(st, "mf", [128, 512], F32) for _ in range(2)])
            tf_r = Rot([self.sb(st, "tf", [128, 512], F32) for _ in range(3)])
            mT_r = Rot([self.sb(st, "mT", [128, 8, 512], BF16) for _ in range(2)])
            xt_r = Rot([self.sb(st, "xtg", [128, D], F32) for _ in range(2)])
            xn_r = Rot([self.sb(st, "xng", [128, D], F32) for _ in range(2)])

            def load_blk(b):
                c0, c1 = b * 512, (b + 1) * 512
                ob, hb = ob_r.next(), hb_r.next()
                for n in range(4):
                    s.dma("sp", ob[:, n, :, :], self.oT[n, :, c0:c1].rearrange("(k p) t -> p k t", p=128), reads=[self.oT], writes=[ob])
                s.dma("sp", hb[:], self.hTd[:, c0:c1].rearrange("(k p) t -> p k t", p=128), reads=[self.hTd], writes=[hb])
                return ob, hb

            nxt = load_blk(0)
            for b in range(NB):
                c0, c1 = b * 512, (b + 1) * 512
                ob, hb = nxt
                if b + 1 < NB:
                    nxt = load_blk(b + 1)
                mT = mT_r.next()
                for cg in range(8):
                    mf = mf_r.next()
                    for n in range(4):
                        g = n * 8 + cg
                        pg = psg.next()
                        for k in range(8):
                            self.mm(pg[:, :], wgt[:, k, g * 128:(g + 1) * 128], hb[:, k, :], k == 0, k == 7, [wgt, hb], [pg])
                        p = psy.next()
                        for k in range(2):
                            self.mm(p[:, :], wbb[:, n, k, cg * 128:(cg + 1) * 128], ob[:, n, k, :], k == 0, k == 1, [wbb, ob], [p])
                        gs = gs_r.next()
                        self.act(gs[:], pg[:, :], AF.Sigmoid, [pg], [gs])
                        if n == 0:
                            self.tt("dve", mf[:], p[:, :], gs[:], ALU.mult, [p, gs], [mf])
                        else:
                            tf = tf_r.next()
                            self.tt("dve", tf[:], p[:, :], gs[:], ALU.mult, [p, gs], [tf])
                            if n < 3:
                                self.tt("pool", mf[:], mf[:], tf[:], ALU.add, [mf, tf], [mf])
                            else:
                                self.tt("pool", mT[:, cg, :], mf[:], tf[:], ALU.add, [mf, tf], [mT])
                for tt_ in range(4):
                    r0 = c0 + tt_ * 128
                    xt, xn = xt_r.next(), xn_r.next()
                    s.dma("sp", xt[:], src[r0:r0 + 128, :], reads=[src], writes=[xt])
                    for half in range(2):
                        p = psx.next()
                        for cg in range(8):
                            self.mm(p[:, :], mT[:, cg, tt_ * 128:(tt_ + 1) * 128], wob[:, cg, half * 512:(half + 1) * 512], cg == 0, cg == 7, [mT, wob], [p])
                        self.tt("dve", xn[:, half * 512:(half + 1) * 512], p[:, :], xt[:, half * 512:(half + 1) * 512], ALU.add, [p, xt], [xn])
                    s.dma("pool", self.xres[r0:r0 + 128, :], xn[:], reads=[xn], writes=[self.xres])

    def phase_H(self, l):
        s, S = self.s, self.S
        SB = 1024
        NG = DFF // 128
        with self.scope() as st:
            hT = self.sb(st, "h2T", [128, 8, SB], BF16)
            aT = self.sb(st, "aT", [128, NG, SB], BF16)
            nt = self.norm_tiles(st)
            ps = Rot([self.psb(st, "psH") for _ in range(4)])
            wgb_r = Rot([self.sb(st, "wgbf", [128, 8, 128], BF16) for _ in range(3)])
            wub_r = Rot([self.sb(st, "wubf", [128, 8, 128], BF16) for _ in range(3)])
            wdb_r = Rot([self.sb(st, "wdb", [128, NG, 512], BF16) for _ in range(2)])
            sg_r = Rot([self.sb(st, "sg", [128, 512], BF16) for _ in range(3)])
            xt_r = Rot([self.sb(st, "xth", [128, 512], F32) for _ in range(3)])
            xn_r = Rot([self.sb(st, "xnh", [128, 512], F32) for _ in range(3)])

            def load_gu(g):
                wgb, wub = wgb_r.next(), wub_r.next()
                s.dma("pool", wgb[:], self.w_fg[l, :, g * 128:(g + 1) * 128].rearrange("(k p) n -> p k n", p=128), reads=[self.w_fg], writes=[wgb])
                s.dma("pool", wub[:], self.w_fu[l, :, g * 128:(g + 1) * 128].rearrange("(k p) n -> p k n", p=128), reads=[self.w_fu], writes=[wub])
                return wgb, wub

            def load_d(cq):
                wdb = wdb_r.next()
                for kk in range(2):
                    s.dma("pool", wdb[:, kk * 11:(kk + 1) * 11, :], self.w_fd[l, kk * 1408:(kk + 1) * 1408, cq * 512:(cq + 1) * 512].rearrange("(k p) n -> p k n", p=128),
                          reads=[self.w_fd], writes=[wdb])
                return wdb

            for sbk in range(S // SB):
                t0 = sbk * SB
                self.norm_to_hT(nt, self.xres, self.gffn, l, hT, t0, SB // 128)
                nxt = load_gu(0)
                for g in range(NG):
                    wgb, wub = nxt
                    if g + 1 < NG:
                        nxt = load_gu(g + 1)
                    for b in range(SB // 512):
                        pg, pu = ps.next(), ps.next()
                        for k in range(8):
                            self.mm(pg[:, :], wgb[:, k, :], hT[:, k, b * 512:(b + 1) * 512], k == 0, k == 7, [wgb, hT], [pg])
                        for k in range(8):
                            self.mm(pu[:, :], wub[:, k, :], hT[:, k, b * 512:(b + 1) * 512], k == 0, k == 7, [wub, hT], [pu])
                        sg = sg_r.next()
                        self.act(sg[:], pg[:, :], AF.Silu, [pg], [sg])
                        self.tt("dve", aT[:, g, b * 512:(b + 1) * 512], pu[:, :], sg[:], ALU.mult, [pu, sg], [aT])
                nxtd = load_d(0)
                for cq in range(2):
                    wdb = nxtd
                    if cq + 1 < 2:
                        nxtd = load_d(cq + 1)
                    for tt_ in range(SB // 128):
                        r0 = t0 + tt_ * 128
                        xt, xn = xt_r.next(), xn_r.next()
                        s.dma("sp", xt[:], self.xres[r0:r0 + 128, cq * 512:(cq + 1) * 512], reads=[self.xres], writes=[xt])
                        p = ps.next()
                        for k in range(NG):
                            self.mm(p[:, :], aT[:, k, tt_ * 128:(tt_ + 1) * 128], wdb[:, k, :], k == 0, k == NG - 1, [aT, wdb], [p])
                        self.tt("dve", xn[:], p[:, :], xt[:], ALU.add, [p, xt], [xn])
                        s.dma("pool", self.xres[r0:r0 + 128, cq * 512:(cq + 1) * 512], xn[:], reads=[xn], writes=[self.xres])

    def phase_final(self):
        s, S, NT = self.s, self.S, self.NT
        with self.scope() as st:
            gf = self.sb(st, "gf", [128, D], F32)
            s.dma("sp", gf[:], self.g_final[:].partition_broadcast(128), reads=[self.g_final], writes=[gf])
            xt_r = Rot([self.sb(st, "xtf", [128, D], F32) for _ in range(3)])
            yo_r = Rot([self.sb(st, "yof", [128, D], F32) for _ in range(3)])
            junk = self.sb(st, "junkf", [128, D], BF16)
            ss_r = Rot([self.sb(st, "ssf", [128, 1], F32) for _ in range(3)])
            for t in range(NT):
                xt, yo, ss = xt_r.next(), yo_r.next(), ss_r.next()
                s.dma("sp", xt[:], self.xres[t * 128:(t + 1) * 128, :], reads=[self.xres], writes=[xt])
                self.act(junk[:], xt[:], AF.Square, [xt], [junk, ss], accum_out=ss[:])
                self.act(ss[:], ss[:], AF.Sqrt, [ss, self.epsb], [ss], scale=1.0 / D, bias=self.epsb[:, 0:1])
                self.recip(ss[:], ss[:], [ss], [ss])
                self.stt(yo[:], xt[:], ss[:, 0:1], gf[:], ALU.mult, ALU.mult, [xt, ss, gf], [yo])
                s.dma("pool", self.y_out[t * 128:(t + 1) * 128, :], yo[:], reads=[yo], writes=[self.y_out])


def make_in_maps(inputs, S, batch_ids):
    f = lambda a: np.ascontiguousarray(np.asarray(a))
    consts = host_consts()
    shared = {
        "w_in": f(inputs["w_in"]), "w_qup": f(inputs["mla_w_q_up"]), "w_kvup": f(inputs["mla_w_kv_up"]),
        "w_br": f(inputs["w_branch"]), "w_out": f(inputs["w_out"]), "w_fg": f(inputs["w_ffn_gate"]),
        "w_fu": f(inputs["w_ffn_up"]), "w_fd": f(inputs["w_ffn_down"]),
        "g_attn": f(np.asarray(inputs["attn_norm_g"]).reshape(NLAYER, 8, 128).transpose(2, 0, 1)),
        "g_ffn": f(np.asarray(inputs["ffn_norm_g"]).reshape(NLAYER, 8, 128).transpose(2, 0, 1)),
        "g_final": f(inputs["final_norm_g"]),
        "g_q": f(np.asarray(inputs["mla_q_norm_g"]).reshape(NLAYER, 2, 128).transpose(2, 0, 1)),
        "g_kv": f(np.asarray(inputs["mla_kv_norm_g"]).transpose(1, 0)),
        "conv_w": f(np.asarray(inputs["conv_w"]).reshape(NLAYER, 3, 2, 128).transpose(3, 0, 2, 1)),
        "gla_wg": f(inputs["gla_w_gate_up"]),
        "gla_bg": f(np.asarray(inputs["gla_b_gate"]).reshape(NLAYER, 1, 128)),
        "gla_ng": f(inputs["gla_norm_g"]),
    }
    shared.update(consts)
    maps = []
    x = np.asarray(inputs["x"])
    pos = np.asarray(inputs["positions"])
    for b in batch_ids:
        m = dict(shared)
        m["x"] = f(x[b, :S])
        m["pos"] = f(pos[b, :S].astype(np.int32))
        maps.append(m)
    return maps


_NC_CACHE = {}


def kernel(**inputs):
    S = 4096
    key = ("full", S)
    if key not in _NC_CACHE:
        _NC_CACHE[key] = Builder(S, NLAYER).build()
    nc = _NC_CACHE[key]
    in_maps = make_in_maps(inputs, S, list(range(8)))
    res = run_bass_kernel_spmd(nc, in_maps, core_ids=list(range(8)))
    out = np.stack([np.asarray(r["y"]) for r in res.results], axis=0).astype(np.float32)
    return out
```

```python
import numpy as np
from contextlib import ExitStack, contextmanager
import concourse.bass as bass
import concourse.mybir as mybir
from concourse.bass_utils import run_bass_kernel_spmd

F32 = mybir.dt.float32
BF16 = mybir.dt.bfloat16
I32 = mybir.dt.int32
AF = mybir.ActivationFunctionType
ALU = mybir.AluOpType
AX = mybir.AxisListType

D = 1024
DFF = 2816
NLAYER = 4
INTOT = 6744
EPS = 1e-6
EPOCH = 30000
NBIS = 12

C_CQ, C_CKV, C_KROPE = 0, 256, 384
C_CB, C_CC, C_CX = 416, 672, 928
C_DQ, C_DK, C_DV = 1184, 1440, 1504
C_IQ, C_IK, C_IW = 1568, 1824, 1856
C_GQ, C_GK, C_GV, C_GLR, C_GR = 1864, 1992, 2120, 2376, 2392
C_GATE = 2648


class Buf:
    def __init__(self, name, t=None):
        self.name = name
        self.t = t
        self.last_write = None
        self.readers = {}
        self.wsem = None
        self.rsem = None

    def __getitem__(self, idx):
        return self.t[idx]


class Sched:
    COMPUTE = ("pe", "act", "dve", "pool")
    SEMCAP = 30000

    def __init__(self, nc):
        self.nc = nc
        self.engs = list(self.COMPUTE) + ["sp"]
        self.ops = {e: [] for e in self.engs}
        self.cnt = {e: 0 for e in self.COMPUTE}
        self.sems = {}
        self.sem_names = []
        self.waited = {e: {} for e in self.engs}
        self.nops = 0
        self.semval = {}
        self.free_dma = []
        self.ndma_sems = 0

    def _sem(self, name):
        if name not in self.sems:
            self.sems[name] = None
            self.sem_names.append(name)
        return name

    def _dma_sem(self):
        if self.free_dma:
            return self.free_dma.pop()
        self.ndma_sems += 1
        name = self._sem(f"d_{self.ndma_sems}")
        self.semval[name] = 0
        return name

    def release(self, bufs):
        for b in bufs:
            for nm in (b.wsem, b.rsem):
                if nm is not None and self.semval[nm] < self.SEMCAP:
                    self.free_dma.append(nm)
            b.wsem = b.rsem = None

    def _compute_token(self, e):
        self.cnt[e] += 1
        k = self.cnt[e]
        ep = (k - 1) // EPOCH
        return (self._sem(f"c_{e}_{ep}"), (k - 1) % EPOCH + 1, e)

    def _need(self, e, tok, waits):
        if tok is None:
            return
        sem, val, src = tok
        if src == e and e == "pe":
            return
        w = self.waited[e]
        if w.get(sem, 0) >= val:
            return
        w[sem] = val
        waits.append((sem, val))

    def _deps(self, e, reads, writes):
        waits = []
        for b in reads:
            self._need(e, b.last_write, waits)
        for b in writes:
            self._need(e, b.last_write, waits)
            for r in b.readers.values():
                self._need(e, r, waits)
        return waits

    def _mark(self, tok, reads, writes):
        for b in reads:
            old = b.readers.get(tok[0])
            if old is None or old[1] < tok[1]:
                b.readers[tok[0]] = tok
        for b in writes:
            b.last_write = tok
            b.readers = {}

    def op(self, e, method, args, kwargs, reads=(), writes=()):
        waits = self._deps(e, reads, writes)
        tok = self._compute_token(e)
        self._mark(tok, reads, writes)
        self.ops[e].append((waits, method, args, kwargs, (tok[0], 1)))
        self.nops += 1
        return tok

    def dma(self, q, out_ap, in_ap, reads=(), writes=(), **kw):
        waits = self._deps(q, reads, writes)
        if writes:
            b = writes[0]
            if b.wsem is None:
                b.wsem = self._dma_sem()
            nm = b.wsem
        else:
            b = reads[0]
            if b.rsem is None:
                b.rsem = self._dma_sem()
            nm = b.rsem
        self.semval[nm] += 16
        tok = (nm, self.semval[nm], "dma")
        self._mark(tok, reads, writes)
        kw = dict(kw)
        kw["out"] = out_ap
        kw["in_"] = in_ap
        self.ops[q].append((waits, "dma_start", (), kw, (nm, 16)))
        self.nops += 1
        return tok

    def wait_all(self, e, bufs):
        waits = self._deps(e, (), bufs)
        if waits:
            self.ops[e].append((waits, None, None, None, None))

    def barrier(self):
        toks = []
        for e in self.COMPUTE:
            k = self.cnt[e]
            if k > 0:
                ep = (k - 1) // EPOCH
                toks.append((f"c_{e}_{ep}", (k - 1) % EPOCH + 1, e))
        for nm, v in self.semval.items():
            if v > 0:
                toks.append((nm, v, "dma"))
        for e in self.engs:
            waits = []
            for tok in toks:
                self._need(e, tok, waits)
            if waits:
                self.ops[e].append((waits, None, None, None, None))

    def emit(self, stack):
        nc = self.nc
        if not hasattr(self, "sem_stack"):
            self.sem_stack = stack
        for name in self.sem_names:
            if self.sems[name] is None:
                self.sems[name] = self.sem_stack.enter_context(nc.semaphore(name))
        if not any(self.ops[e] for e in self.engs):
            return
        with nc.Block() as block:
            amap = {"pe": block.tensor, "act": block.scalar, "dve": block.vector,
                    "pool": block.gpsimd, "sp": block.sync}
            for e in self.engs:
                ops = self.ops[e]
                if not ops:
                    continue

                def body(eng, ops=ops):
                    for waits, method, args, kwargs, inc in ops:
                        for sem, val in waits:
                            eng.wait_ge(self.sems[sem], val)
                        if method is not None:
                            ins = getattr(eng, method)(*args, **kwargs)
                            ins.then_inc(self.sems[inc[0]], inc[1])

                amap[e](body)
                self.ops[e] = []


class Rot:
    def __init__(self, bufs):
        self.bufs = bufs
        self.i = 0

    def next(self):
        b = self.bufs[self.i % len(self.bufs)]
        self.i += 1
        return b


def host_consts():
    j = np.arange(128)[:, None]
    i = np.arange(128)[None, :]
    same = (j // 64) == (i // 64)
    c = {}
    c["c_ident"] = np.eye(128, dtype=np.float32)
    c["c_tri"] = np.where(same & (j <= i), -1.0 / 16.0, 0.0).astype(np.float32)
    c["c_rev"] = np.where(same & (j > i), -1.0 / 16.0, 0.0).astype(np.float32)
    c["c_gmask"] = np.where(same & (j <= i), 1.0, 0.0).astype(np.float32)
    c["c_cmt"] = np.where(j > i, -30000.0, 0.0).astype(np.float32)
    c["c_cmq"] = np.ascontiguousarray(c["c_cmt"].T)
    rot = np.zeros((32, 32), np.float32)
    for fp in range(16):
        rot[fp + 16, fp] = -1.0
    for fp in range(16, 32):
        rot[fp - 16, fp] = 1.0
    c["c_rot"] = rot
    half = 16
    invf = (np.float32(10000.0) ** (-np.arange(half, dtype=np.float32) / np.float32(half))).astype(np.float32)
    c["c_invf"] = np.concatenate([invf, invf]).reshape(32, 1).astype(np.float32)
    return c


class Builder:
    def __init__(self, S, nl, dbg=(), stop_after=None, phases=None):
        self.S = S
        self.NT = S // 128
        self.NB = S // 512
        self.nl = nl
        self.dbg = set(dbg)
        self.stop_after = stop_after
        self.phases = phases
        self.nc = bass.Bass("TRN2", target_bir_lowering=False)
        self.s = Sched(self.nc)
        self.uid = 0
        self.live = []

    def din(self, name, shape, dt=F32):
        return self.nc.dram_tensor(name, list(shape), dt, kind="ExternalInput").ap()

    def dscr(self, name, shape, dt=BF16):
        kind = "ExternalOutput" if name in self.dbg else "Internal"
        ap = self.nc.dram_tensor(name, list(shape), dt, kind=kind).ap()
        return Buf(name, ap)

    def sb(self, st, name, shape, dt):
        self.uid += 1
        nm = f"{name}_{self.uid}"
        b = Buf(nm, st.enter_context(self.nc.sbuf_tensor(nm, list(shape), dt)))
        self.live.append(b)
        return b

    def psb(self, st, name, dt=F32):
        self.uid += 1
        nm = f"{name}_{self.uid}"
        shape = [128, 512] if dt == F32 else [128, 1024]
        b = Buf(nm, st.enter_context(self.nc.psum_tensor(nm, shape, dt)))
        self.live.append(b)
        return b

    @contextmanager
    def scope(self):
        st = ExitStack()
        mark = len(self.live)
        yield st
        self.s.barrier()
        self.s.release(self.live[mark:])
        del self.live[mark:]
        self.s.emit(self.top)
        st.close()

    def mm(self, out, lhsT, rhs, start, stop, reads, writes):
        self.s.op("pe", "matmul", (out,), dict(lhsT=lhsT, rhs=rhs, start=start, stop=stop), reads, writes)

    def tr(self, out, in_, reads, writes):
        self.s.op("pe", "transpose", (), dict(out=out, in_=in_, identity=self.identb[:]), list(reads) + [self.identb], writes)

    def act(self, out, in_, func, reads, writes, **kw):
        self.s.op("act", "activation", (), dict(out=out, in_=in_, func=func, **kw), reads, writes)

    def tt(self, eng, out, in0, in1, op, reads, writes):
        self.s.op(eng, "tensor_tensor", (), dict(out=out, in0=in0, in1=in1, op=op), reads, writes)

    def ts(self, eng, out, in0, s1, s2, op0, op1=None, reads=(), writes=(), accum_out=None):
        kw = dict(out=out, in0=in0, scalar1=s1, scalar2=s2, op0=op0)
        if op1 is not None:
            kw["op1"] = op1
        if accum_out is not None:
            kw["accum_out"] = accum_out
        self.s.op(eng, "tensor_scalar", (), kw, reads, writes)

    def stt(self, out, in0, scalar, in1, op0, op1, reads, writes):
        self.s.op("dve", "scalar_tensor_tensor", (), dict(out=out, in0=in0, scalar=scalar, in1=in1, op0=op0, op1=op1), reads, writes)

    def cp(self, eng, out, in_, reads, writes):
        self.s.op(eng, "tensor_copy", (), dict(out=out, in_=in_), reads, writes)

    def ms(self, eng, ap, val, writes):
        self.s.op(eng, "memset", (ap, val), {}, (), writes)

    def recip(self, out, in_, reads, writes):
        self.s.op("dve", "reciprocal", (), dict(out=out, in_=in_), reads, writes)

    def build(self):
        nc, s, S, NT = self.nc, self.s, self.S, self.NT
        self.x_in = Buf("x", self.din("x", [S, D]))
        self.pos = Buf("pos", self.din("pos", [S], I32))
        self.w_in = Buf("w_in", self.din("w_in", [NLAYER, D, INTOT]))
        self.w_qup = Buf("w_qup", self.din("w_qup", [NLAYER, 256, 384]))
        self.w_kvup = Buf("w_kvup", self.din("w_kvup", [NLAYER, 128, 512]))
        self.w_br = Buf("w_br", self.din("w_br", [NLAYER, 4, 256, D]))
        self.w_out = Buf("w_out", self.din("w_out", [NLAYER, D, D]))
        self.w_fg = Buf("w_fg", self.din("w_fg", [NLAYER, D, DFF]))
        self.w_fu = Buf("w_fu", self.din("w_fu", [NLAYER, D, DFF]))
        self.w_fd = Buf("w_fd", self.din("w_fd", [NLAYER, DFF, D]))
        self.g_attn = Buf("g_attn", self.din("g_attn", [128, NLAYER, 8]))
        self.g_ffn = Buf("g_ffn", self.din("g_ffn", [128, NLAYER, 8]))
        self.g_final = Buf("g_final", self.din("g_final", [D]))
        self.g_q = Buf("g_q", self.din("g_q", [128, NLAYER, 2]))
        self.g_kv = Buf("g_kv", self.din("g_kv", [128, NLAYER]))
        self.conv_w = Buf("conv_w", self.din("conv_w", [128, NLAYER, 2, 3]))
        self.gla_wg = Buf("gla_wg", self.din("gla_wg", [NLAYER, 16, 128]))
        self.gla_bg = Buf("gla_bg", self.din("gla_bg", [NLAYER, 1, 128]))
        self.gla_ng = Buf("gla_ng", self.din("gla_ng", [NLAYER, 64]))
        cin = {k: Buf(k, self.din(k, list(v.shape))) for k, v in host_consts().items()}
        self.y_out = Buf("y", nc.dram_tensor("y", [S, D], F32, kind="ExternalOutput").ap())
        self.xres = self.dscr("xres", [S, D], F32)
        self.cosT = self.dscr("cosT", [32, S], F32)
        self.sinT = self.dscr("sinT", [32, S], F32)
        self.cqT = self.dscr("cqT", [256, S])
        self.ckvT = self.dscr("ckvT", [128, S])
        self.kropeT = self.dscr("kropeT", [32, S])
        self.cbT = self.dscr("cbT", [256, S])
        self.ccT = self.dscr("ccT", [256, S])
        self.cxT = self.dscr("cxT", [256, S])
        self.dqT = self.dscr("dqT", [128, 4, S])
        self.dkT = self.dscr("dkT", [128, S])
        self.iqT = self.dscr("iqT", [128, 8, S])
        self.ikT = self.dscr("ikT", [128, S])
        self.gqT = self.dscr("gqT", [32, 4, S])
        self.gkT = self.dscr("gkT", [32, 4, S])
        self.glrT = self.dscr("glrT", [17, S])
        self.hTd = self.dscr("hTd", [1024, S])
        self.dvaug = self.dscr("dvaug", [S, 65])
        self.iw = self.dscr("iw", [S, 8], F32)
        self.gk = self.dscr("gk", [S, 128])
        self.gv = self.dscr("gv", [S, 256])
        self.grs = self.dscr("grs", [S, 256])
        self.qT = self.dscr("qT", [4, 96, S])
        self.kT = self.dscr("kT", [4, 96, S])
        self.vaug = self.dscr("vaug", [S, 4, 65])
        self.oT = self.dscr("oT", [4, 256, S])
        self.dsc = self.dscr("dsc", [S, S], F32) if "dsc" in self.dbg else None

        with ExitStack() as top:
            self.top = top
            cst = {}
            for k in ("c_ident", "c_tri", "c_rev", "c_gmask", "c_cmt", "c_cmq"):
                cst[k] = self.sb(top, k, [128, 128], F32)
                s.dma("sp", cst[k][:], cin[k][:], reads=[cin[k]], writes=[cst[k]])
            self.identf = cst["c_ident"]
            self.tri = cst["c_tri"]
            self.rev = cst["c_rev"]
            self.identb = self.sb(top, "identb", [128, 128], BF16)
            self.gmaskb = self.sb(top, "gmaskb", [128, 128], BF16)
            self.cmtb = self.sb(top, "cmtb", [128, 128], BF16)
            self.cmqb = self.sb(top, "cmqb", [128, 128], BF16)
            self.i4b = self.sb(top, "i4b", [128, 512], BF16)
            self.onesb = self.sb(top, "onesb", [128, 128], BF16)
            self.epsb = self.sb(top, "epsb", [128, 1], F32)
            self.rotb = self.sb(top, "rotb", [32, 32], BF16)
            rotf = self.sb(top, "rotf", [32, 32], F32)
            self.invf = self.sb(top, "invf", [32, 1], F32)
            s.dma("sp", rotf[:], cin["c_rot"][:], reads=[cin["c_rot"]], writes=[rotf])
            s.dma("sp", self.invf[:], cin["c_invf"][:], reads=[cin["c_invf"]], writes=[self.invf])
            self.cp("dve", self.identb[:], self.identf[:], [self.identf], [self.identb])
            self.cp("dve", self.gmaskb[:], cst["c_gmask"][:], [cst["c_gmask"]], [self.gmaskb])
            self.cp("dve", self.cmtb[:], cst["c_cmt"][:], [cst["c_cmt"]], [self.cmtb])
            self.cp("dve", self.cmqb[:], cst["c_cmq"][:], [cst["c_cmq"]], [self.cmqb])
            self.cp("dve", self.rotb[:], rotf[:], [rotf], [self.rotb])
            for h in range(4):
                self.cp("dve", self.i4b[:, h * 128:(h + 1) * 128], self.identf[:], [self.identf], [self.i4b])
            self.ms("dve", self.onesb[:], 1.0, [self.onesb])
            self.ms("dve", self.epsb[:], EPS, [self.epsb])
            self.gattn = self.sb(top, "gattn", [128, NLAYER, 8], F32)
            self.gffn = self.sb(top, "gffn", [128, NLAYER, 8], F32)
            self.gq = self.sb(top, "gq", [128, NLAYER, 2], F32)
            self.gkv = self.sb(top, "gkv", [128, NLAYER], F32)
            self.convw = self.sb(top, "convw", [128, NLAYER, 2, 3], F32)
            for dst, srcb in ((self.gattn, self.g_attn), (self.gffn, self.g_ffn), (self.gq, self.g_q),
                              (self.gkv, self.g_kv), (self.convw, self.conv_w)):
                s.dma("sp", dst[:], srcb[:], reads=[srcb], writes=[dst])
            s.emit(top)

            allph = "RABCEDFGH"
            ph = self.phases if self.phases is not None else allph
            if "R" in ph:
                self.phase_rope()
            done = False
            for l in range(self.nl):
                for name, fn in (("A", self.phase_A), ("B", self.phase_B), ("E", self.phase_E),
                                 ("D", self.phase_D), ("F", self.phase_F), ("G", self.phase_G), ("H", self.phase_H)):
                    if name in ph:
                        fn(l)
                    if self.stop_after == (l, name):
                        done = True
                        break
                if done:
                    break
            if not done:
                self.phase_final()
            outs = [self.y_out] + [getattr(self, n) for n in self.dbg if getattr(self, n, None) is not None]
            s.wait_all("sp", outs)
            s.wait_all("pool", outs)
            s.barrier()
            s.emit(top)
        return nc

    def phase_rope(self):
        s, S = self.s, self.S
        W = min(S, 1024)
        with self.scope() as st:
            pi = self.sb(st, "pi", [32, W], I32)
            pf = self.sb(st, "pf", [32, W], F32)
            pk = self.sb(st, "pk", [32, W], I32)
            pkf = self.sb(st, "pkf", [32, W], F32)
            pc = self.sb(st, "pc", [32, W], F32)
            pa = self.sb(st, "pa", [32, W], F32)
            res = self.sb(st, "res", [32, W], F32)
            for c0 in range(0, S, W):
                s.dma("sp", pi[:], self.pos[c0:c0 + W].partition_broadcast(32), reads=[self.pos], writes=[pi])
                self.cp("dve", pf[:], pi[:], [pi], [pf])
                self.ts("dve", pf[:], pf[:], self.invf[:, 0:1], None, ALU.mult, reads=[pf, self.invf], writes=[pf])
                for shift, dst in ((0.0, self.sinT), (0.25, self.cosT)):
                    self.ts("dve", pa[:], pf[:], float(1.0 / (2 * np.pi)), shift, ALU.mult, ALU.add, reads=[pf], writes=[pa])
                    self.cp("dve", pk[:], pa[:], [pa], [pk])
                    self.cp("dve", pkf[:], pk[:], [pk], [pkf])
                    self.tt("dve", pa[:], pa[:], pkf[:], ALU.subtract, [pa, pkf], [pa])
                    self.ts("dve", pc[:], pa[:], 0.5, None, ALU.is_gt, reads=[pa], writes=[pc])
                    self.tt("dve", pa[:], pa[:], pc[:], ALU.subtract, [pa, pc], [pa])
                    self.ts("dve", pc[:], pa[:], -0.5, None, ALU.is_lt, reads=[pa], writes=[pc])
                    self.tt("dve", pa[:], pa[:], pc[:], ALU.add, [pa, pc], [pa])
                    self.act(res[:], pa[:], AF.Sin, [pa], [res], scale=float(2 * np.pi))
                    s.dma("pool", dst[:, c0:c0 + W], res[:], reads=[res], writes=[dst])

    def norm_tiles(self, st):
        return dict(
            xt=Rot([self.sb(st, "xt", [128, D], F32) for _ in range(3)]),
            xs=Rot([self.sb(st, "xs", [128, D], BF16) for _ in range(2)]),
            junk=self.sb(st, "junk", [128, D], BF16),
            ss=Rot([self.sb(st, "ss", [128, 1], F32) for _ in range(3)]),
            rs=Rot([self.sb(st, "rs", [128, 1], F32) for _ in range(3)]),
            psT=Rot([self.psb(st, "psT", BF16) for _ in range(2)]))

    def norm_to_hT(self, nt, src, g_tile, l, hT, tok0, ntiles):
        s = self.s
        for t in range(ntiles):
            xt, xs, ss, rs = nt["xt"].next(), nt["xs"].next(), nt["ss"].next(), nt["rs"].next()
            junk = nt["junk"]
            r0 = tok0 + t * 128
            s.dma("sp", xt[:], src[r0:r0 + 128, :], reads=[src], writes=[xt])
            self.act(junk[:], xt[:], AF.Square, [xt], [junk, ss], accum_out=ss[:])
            self.act(rs[:], ss[:], AF.Sqrt, [ss, self.epsb], [rs], scale=1.0 / D, bias=self.epsb[:, 0:1])
            self.recip(rs[:], rs[:], [rs], [rs])
            self.ts("dve", xs[:], xt[:], rs[:, 0:1], None, ALU.mult, reads=[xt, rs], writes=[xs])
            pT = nt["psT"].next()
            for k in range(8):
                self.tr(pT[:, k * 128:(k + 1) * 128], xs[:, k * 128:(k + 1) * 128], [xs], [pT])
            self.tt("dve", hT[:, :, t * 128:(t + 1) * 128], pT[:, :].rearrange("p (k q) -> p k q", k=8),
                    g_tile[:, l, :].unsqueeze(2).to_broadcast([128, 8, 128]), ALU.mult, [pT, g_tile], [hT])

    def phase_A(self, l):
        s, S, NT, NB = self.s, self.S, self.NT, self.NB
        src = self.x_in if l == 0 else self.xres
        with self.scope() as st:
            hT = self.sb(st, "hT", [128, 8, S], BF16)
            with self.scope() as st1:
                ones_row = self.sb(st1, "ones_row", [1, S], BF16)
                self.ms("dve", ones_row[:], 1.0, [ones_row])
                s.dma("pool", self.glrT[16:17, :], ones_row[:], reads=[ones_row], writes=[self.glrT])
                nt = self.norm_tiles(st1)
                self.norm_to_hT(nt, src, self.gattn, l, hT, 0, NT)
            ps = Rot([self.psb(st, "psA") for _ in range(4)])
            wbf_r = Rot([self.sb(st, "wbf", [128, 8, 256], BF16) for _ in range(3)])
            stg_r = Rot([self.sb(st, "stg", [128, S], BF16) for _ in range(4)])
            evac = [0]

            def load_w(c0, n):
                wbf = wbf_r.next()
                s.dma("pool", wbf[:, :, 0:n], self.w_in[l, :, c0:c0 + n].rearrange("(k p) n -> p k n", p=128),
                      reads=[self.w_in], writes=[wbf])
                return wbf

            def fm_group(wbf, n, outs, func, lo_outs=None):
                stg = stg_r.next()
                stl = stg_r.next() if lo_outs else None
                for b in range(NB):
                    p = ps.next()
                    for k in range(8):
                        self.mm(p[0:n, :], wbf[:, k, 0:n], hT[:, k, b * 512:(b + 1) * 512], k == 0, k == 7, [wbf, hT], [p])
                    evac[0] += 1
                    if lo_outs:
                        self.act(stg[0:n, b * 512:(b + 1) * 512], p[0:n, :], func, [p], [stg])
                        self.tt("dve", stl[0:n, b * 512:(b + 1) * 512], p[0:n, :], stg[0:n, b * 512:(b + 1) * 512], ALU.subtract, [p, stg], [stl])
                    elif func != AF.Copy or evac[0] % 2 == 0:
                        self.act(stg[0:n, b * 512:(b + 1) * 512], p[0:n, :], func, [p], [stg])
                    else:
                        self.cp("dve", stg[0:n, b * 512:(b + 1) * 512], p[0:n, :], [p], [stg])
                for (r0, r1, dbuf, dap) in outs:
                    s.dma("pool", dap, stg[r0:r1, :], reads=[stg], writes=[dbuf])
                for (r0, r1, dbuf, dap) in (lo_outs or []):
                    s.dma("pool", dap, stl[r0:r1, :], reads=[stl], writes=[dbuf])

            fm = []
            for g in range(2):
                fm.append((C_CQ + 128 * g, 128, [(0, 128, self.cqT, self.cqT[128 * g:128 * (g + 1), :])], AF.Copy))
            fm.append((C_CKV, 128, [(0, 128, self.ckvT, self.ckvT[:, :])], AF.Copy))
            fm.append((C_KROPE, 32, [(0, 32, self.kropeT, self.kropeT[:, :])], AF.Copy))
            for c0, dst in ((C_CB, self.cbT), (C_CC, self.ccT), (C_CX, self.cxT)):
                for g in range(2):
                    fm.append((c0 + 128 * g, 128, [(0, 128, dst, dst[128 * g:128 * (g + 1), :])], AF.Copy))
            for g in range(2):
                fm.append((C_DQ + 128 * g, 128, [(64 * j, 64 * (j + 1), self.dqT, self.dqT[64 * r:64 * (r + 1), 2 * g + j, :]) for j in range(2) for r in range(2)], AF.Copy))
            fm.append((C_DK, 64, [(0, 64, self.dkT, self.dkT[0:64, :])], AF.Copy, [(0, 64, self.dkT, self.dkT[64:128, :])]))
            for g in range(2):
                fm.append((C_IQ + 128 * g, 128, [(32 * j, 32 * (j + 1), self.iqT, self.iqT[32 * r:32 * (r + 1), 4 * g + j, :]) for j in range(4) for r in (0, 1)], AF.Copy,
                           [(32 * j, 32 * (j + 1), self.iqT, self.iqT[32 * r:32 * (r + 1), 4 * g + j, :]) for j in range(4) for r in (2, 3)]))
            fm.append((C_IK, 32, [(0, 32, self.ikT, self.ikT[32 * r:32 * (r + 1), :]) for r in (0, 2)], AF.Copy,
                       [(0, 32, self.ikT, self.ikT[32 * r:32 * (r + 1), :]) for r in (1, 3)]))
            fm.append((C_GQ, 128, [(32 * j, 32 * (j + 1), self.gqT, self.gqT[:, j, :]) for j in range(4)], AF.Copy))
            fm.append((C_GK, 128, [(32 * j, 32 * (j + 1), self.gkT, self.gkT[:, j, :]) for j in range(4)], AF.Copy))
            fm.append((C_GLR, 16, [(0, 16, self.glrT, self.glrT[0:16, :])], AF.Copy))
            for k in range(8):
                s.dma("pool", self.hTd[k * 128:(k + 1) * 128, :], hT[:, k, :], reads=[hT], writes=[self.hTd])
            nxt = load_w(fm[0][0], fm[0][1])
            for i, ent in enumerate(fm):
                c0, n, outs, func = ent[:4]
                wbf = nxt
                if i + 1 < len(fm):
                    nxt = load_w(fm[i + 1][0], fm[i + 1][1])
                fm_group(wbf, n, outs, func, ent[4] if len(ent) > 4 else None)

            tms = [(C_DV, 64, self.dvaug, BF16, AF.Copy, True), (C_IW, 8, self.iw, F32, AF.Copy, False),
                   (C_GK, 128, self.gk, BF16, AF.Copy, False), (C_GV, 256, self.gv, BF16, AF.Copy, False),
                   (C_GR, 256, self.grs, BF16, AF.Silu, False)]
            for (c0, n, dst, dt, func, ones_col) in tms:
                wbf = load_w(c0, n)
                nn = n + (1 if ones_col else 0)
                tstg = self.sb(st, "tstg", [128, NT, nn], dt)
                if ones_col:
                    self.ms("dve", tstg[:, :, n:n + 1], 1.0, [tstg])
                for t in range(NT):
                    p = ps.next()
                    for k in range(8):
                        self.mm(p[:, 0:n], hT[:, k, t * 128:(t + 1) * 128], wbf[:, k, 0:n], k == 0, k == 7, [wbf, hT], [p])
                    self.act(tstg[:, t, 0:n], p[:, 0:n], func, [p], [tstg])
                s.dma("pool", dst[:, :].rearrange("(t p) n -> p t n", p=128), tstg[:], reads=[tstg], writes=[dst])

    def phase_B(self, l):
        s, S, NT, NB = self.s, self.S, self.NT, self.NB
        with self.scope() as st:
            wqf = self.sb(st, "wqf", [128, 2, 384], F32)
            wqb = self.sb(st, "wqb", [128, 2, 384], BF16)
            wkf = self.sb(st, "wkf", [128, 512], F32)
            wkb = self.sb(st, "wkb", [128, 512], BF16)
            s.dma("sp", wqf[:], self.w_qup[l].rearrange("(k p) n -> p k n", p=128), reads=[self.w_qup], writes=[wqf])
            s.dma("sp", wkf[:], self.w_kvup[l], reads=[self.w_kvup], writes=[wkf])
            for k in range(2):
                self.ts("dve", wqb[:, k, :], wqf[:, k, :], self.gq[:, l, k:k + 1], None, ALU.mult, reads=[wqf, self.gq], writes=[wqb])
            self.ts("dve", wkb[:], wkf[:], self.gkv[:, l:l + 1], None, ALU.mult, reads=[wkf, self.gkv], writes=[wkb])
            ps = Rot([self.psb(st, "psB") for _ in range(6)])
            cq_r = Rot([self.sb(st, "cqb", [128, 2, 512], BF16) for _ in range(2)])
            ckv_r = Rot([self.sb(st, "ckvb", [128, 512], BF16) for _ in range(2)])
            kr_r = Rot([self.sb(st, "krb", [32, 512], BF16) for _ in range(2)])
            cos_r = Rot([self.sb(st, "cosb", [32, 512], F32) for _ in range(2)])
            sin_r = Rot([self.sb(st, "sinb", [32, 512], F32) for _ in range(2)])
            sq_r = Rot([self.sb(st, "sqb", [128, 2, 512], BF16) for _ in range(2)])
            sqk_r = Rot([self.sb(st, "sqk", [128, 512], BF16) for _ in range(2)])
            rq_r = Rot([self.sb(st, "rq", [128, 512], F32) for _ in range(2)])
            rk_r = Rot([self.sb(st, "rk", [128, 512], F32) for _ in range(2)])
            rkt_r = Rot([self.sb(st, "rkt", [128, 4], F32) for _ in range(2)])
            qo_r = Rot([self.sb(st, "qo", [64, 512], BF16) for _ in range(3)])
            xr_r = Rot([self.sb(st, "xr", [32, 512], BF16) for _ in range(3)])
            t1_r = Rot([self.sb(st, "t1", [32, 512], F32) for _ in range(3)])
            t2_r = Rot([self.sb(st, "t2", [32, 512], F32) for _ in range(3)])
            ro_r = Rot([self.sb(st, "ro", [32, 512], BF16) for _ in range(3)])
            vst_r = Rot([self.sb(st, "vst", [128, 4, 4, 65], BF16) for _ in range(2)])
            for v in vst_r.bufs:
                self.ms("dve", v[:, :, :, 64:65], 1.0, [v])

            def rope(xr, cosb, sinb, dsts):
                p = ps.next()
                self.mm(p[0:32, :], self.rotb[:], xr[:], True, True, [self.rotb, xr], [p])
                t1, t2, ro = t1_r.next(), t2_r.next(), ro_r.next()
                self.tt("pool", t1[:], xr[:], cosb[:], ALU.mult, [xr, cosb], [t1])
                self.tt("dve", t2[:], p[0:32, :], sinb[:], ALU.mult, [p, sinb], [t2])
                self.tt("dve", ro[:], t1[:], t2[:], ALU.add, [t1, t2], [ro])
                for dbuf, dap in dsts:
                    s.dma("pool", dap, ro[:], reads=[ro], writes=[dbuf])

            for b in range(NB):
                c0, c1 = b * 512, (b + 1) * 512
                cq, ckv, kr, cosb, sinb = cq_r.next(), ckv_r.next(), kr_r.next(), cos_r.next(), sin_r.next()
                s.dma("sp", cq[:], self.cqT[:, c0:c1].rearrange("(k p) t -> p k t", p=128), reads=[self.cqT], writes=[cq])
                s.dma("sp", ckv[:], self.ckvT[:, c0:c1], reads=[self.ckvT], writes=[ckv])
                s.dma("sp", kr[:], self.kropeT[:, c0:c1], reads=[self.kropeT], writes=[kr])
                s.dma("sp", cosb[:], self.cosT[:, c0:c1], reads=[self.cosT], writes=[cosb])
                s.dma("sp", sinb[:], self.sinT[:, c0:c1], reads=[self.sinT], writes=[sinb])
                sq, sqk, rq, rk, rkt = sq_r.next(), sqk_r.next(), rq_r.next(), rk_r.next(), rkt_r.next()
                self.tt("pool", sq[:], cq[:], cq[:], ALU.mult, [cq], [sq])
                p = ps.next()
                for k in range(2):
                    self.mm(p[:, :], self.onesb[:], sq[:, k, :], k == 0, k == 1, [self.onesb, sq], [p])
                self.act(rq[:], p[:, :], AF.Sqrt, [p, self.epsb], [rq], scale=1.0 / 256, bias=self.epsb[:, 0:1])
                self.recip(rq[:], rq[:], [rq], [rq])
                self.tt("pool", sqk[:], ckv[:], ckv[:], ALU.mult, [ckv], [sqk])
                p2 = ps.next()
                self.mm(p2[:, :], self.onesb[:], sqk[:], True, True, [self.onesb, sqk], [p2])
                self.act(rk[:], p2[:, :], AF.Sqrt, [p2, self.epsb], [rk], scale=1.0 / 128, bias=self.epsb[:, 0:1])
                self.recip(rk[:], rk[:], [rk], [rk])
                p3 = ps.next()
                for tt_ in range(4):
                    self.mm(p3[:, tt_:tt_ + 1], sqk[:, tt_ * 128:(tt_ + 1) * 128], self.onesb[:, 0:1], True, True, [self.onesb, sqk], [p3])
                self.act(rkt[:], p3[:, 0:4], AF.Sqrt, [p3, self.epsb], [rkt], scale=1.0 / 128, bias=self.epsb[:, 0:1])
                self.recip(rkt[:], rkt[:], [rkt], [rkt])
                for h in range(4):
                    pn = ps.next()
                    for k in range(2):
                        self.mm(pn[0:64, :], wqb[:, k, h * 96:h * 96 + 64], cq[:, k, :], k == 0, k == 1, [wqb, cq], [pn])
                    qo = qo_r.next()
                    self.tt("dve", qo[:], pn[0:64, :], rq[0:64, :], ALU.mult, [pn, rq], [qo])
                    s.dma("pool", self.qT[h, 0:64, c0:c1], qo[:], reads=[qo], writes=[self.qT])
                    pr = ps.next()
                    for k in range(2):
                        self.mm(pr[0:32, :], wqb[:, k, h * 96 + 64:h * 96 + 96], cq[:, k, :], k == 0, k == 1, [wqb, cq], [pr])
                    xr = xr_r.next()
                    self.tt("dve", xr[:], pr[0:32, :], rq[0:32, :], ALU.mult, [pr, rq], [xr])
                    rope(xr, cosb, sinb, [(self.qT, self.qT[h, 64:96, c0:c1])])
                for h in range(4):
                    pn = ps.next()
                    self.mm(pn[0:64, :], wkb[:, h * 128:h * 128 + 64], ckv[:], True, True, [wkb, ckv], [pn])
                    qo = qo_r.next()
                    self.tt("dve", qo[:], pn[0:64, :], rk[0:64, :], ALU.mult, [pn, rk], [qo])
                    s.dma("pool", self.kT[h, 0:64, c0:c1], qo[:], reads=[qo], writes=[self.kT])
                rope(kr, cosb, sinb, [(self.kT, self.kT[h, 64:96, c0:c1]) for h in range(4)])
                vst = vst_r.next()
                for tt_ in range(4):
                    pv = ps.next()
                    self.mm(pv[:, :], ckv[:, tt_ * 128:(tt_ + 1) * 128], wkb[:], True, True, [wkb, ckv], [pv])
                    self.act(vst[:, tt_, :, 0:64], pv[:, :].rearrange("p (h c) -> p h c", h=4)[:, :, 64:128], AF.Copy, [pv, rkt], [vst],
                             scale=rkt[:, tt_:tt_ + 1])
                s.dma("pool", self.vaug[c0:c1, :, :].rearrange("(t p) h c -> p t h c", p=128), vst[:], reads=[vst], writes=[self.vaug])

    def attn_finish(self, oacc, obuf_r, oT_r, psT_r, branch, t):
        rec = self.rec_r.next()
        o = obuf_r.next()
        acc3 = oacc[:, 0:260].rearrange("p (h c) -> p h c", h=4)
        self.recip(rec[:, :], acc3[:, :, 64], [oacc], [rec])
        self.tt("dve", o[:].rearrange("p (h c) -> p h c", h=4), acc3[:, :, 0:64],
                rec[:, :].unsqueeze(2).to_broadcast([128, 4, 64]), ALU.mult, [oacc, rec], [o])
        self.to_oT(o, oT_r, psT_r, branch, t)

    def to_oT(self, o, oT_r, psT_r, branch, t):
        s = self.s
        pT = psT_r.next()
        for k in range(2):
            self.tr(pT[:, k * 128:(k + 1) * 128], o[:, k * 128:(k + 1) * 128], [o], [pT])
        oTt = oT_r.next()
        self.act(oTt[:].rearrange("p k q -> p (k q)"), pT[:, 0:256], AF.Copy, [pT], [oTt])
        s.dma("pool", self.oT[branch, :, t * 128:(t + 1) * 128].rearrange("(k p) q -> p k q", p=128), oTt[:], reads=[oTt], writes=[self.oT])

    def phase_C(self, l):
        s, S, NT = self.s, self.S, self.NT
        scale = float(96 ** -0.5)
        with self.scope() as st:
            kT = self.sb(st, "kTs", [96, 4, S], BF16)
            qT = self.sb(st, "qTs", [96, 4, S], BF16)
            va = self.sb(st, "vas", [128, NT, 4, 65], BF16)
            for h in range(4):
                s.dma("sp", kT[:, h, :], self.kT[h], reads=[self.kT], writes=[kT])
                s.dma("sp", qT[:, h, :], self.qT[h], reads=[self.qT], writes=[qT])
            s.dma("sp", va[:], self.vaug[:, :, :].rearrange("(t p) h c -> p t h c", p=128), reads=[self.vaug], writes=[va])
            ps_st = Rot([self.psb(st, "psS") for _ in range(3)])
            ps_o = Rot([self.psb(st, "psO") for _ in range(2)])
            psT_r = Rot([self.psb(st, "psT", BF16) for _ in range(1)])
            pT_r = Rot([self.sb(st, "pT", [128, 512], BF16) for _ in range(3)])
            self.rec_r = Rot([self.sb(st, "rec", [128, 4], F32) for _ in range(2)])
            obuf_r = Rot([self.sb(st, "ob", [128, 256], BF16) for _ in range(2)])
            oT_r = Rot([self.sb(st, "oTt", [128, 2, 128], BF16) for _ in range(2)])
            for t in range(NT):
                oacc = ps_o.next()
                q0, q1 = t * 128, (t + 1) * 128
                def pv(j, pT):
                    for h in range(4):
                        self.mm(oacc[:, h * 65:(h + 1) * 65], pT[:, h * 128:(h + 1) * 128], va[:, j, h, :], (j == 0 and h == 0), j == t, [pT, va], [oacc])

                prev = None
                for j in range(t + 1):
                    k0, k1 = j * 128, (j + 1) * 128
                    sp = ps_st.next()
                    for h in range(4):
                        self.mm(sp[:, h * 128:(h + 1) * 128], kT[:, h, k0:k1], qT[:, h, q0:q1], h == 0, j != t, [kT, qT], [sp])
                    if j == t:
                        for h in range(4):
                            self.mm(sp[:, h * 128:(h + 1) * 128], self.identb[:], self.cmtb[:], False, True, [self.identb, self.cmtb], [sp])
                    pT = pT_r.next()
                    self.act(pT[:], sp[:, :], AF.Exp, [sp], [pT], scale=scale)
                    if prev is not None:
                        pv(*prev)
                    prev = (j, pT)
                pv(*prev)
                self.attn_finish(oacc, obuf_r, oT_r, psT_r, 0, t)

    def phase_E(self, l):
        s, S = self.s, self.S
        cw = self.convw
        with self.scope() as st:
            cb = self.sb(st, "cb", [128, S], BF16)
            cc = self.sb(st, "cc", [128, S], BF16)
            cx = self.sb(st, "cx", [128, S], BF16)
            u = self.sb(st, "u", [128, S + 2], F32)
            y = self.sb(st, "y", [128, S], F32)
            o = self.sb(st, "o", [128, S], BF16)
            for c in range(2):
                r0, r1 = c * 128, (c + 1) * 128
                s.dma("sp", cb[:], self.cbT[r0:r1, :], reads=[self.cbT], writes=[cb])
                s.dma("sp", cc[:], self.ccT[r0:r1, :], reads=[self.ccT], writes=[cc])
                s.dma("sp", cx[:], self.cxT[r0:r1, :], reads=[self.cxT], writes=[cx])
                self.ms("pool", u[:, 0:2], 0.0, [u])
                self.tt("pool", u[:, 2:S + 2], cc[:], cx[:], ALU.mult, [cc, cx], [u])
                self.ts("dve", y[:], u[:, 2:S + 2], cw[:, l, c, 2:3], None, ALU.mult, reads=[u, cw], writes=[y])
                self.stt(y[:], u[:, 1:S + 1], cw[:, l, c, 1:2], y[:], ALU.mult, ALU.add, [u, cw, y], [y])
                self.stt(y[:], u[:, 0:S], cw[:, l, c, 0:1], y[:], ALU.mult, ALU.add, [u, cw, y], [y])
                self.tt("pool", o[:], cb[:], y[:], ALU.mult, [cb, y], [o])
                s.dma("pool", self.oT[1, r0:r1, :], o[:], reads=[o], writes=[self.oT])

    def phase_D(self, l):
        s, S, NT = self.s, self.S, self.NT
        cidx = float(32 ** -0.5 * 8 ** -0.5)
        scale = float(64 ** -0.5)
        mscale = float(96 ** -0.5)
        with self.scope() as st:
            ikT = self.sb(st, "ikTs", [128, S], BF16)
            dkT = self.sb(st, "dkTs", [128, S], BF16)
            dva = self.sb(st, "dvas", [128, NT, 65], BF16)
            iw = self.sb(st, "iws", [128, NT, 8], F32)
            kT = self.sb(st, "kTs", [96, 4, S], BF16)
            va = self.sb(st, "vas", [128, NT, 4, 65], BF16)
            s.dma("sp", ikT[:], self.ikT[:, :], reads=[self.ikT], writes=[ikT])
            s.dma("sp", dkT[:], self.dkT[:, :], reads=[self.dkT], writes=[dkT])
            s.dma("sp", dva[:], self.dvaug[:, :].rearrange("(t p) c -> p t c", p=128), reads=[self.dvaug], writes=[dva])
            s.dma("sp", iw[:], self.iw[:, :].rearrange("(t p) c -> p t c", p=128), reads=[self.iw], writes=[iw])
            for h in range(4):
                s.dma("sp", kT[:, h, :], self.kT[h], reads=[self.kT], writes=[kT])
            s.dma("sp", va[:], self.vaug[:, :, :].rearrange("(t p) h c -> p t h c", p=128), reads=[self.vaug], writes=[va])
            iq_r = Rot([self.sb(st, "iqt", [128, 8, 128], BF16) for _ in range(3)])
            dq_r = Rot([self.sb(st, "dqt", [128, 4, 128], BF16) for _ in range(4)])
            mq_r = Rot([self.sb(st, "mqt", [96, 4, 128], BF16) for _ in range(3)])
            sc_r = [self.sb(st, "sc", [128, S], F32) for _ in range(2)]
            mb_r = [self.sb(st, "mb", [128, S], BF16) for _ in range(2)]
            junk = self.sb(st, "junkd", [128, S], BF16)
            ps_rel = Rot([self.psb(st, "psR") for _ in range(2)])
            ps_sc = Rot([self.psb(st, "psSc") for _ in range(1)])
            ps_st = Rot([self.psb(st, "psS") for _ in range(2)])
            ps_od = Rot([self.psb(st, "psOd") for _ in range(1)])
            ps_om = Rot([self.psb(st, "psOm") for _ in range(1)])
            psT_r = Rot([self.psb(st, "psT", BF16) for _ in range(1)])
            rl_r = Rot([self.sb(st, "rl", [128, 512], BF16) for _ in range(4)])
            dm_r = Rot([self.sb(st, "dm", [128, 8, 128], BF16) for _ in range(2)])
            pT_r = Rot([self.sb(st, "pT", [128, 512], BF16) for _ in range(4)])
            self.rec_r = Rot([self.sb(st, "rec", [128, 4], F32) for _ in range(2)])
            obuf_r = Rot([self.sb(st, "ob", [128, 256], BF16) for _ in range(2)])
            oT_r = Rot([self.sb(st, "oTt", [128, 2, 128], BF16) for _ in range(2)])
            sm = {k: self.sb(st, k, [128, 1], F32) for k in ("w0", "lo", "mid", "cnt", "tmp", "thr")}
            mx_r = Rot([self.sb(st, "mxr", [128, 1], F32) for _ in range(3)])
            lo_r = Rot([self.sb(st, "lor", [128, 1], F32) for _ in range(3)])
            thc_r = Rot([self.sb(st, "thc", [128, 1], F32) for _ in range(2)])
            dq_of = {}
            mm_of = {}

            def score(t):
                q0, q1 = t * 128, (t + 1) * 128
                nk = (t + 1) * 128
                sc = sc_r[t % 2]
                iq = iq_r.next()
                dq = dq_r.next()
                dq_of[t] = dq
                s.dma("sp", iq[:], self.iqT[:, :, q0:q1], reads=[self.iqT], writes=[iq])
                s.dma("sp", dq[:], self.dqT[:, :, q0:q1], reads=[self.dqT], writes=[dq])
                dm = dm_r.next()
                for h in range(8):
                    self.ts("pool", dm[:, h, :], self.identf[:], iw[:, t, h:h + 1], cidx, ALU.mult, ALU.mult, reads=[self.identf, iw], writes=[dm])
                nb = (nk + 511) // 512
                for b in range(nb):
                    k0 = b * 512
                    w = min(512, nk - k0)
                    psc = ps_sc.next()
                    diag = (b == nb - 1)
                    prev = None
                    for h in range(8):
                        pr = ps_rel.next()
                        self.mm(pr[:, 0:w], iq[:, h, :], ikT[:, k0:k0 + w], True, True, [iq, ikT], [pr])
                        rl = rl_r.next()
                        self.act(rl[:, 0:w], pr[:, 0:w], AF.Relu, [pr], [rl])
                        if prev is not None:
                            hh, prl = prev
                            self.mm(psc[:, 0:w], dm[:, hh, :], prl[:, 0:w], hh == 0, False, [dm, prl], [psc])
                        prev = (h, rl)
                    hh, prl = prev
                    self.mm(psc[:, 0:w], dm[:, hh, :], prl[:, 0:w], False, not diag, [dm, prl], [psc])
                    if diag:
                        self.mm(psc[:, w - 128:w], self.identb[:], self.cmqb[:], False, True, [self.identb, self.cmqb], [psc])
                    self.act(sc[:, k0:k0 + w], psc[:, 0:w], AF.Copy, [psc], [sc])
                if self.dsc is not None:
                    s.dma("pool", self.dsc[q0:q1, 0:nk], sc[:, 0:nk], reads=[sc], writes=[self.dsc])

            def bisect(t):
                nk = (t + 1) * 128
                sc, mb = sc_r[t % 2], mb_r[t % 2]
                thr = sm["thr"]
                if t < 2:
                    self.ms("dve", thr[:], -10000.0, [thr])
                else:
                    w0, lo, mid, cnt, tmp = (sm[k] for k in ("w0", "lo", "mid", "cnt", "tmp"))
                    mx = mx_r.next()
                    s.op("dve", "tensor_reduce", (), dict(out=mx[:], in_=sc[:, 0:nk], axis=AX.X, op=ALU.max), [sc], [mx])
                    s.op("dve", "tensor_reduce", (), dict(out=lo[:], in_=sc[:, 0:256], axis=AX.X, op=ALU.min), [sc], [lo])
                    self.tt("dve", w0[:], mx[:], lo[:], ALU.subtract, [mx, lo], [w0])
                    self.ts("dve", w0[:], w0[:], 1.001, 1e-6, ALU.mult, ALU.add, reads=[w0], writes=[w0])
                    for it in range(NBIS):
                        f = float(2.0 ** -(it + 1))
                        self.stt(mid[:], w0[:], f, lo[:], ALU.mult, ALU.add, [w0, lo], [mid])
                        self.ts("dve", junk[:, 0:nk], sc[:, 0:nk], mid[:, 0:1], None, ALU.is_ge, ALU.add, reads=[sc, mid], writes=[junk, cnt], accum_out=cnt[:])
                        self.ts("dve", tmp[:], cnt[:], 255.5, f, ALU.is_ge, ALU.mult, reads=[cnt], writes=[tmp])
                        self.stt(lo[:], tmp[:], w0[:, 0:1], lo[:], ALU.mult, ALU.add, [tmp, w0, lo], [lo])
                    self.cp("dve", thr[:], lo[:], [lo], [thr])
                self.ts("dve", mb[:, 0:nk], sc[:, 0:nk], thr[:, 0:1], -30000.0, ALU.is_lt, ALU.mult, reads=[sc, thr], writes=[mb])

            def attend(t):
                mb = mb_r[t % 2]
                dq = dq_of.pop(t)
                oacc = ps_od.next()

                def pv(j, pT):
                    for h in range(4):
                        self.mm(oacc[:, h * 65:(h + 1) * 65], pT[:, h * 128:(h + 1) * 128], dva[:, j, :], (j == 0 and h == 0), j == t, [pT, dva], [oacc])

                prev = None
                for j in range(t + 1):
                    k0, k1 = j * 128, (j + 1) * 128
                    sp = ps_st.next()
                    self.mm(sp[:, :], dkT[:, k0:k1], dq[:, :, :].rearrange("d h q -> d (h q)"), True, False, [dkT, dq], [sp])
                    self.mm(sp[:, :], mb[:, k0:k1], self.i4b[:], False, True, [mb, self.i4b], [sp])
                    pT = pT_r.next()
                    self.act(pT[:], sp[:, :], AF.Exp, [sp], [pT], scale=scale)
                    if prev is not None:
                        pv(*prev)
                    prev = (j, pT)
                pv(*prev)
                self.attn_finish(oacc, obuf_r, oT_r, psT_r, 2, t)

            def attend_mla(t):
                q0, q1 = t * 128, (t + 1) * 128
                mq = mq_r.next()
                s.dma("sp", mq[:], self.qT[:, :, q0:q1].rearrange("h d q -> d h q"), reads=[self.qT], writes=[mq])
                oacc = ps_om.next()

                def pv(j, pT):
                    for h in range(4):
                        self.mm(oacc[:, h * 65:(h + 1) * 65], pT[:, h * 128:(h + 1) * 128], va[:, j, h, :], (j == 0 and h == 0), j == t, [pT, va], [oacc])

                prev = None
                for j in range(t + 1):
                    k0, k1 = j * 128, (j + 1) * 128
                    sp = ps_st.next()
                    for h in range(4):
                        self.mm(sp[:, h * 128:(h + 1) * 128], kT[:, h, k0:k1], mq[:, h, :], h == 0, j != t, [kT, mq], [sp])
                    if j == t:
                        for h in range(4):
                            self.mm(sp[:, h * 128:(h + 1) * 128], self.identb[:], self.cmtb[:], False, True, [self.identb, self.cmtb], [sp])
                    pT = pT_r.next()
                    self.act(pT[:], sp[:, :], AF.Exp, [sp], [pT], scale=mscale)
                    if prev is not None:
                        pv(*prev)
                    prev = (j, pT)
                pv(*prev)
                self.attn_finish(oacc, obuf_r, oT_r, psT_r, 0, t)

            score(0)
            if NT > 1:
                score(1)
            bisect(0)
            for t in range(NT):
                if t + 2 < NT:
                    score(t + 2)
                if t + 1 < NT:
                    bisect(t + 1)
                attend(t)
                attend_mla(t)

    def phase_F(self, l):
        s, S, NT = self.s, self.S, self.NT
        with self.scope() as st:
            gqT = self.sb(st, "gqTs", [32, 4, S], BF16)
            gkT = self.sb(st, "gkTs", [32, 4, S], BF16)
            gk = self.sb(st, "gks", [128, NT, 128], BF16)
            gv = self.sb(st, "gvs", [128, NT, 256], BF16)
            grs = self.sb(st, "grss", [128, NT, 256], BF16)
            glrT = self.sb(st, "glrTs", [17, S], BF16)
            wgf = self.sb(st, "wgf", [17, 128], F32)
            wgb = self.sb(st, "wgb", [17, 128], BF16)
            ngb = self.sb(st, "ngb", [128, 64], F32)
            s.dma("sp", gqT[:], self.gqT[:, :, :], reads=[self.gqT], writes=[gqT])
            s.dma("sp", gkT[:], self.gkT[:, :, :], reads=[self.gkT], writes=[gkT])
            s.dma("sp", gk[:], self.gk[:, :].rearrange("(t p) c -> p t c", p=128), reads=[self.gk], writes=[gk])
            s.dma("sp", gv[:], self.gv[:, :].rearrange("(t p) c -> p t c", p=128), reads=[self.gv], writes=[gv])
            s.dma("sp", grs[:], self.grs[:, :].rearrange("(t p) c -> p t c", p=128), reads=[self.grs], writes=[grs])
            s.dma("sp", glrT[:], self.glrT[:, :], reads=[self.glrT], writes=[glrT])
            s.dma("sp", wgf[0:16, :], self.gla_wg[l], reads=[self.gla_wg], writes=[wgf])
            s.dma("sp", wgf[16:17, :], self.gla_bg[l], reads=[self.gla_bg], writes=[wgf])
            s.dma("sp", ngb[:], self.gla_ng[l].partition_broadcast(128), reads=[self.gla_ng], writes=[ngb])
            self.cp("dve", wgb[:], wgf[:], [wgf], [wgb])
            state = self.sb(st, "state", [32, 4, 64], F32)
            stateb = self.sb(st, "stateb", [32, 4, 64], BF16)
            self.ms("dve", state[:], 0.0, [state])
            self.ms("dve", stateb[:], 0.0, [stateb])
            ps_z = Rot([self.psb(st, "psZ") for _ in range(1)])
            ps_c = Rot([self.psb(st, "psC") for _ in range(2)])
            ps_a = Rot([self.psb(st, "psA") for _ in range(1)])
            ps_o = Rot([self.psb(st, "psO") for _ in range(2)])
            ps_u = Rot([self.psb(st, "psU") for _ in range(1)])
            psT_r = Rot([self.psb(st, "psT", BF16) for _ in range(1)])
            ez_r = Rot([self.sb(st, "ez", [128, 128], F32) for _ in range(2)])
            sp_r = Rot([self.sb(st, "sp", [128, 128], F32) for _ in range(2)])
            ec_r = Rot([self.sb(st, "ec", [32, 4, 128], F32) for _ in range(2)])
            en_r = Rot([self.sb(st, "en", [32, 4, 128], F32) for _ in range(2)])
            er_r = Rot([self.sb(st, "er", [128, 128], F32) for _ in range(2)])
            qt_r = Rot([self.sb(st, "qt", [32, 4, 128], BF16) for _ in range(2)])
            kt_r = Rot([self.sb(st, "kt", [32, 4, 128], BF16) for _ in range(2)])
            kh_r = Rot([self.sb(st, "kh", [128, 128], BF16) for _ in range(2)])
            am_r = Rot([self.sb(st, "am", [128, 4, 128], BF16) for _ in range(2)])
            on_r = Rot([self.sb(st, "on", [128, 4, 64], F32) for _ in range(2)])
            og_r = Rot([self.sb(st, "og", [128, 256], BF16) for _ in range(2)])
            ss_r = Rot([self.sb(st, "ssg", [128, 4], F32) for _ in range(2)])
            junk = self.sb(st, "junkg", [128, 64], F32)
            oT_r = Rot([self.sb(st, "oTt", [128, 2, 128], BF16) for _ in range(2)])
            qscale = float(32 ** -0.5)
            def pre(t):
                c0, c1 = t * 128, (t + 1) * 128
                pz = ps_z.next()
                self.mm(pz[:, 0:128], glrT[:, c0:c1], wgb[:], True, True, [glrT, wgb], [pz])
                ez, sp = ez_r.next(), sp_r.next()
                self.act(ez[:], pz[:, 0:128], AF.Exp, [pz], [ez], scale=-1.0)
                self.act(sp[:], ez[:], AF.Ln, [ez], [sp], bias=1.0)
                pc = ps_c.next()
                for h in range(4):
                    self.mm(pc[0:32, h * 128:(h + 1) * 128], sp[:, h * 32:(h + 1) * 32], self.tri[:], True, True, [sp, self.tri], [pc])
                prv = ps_c.next()
                self.mm(prv[:, 0:128], self.rev[:], sp[:], True, True, [sp, self.rev], [prv])
                ec, en, er = ec_r.next(), en_r.next(), er_r.next()
                self.act(ec[:].rearrange("p h i -> p (h i)"), pc[0:32, :], AF.Exp, [pc], [ec])
                self.act(en[:].rearrange("p h i -> p (h i)"), pc[0:32, :], AF.Exp, [pc], [en], scale=-1.0)
                self.act(er[:], prv[:, 0:128], AF.Exp, [prv], [er])
                qt, kt, kh = qt_r.next(), kt_r.next(), kh_r.next()
                self.stt(qt[:], gqT[:, :, c0:c1], qscale, ec[:], ALU.mult, ALU.mult, [gqT, ec], [qt])
                self.tt("pool", kt[:], gkT[:, :, c0:c1], en[:], ALU.mult, [gkT, en], [kt])
                self.tt("pool", kh[:], gk[:, t, :], er[:], ALU.mult, [gk, er], [kh])
                pa = ps_a.next()
                for h in range(4):
                    self.mm(pa[:, h * 128:(h + 1) * 128], kt[:, h, :], qt[:, h, :], True, True, [kt, qt], [pa])
                am = am_r.next()
                self.tt("dve", am[:], pa[:, :].rearrange("p (h i) -> p h i", h=4), self.gmaskb[:].unsqueeze(1).to_broadcast([128, 4, 128]),
                        ALU.mult, [pa, self.gmaskb], [am])
                return ec, qt, kh, am

            def rec(t, ec, qt, kh, am):
                po = ps_o.next()
                for h in range(4):
                    self.mm(po[:, h * 64:(h + 1) * 64], am[:, h, :], gv[:, t, h * 64:(h + 1) * 64], h == 0, False, [am, gv], [po])
                for c in range(2):
                    i0, i1 = c * 64, (c + 1) * 64
                    for h in range(4):
                        self.mm(po[i0:i1, h * 64:(h + 1) * 64], qt[:, h, i0:i1], stateb[:, h, :], False, True, [qt, stateb], [po])
                    pu = ps_u.next()
                    for h in range(4):
                        self.mm(pu[0:32, h * 64:(h + 1) * 64], kh[i0:i1, h * 32:(h + 1) * 32], gv[i0:i1, t, h * 64:(h + 1) * 64], True, True, [kh, gv], [pu])
                    self.tt("dve", state[:], state[:], ec[:, :, i1 - 1:i1].to_broadcast([32, 4, 64]), ALU.mult, [state, ec], [state])
                    self.tt("dve", state[:], state[:], pu[0:32, 0:256].rearrange("p (h v) -> p h v", h=4), ALU.add, [state, pu], [state])
                    self.cp("dve", stateb[:], state[:], [state], [stateb])
                ss, on, og = ss_r.next(), on_r.next(), og_r.next()
                for h in range(4):
                    self.act(junk[:], po[:, h * 64:(h + 1) * 64], AF.Square, [po], [junk, ss], accum_out=ss[:, h:h + 1])
                self.act(ss[:], ss[:], AF.Sqrt, [ss, self.epsb], [ss], scale=1.0 / 64, bias=self.epsb[:, 0:1])
                self.recip(ss[:], ss[:], [ss], [ss])
                self.tt("dve", on[:], po[:, 0:256].rearrange("p (h v) -> p h v", h=4), ss[:].unsqueeze(2).to_broadcast([128, 4, 64]),
                        ALU.mult, [po, ss], [on])
                self.tt("pool", on[:], on[:], ngb[:].unsqueeze(1).to_broadcast([128, 4, 64]), ALU.mult, [on, ngb], [on])
                self.tt("pool", og[:], on[:].rearrange("p h v -> p (h v)"), grs[:, t, :], ALU.mult, [on, grs], [og])
                self.to_oT(og, oT_r, psT_r, 3, t)

            nxt = pre(0)
            for t in range(NT):
                cur = nxt
                if t + 1 < NT:
                    nxt = pre(t + 1)
                rec(t, *cur)

    def phase_G(self, l):
        s, S, NT, NB = self.s, self.S, self.NT, self.NB
        src = self.x_in if l == 0 else self.xres
        with self.scope() as st:
            wbb = self.sb(st, "wbb", [128, 4, 2, D], BF16)
            wob = self.sb(st, "wob", [128, 8, D], BF16)
            wgt = self.sb(st, "wgt", [128, 8, 4096], BF16)
            for n in range(4):
                s.dma("pool", wbb[:, n, :, :], self.w_br[l, n].rearrange("(k p) c -> p k c", p=128), reads=[self.w_br], writes=[wbb])
            for k in range(8):
                s.dma("pool", wgt[:, k, :], self.w_in[l, k * 128:(k + 1) * 128, C_GATE:C_GATE + 4096], reads=[self.w_in], writes=[wgt])
            for kk in range(4):
                s.dma("pool", wob[:, 2 * kk:2 * kk + 2, :], self.w_out[l, kk * 256:(kk + 1) * 256, :].rearrange("(k p) c -> p k c", p=128),
                      reads=[self.w_out], writes=[wob])
            psy = Rot([self.psb(st, "psGy") for _ in range(2)])
            psg = Rot([self.psb(st, "psGg") for _ in range(3)])
            psx = Rot([self.psb(st, "psGx") for _ in range(2)])
            ob_r = Rot([self.sb(st, "obk", [128, 4, 2, 512], BF16) for _ in range(2)])
            hb_r = Rot([self.sb(st, "hbk", [128, 8, 512], BF16) for _ in range(2)])
            gs_r = Rot([self.sb(st, "gsb", [128, 512], BF16) for _ in range(3)])
            mf_r = Rot([self.sb(st, "mf", [128, 512], F32) for _ in range(2)])
            tf_r = Rot([self.sb(st, "tf", [128, 512], F32) for _ in range(3)])
            mT_r = Rot([self.sb(st, "mT", [128, 8, 512], BF16) for _ in range(2)])
            xt_r = Rot([self.sb(st, "xtg", [128, D], F32) for _ in range(2)])
            xn_r = Rot([self.sb(st, "xng", [128, D], F32) for _ in range(2)])

            def load_blk(b):
                c0, c1 = b * 512, (b + 1) * 512
                ob, hb = ob_r.next(), hb_r.next()
                for n in range(4):
                    s.dma("sp", ob[:, n, :, :], self.oT[n, :, c0:c1].rearrange("(k p) t -> p k t", p=128), reads=[self.oT], writes=[ob])
                s.dma("sp", hb[:], self.hTd[:, c0:c1].rearrange("(k p) t -> p k t", p=128), reads=[self.hTd], writes=[hb])
                return ob, hb

            nxt = load_blk(0)
            for b in range(NB):
                c0, c1 = b * 512, (b + 1) * 512
                ob, hb = nxt
                if b + 1 < NB:
                    nxt = load_blk(b + 1)
                mT = mT_r.next()
                for cg in range(8):
                    mf = mf_r.next()
                    for n in range(4):
                        g = n * 8 + cg
                        pg = psg.next()
                        for k in range(8):
                            self.mm(pg[:, :], wgt[:, k, g * 128:(g + 1) * 128], hb[:, k, :], k == 0, k == 7, [wgt, hb], [pg])
                        p = psy.next()
                        for k in range(2):
                            self.mm(p[:, :], wbb[:, n, k, cg * 128:(cg + 1) * 128], ob[:, n, k, :], k == 0, k == 1, [wbb, ob], [p])
                        gs = gs_r.next()
                        self.act(gs[:], pg[:, :], AF.Sigmoid, [pg], [gs])
                        if n == 0:
                            self.tt("dve", mf[:], p[:, :], gs[:], ALU.mult, [p, gs], [mf])
                        else:
                            tf = tf_r.next()
                            self.tt("dve", tf[:], p[:, :], gs[:], ALU.mult, [p, gs], [tf])
                            if n < 3:
                                self.tt("pool", mf[:], mf[:], tf[:], ALU.add, [mf, tf], [mf])
                            else:
                                self.tt("pool", mT[:, cg, :], mf[:], tf[:], ALU.add, [mf, tf], [mT])
                for tt_ in range(4):
                    r0 = c0 + tt_ * 128
                    xt, xn = xt_r.next(), xn_r.next()
                    s.dma("sp", xt[:], src[r0:r0 + 128, :], reads=[src], writes=[xt])
                    for half in range(2):
                        p = psx.next()
                        for cg in range(8):
                            self.mm(p[:, :], mT[:, cg, tt_ * 128:(tt_ + 1) * 128], wob[:, cg, half * 512:(half + 1) * 512], cg == 0, cg == 7, [mT, wob], [p])
                        self.tt("dve", xn[:, half * 512:(half + 1) * 512], p[:, :], xt[:, half * 512:(half + 1) * 512], ALU.add, [p, xt], [xn])
                    s.dma("pool", self.xres[r0:r0 + 128, :], xn[:], reads=[xn], writes=[self.xres])

    def phase_H(self, l):
        s, S = self.s, self.S
        SB = 1024
        NG = DFF // 128
        with self.scope() as st:
            hT = self.sb(st, "h2T", [128, 8, SB], BF16)
            aT = self.sb(st, "aT", [128, NG, SB], BF16)
            nt = self.norm_tiles(st)
            ps = Rot([self.psb(st, "psH") for _ in range(4)])
            wgb_r = Rot([self.sb(st, "wgbf", [128, 8, 128], BF16) for _ in range(3)])
            wub_r = Rot([self.sb(st, "wubf", [128, 8, 128], BF16) for _ in range(3)])
            wdb_r = Rot([self.sb(st, "wdb", [128, NG, 512], BF16) for _ in range(2)])
            sg_r = Rot([self.sb(st, "sg", [128, 512], BF16) for _ in range(3)])
            xt_r = Rot([self.sb(st, "xth", [128, 512], F32) for _ in range(3)])
            xn_r = Rot([self.sb(st, "xnh", [128, 512], F32) for _ in range(3)])

            def load_gu(g):
                wgb, wub = wgb_r.next(), wub_r.next()
                s.dma("pool", wgb[:], self.w_fg[l, :, g * 128:(g + 1) * 128].rearrange("(k p) n -> p k n", p=128), reads=[self.w_fg], writes=[wgb])
                s.dma("pool", wub[:], self.w_fu[l, :, g * 128:(g + 1) * 128].rearrange("(k p) n -> p k n", p=128), reads=[self.w_fu], writes=[wub])
                return wgb, wub

            def load_d(cq):
                wdb = wdb_r.next()
                for kk in range(2):
                    s.dma("pool", wdb[:, kk * 11:(kk + 1) * 11, :], self.w_fd[l, kk * 1408:(kk + 1) * 1408, cq * 512:(cq + 1) * 512].rearrange("(k p) n -> p k n", p=128),
                          reads=[self.w_fd], writes=[wdb])
                return wdb

            for sbk in range(S // SB):
                t0 = sbk * SB
                self.norm_to_hT(nt, self.xres, self.gffn, l, hT, t0, SB // 128)
                nxt = load_gu(0)
                for g in range(NG):
                    wgb, wub = nxt
                    if g + 1 < NG:
                        nxt = load_gu(g + 1)
                    for b in range(SB // 512):
                        pg, pu = ps.next(), ps.next()
                        for k in range(8):
                            self.mm(pg[:, :], wgb[:, k, :], hT[:, k, b * 512:(b + 1) * 512], k == 0, k == 7, [wgb, hT], [pg])
                        for k in range(8):
                            self.mm(pu[:, :], wub[:, k, :], hT[:, k, b * 512:(b + 1) * 512], k == 0, k == 7, [wub, hT], [pu])
                        sg = sg_r.next()
                        self.act(sg[:], pg[:, :], AF.Silu, [pg], [sg])
                        self.tt("dve", aT[:, g, b * 512:(b + 1) * 512], pu[:, :], sg[:], ALU.mult, [pu, sg], [aT])
                nxtd = load_d(0)
                for cq in range(2):
                    wdb = nxtd
                    if cq + 1 < 2:
                        nxtd = load_d(cq + 1)
                    for tt_ in range(SB // 128):
                        r0 = t0 + tt_ * 128
                        xt, xn = xt_r.next(), xn_r.next()
                        s.dma("sp", xt[:], self.xres[r0:r0 + 128, cq * 512:(cq + 1) * 512], reads=[self.xres], writes=[xt])
                        p = ps.next()
                        for k in range(NG):
                            self.mm(p[:, :], aT[:, k, tt_ * 128:(tt_ + 1) * 128], wdb[:, k, :], k == 0, k == NG - 1, [aT, wdb], [p])
                        self.tt("dve", xn[:], p[:, :], xt[:], ALU.add, [p, xt], [xn])
                        s.dma("pool", self.xres[r0:r0 + 128, cq * 512:(cq + 1) * 512], xn[:], reads=[xn], writes=[self.xres])

    def phase_final(self):
        s, S, NT = self.s, self.S, self.NT
        with self.scope() as st:
            gf = self.sb(st, "gf", [128, D], F32)
            s.dma("sp", gf[:], self.g_final[:].partition_broadcast(128), reads=[self.g_final], writes=[gf])
            xt_r = Rot([self.sb(st, "xtf", [128, D], F32) for _ in range(3)])
            yo_r = Rot([self.sb(st, "yof", [128, D], F32) for _ in range(3)])
            junk = self.sb(st, "junkf", [128, D], BF16)
            ss_r = Rot([self.sb(st, "ssf", [128, 1], F32) for _ in range(3)])
            for t in range(NT):
                xt, yo, ss = xt_r.next(), yo_r.next(), ss_r.next()
                s.dma("sp", xt[:], self.xres[t * 128:(t + 1) * 128, :], reads=[self.xres], writes=[xt])
                self.act(junk[:], xt[:], AF.Square, [xt], [junk, ss], accum_out=ss[:])
                self.act(ss[:], ss[:], AF.Sqrt, [ss, self.epsb], [ss], scale=1.0 / D, bias=self.epsb[:, 0:1])
                self.recip(ss[:], ss[:], [ss], [ss])
                self.stt(yo[:], xt[:], ss[:, 0:1], gf[:], ALU.mult, ALU.mult, [xt, ss, gf], [yo])
                s.dma("pool", self.y_out[t * 128:(t + 1) * 128, :], yo[:], reads=[yo], writes=[self.y_out])


def make_in_maps(inputs, S, batch_ids):
    f = lambda a: np.ascontiguousarray(np.asarray(a))
    consts = host_consts()
    shared = {
        "w_in": f(inputs["w_in"]), "w_qup": f(inputs["mla_w_q_up"]), "w_kvup": f(inputs["mla_w_kv_up"]),
        "w_br": f(inputs["w_branch"]), "w_out": f(inputs["w_out"]), "w_fg": f(inputs["w_ffn_gate"]),
        "w_fu": f(inputs["w_ffn_up"]), "w_fd": f(inputs["w_ffn_down"]),
        "g_attn": f(np.asarray(inputs["attn_norm_g"]).reshape(NLAYER, 8, 128).transpose(2, 0, 1)),
        "g_ffn": f(np.asarray(inputs["ffn_norm_g"]).reshape(NLAYER, 8, 128).transpose(2, 0, 1)),
        "g_final": f(inputs["final_norm_g"]),
        "g_q": f(np.asarray(inputs["mla_q_norm_g"]).reshape(NLAYER, 2, 128).transpose(2, 0, 1)),
        "g_kv": f(np.asarray(inputs["mla_kv_norm_g"]).transpose(1, 0)),
        "conv_w": f(np.asarray(inputs["conv_w"]).reshape(NLAYER, 3, 2, 128).transpose(3, 0, 2, 1)),
        "gla_wg": f(inputs["gla_w_gate_up"]),
        "gla_bg": f(np.asarray(inputs["gla_b_gate"]).reshape(NLAYER, 1, 128)),
        "gla_ng": f(inputs["gla_norm_g"]),
    }
    shared.update(consts)
    maps = []
    x = np.asarray(inputs["x"])
    pos = np.asarray(inputs["positions"])
    for b in batch_ids:
        m = dict(shared)
        m["x"] = f(x[b, :S])
        m["pos"] = f(pos[b, :S].astype(np.int32))
        maps.append(m)
    return maps


_NC_CACHE = {}


def kernel(**inputs):
    S = 4096
    key = ("full", S)
    if key not in _NC_CACHE:
        _NC_CACHE[key] = Builder(S, NLAYER).build()
    nc = _NC_CACHE[key]
    in_maps = make_in_maps(inputs, S, list(range(8)))
    res = run_bass_kernel_spmd(nc, in_maps, core_ids=list(range(8)))
    out = np.stack([np.asarray(r["y"]) for r in res.results], axis=0).astype(np.float32)
    return out
```
